# Optimizing a Trainium2 kernel written in Bass

```python
import jax, jax.numpy as jnp
from jax import lax
import numpy as np


D_MODEL = 1024
BATCH = 2
SEQ = 8192
DEPTH = 1

HEAD_DIM = 64
A_HEADS = 8
A_WIDTH = A_HEADS * HEAD_DIM
B_HEADS = 8
B_WIDTH = B_HEADS * HEAD_DIM
B_KV = 2
B_HPG = B_HEADS // B_KV
KV_W = B_KV * HEAD_DIM
N_GATES = B_HEADS * 3
CHUNK = 128
L_CMP = 32
STRIDE_CMP = 16
CMP_HIDDEN = 256
L_SEL = 64
N_SEL = 16
WIN = 512
Q_BLOCK = 128
ROPE_THETA = 10000.0
N_GROUPS = 4
EXPERTS_PER_GROUP = 4
N_EXPERTS = N_GROUPS * EXPERTS_PER_GROUP
TOP_K_IN_GROUP = 2
D_FF_EXPERT = 512
D_PLE = 256
EPS = 1e-6
NEG = -1e30
FORCE = 1e6

OFF_Q = 2 * A_WIDTH
OFF_KV = OFF_Q + B_WIDTH
OFF_GATE = OFF_KV + 6 * KV_W
D_IN = OFF_GATE + N_GATES

kernel_name = 'hymba_gmlp_nsa_hiermoe_ple'


def rmsnorm(x, g):
    xf = x.astype(jnp.float32)
    y = xf * lax.rsqrt(jnp.mean(xf * xf, axis=-1, keepdims=True) + EPS)
    return (y * g.astype(jnp.float32)).astype(x.dtype)


def rope(x, pos):
    half = HEAD_DIM // 2
    inv = 1.0 / (ROPE_THETA ** (jnp.arange(half, dtype=jnp.float32) / half))
    ang = pos.astype(jnp.float32)[:, None] * inv[None, :]
    cos = jnp.cos(ang)[None, :, None, :]
    sin = jnp.sin(ang)[None, :, None, :]
    xf = x.astype(jnp.float32)
    x1, x2 = xf[..., :half], xf[..., half:]
    return jnp.concatenate([x1 * cos - x2 * sin, x1 * sin + x2 * cos], axis=-1).astype(x.dtype)


def masked_softmax(s, mask):
    s = jnp.where(mask, s.astype(jnp.float32), NEG)
    m = jnp.max(s, axis=-1, keepdims=True)
    e = jnp.where(mask, jnp.exp(s - m), 0.0)
    return e / jnp.maximum(jnp.sum(e, axis=-1, keepdims=True), 1e-30)


def chunked_gmlp(u, v, g_v, w_s, b_s):
    bsz, t_len, _ = u.shape
    v = rmsnorm(v, g_v).reshape(bsz, t_len // CHUNK, CHUNK, A_HEADS, HEAD_DIM)
    causal = jnp.tril(jnp.ones((CHUNK, CHUNK), dtype=bool))
    w = jnp.where(causal[None], w_s, 0.0).astype(v.dtype)
    mixed = jnp.einsum('gts,bcsgd->bctgd', w, v) + b_s.T[None, None, :, :, None].astype(v.dtype)
    return u * mixed.reshape(bsz, t_len, A_WIDTH)


def compress(kx, pe, w1, w2):
    bsz, g, t_len, _ = kx.shape
    nc = (t_len - L_CMP) // STRIDE_CMP + 1
    idx = jnp.arange(nc)[:, None] * STRIDE_CMP + jnp.arange(L_CMP)[None, :]
    blocks = kx[:, :, idx] + pe
    flat = blocks.reshape(bsz, g, nc, L_CMP * HEAD_DIM)
    return jax.nn.gelu(flat @ w1) @ w2


def nsa_attention(q, k_cmp, v_cmp, k_slc, v_slc, k_win, v_win, gates,
                  pe_k, w1_k, w2_k, pe_v, w1_v, w2_v):
    bsz, t_len = q.shape[0], q.shape[1]
    qh = q.reshape(bsz, t_len, B_KV, B_HPG, HEAD_DIM).transpose(0, 2, 3, 1, 4)
    gh = gates.reshape(bsz, t_len, B_KV, B_HPG, 3).transpose(0, 2, 3, 1, 4)
    to_g = lambda a: a.transpose(0, 2, 1, 3)

    kc = compress(to_g(k_cmp), pe_k, w1_k, w2_k)
    vc = compress(to_g(v_cmp), pe_v, w1_v, w2_v)
    nc = kc.shape[2]
    c_start = jnp.arange(nc) * STRIDE_CMP
    cmp_end = c_start + L_CMP - 1

    n_sb = t_len // L_SEL
    k_top = min(N_SEL, n_sb)
    s_start = jnp.arange(n_sb) * L_SEL
    overlap = ((c_start[:, None] < s_start[None, :] + L_SEL)
               & (c_start[:, None] + L_CMP > s_start[None, :])).astype(jnp.float32)
    ks_blocks = to_g(k_slc).reshape(bsz, B_KV, n_sb, L_SEL, HEAD_DIM)
    vs_blocks = to_g(v_slc).reshape(bsz, B_KV, n_sb, L_SEL, HEAD_DIM)

    pad = ((0, 0), (0, 0), (WIN, 0), (0, 0))
    kw_pad = jnp.pad(to_g(k_win), pad)
    vw_pad = jnp.pad(to_g(v_win), pad)
    b_idx = jnp.arange(bsz)[:, None, None, None]
    g_idx = jnp.arange(B_KV)[None, :, None, None]
    blk = jnp.arange(n_sb)

    def block(qb):
        start = qb * Q_BLOCK
        qi = lax.dynamic_slice_in_dim(qh, start, Q_BLOCK, axis=3)
        gi = lax.dynamic_slice_in_dim(gh, start, Q_BLOCK, axis=3).astype(jnp.float32)
        t = start + jnp.arange(Q_BLOCK)

        s_c = jnp.einsum('bghqd,bgcd->bghqc', qi, kc)
        p_c = masked_softmax(s_c, cmp_end[None, :] <= t[:, None])
        o_c = jnp.einsum('bghqc,bgcd->bghqd', p_c.astype(vc.dtype), vc)

        imp = jnp.einsum('bgqc,cs->bgqs', jnp.sum(p_c, axis=2), overlap)
        cur = (t // L_SEL)[:, None]
        forced = (blk[None, :] == 0) | (blk[None, :] == cur) | (blk[None, :] == cur - 1)
        valid = s_start[None, :] <= t[:, None]
        score = jnp.where(forced, FORCE, jnp.where(valid, imp, -FORCE))
        _, sel = lax.top_k(score, k_top)

        kg = ks_blocks[b_idx, g_idx, sel]
        vg = vs_blocks[b_idx, g_idx, sel]
        s_s = jnp.einsum('bghqd,bgqkld->bghqkl', qi, kg)
        pos = sel[..., None] * L_SEL + jnp.arange(L_SEL)
        m_s = (pos <= t[None, None, :, None, None])[:, :, None]
        flat_shape = s_s.shape[:4] + (k_top * L_SEL,)
        p_s = masked_softmax(s_s.reshape(flat_shape),
                             m_s.reshape(bsz, B_KV, 1, Q_BLOCK, k_top * L_SEL)).reshape(s_s.shape)
        o_s = jnp.einsum('bghqkl,bgqkld->bghqd', p_s.astype(vg.dtype), vg)

        kwi = lax.dynamic_slice_in_dim(kw_pad, start, Q_BLOCK + WIN, axis=2)
        vwi = lax.dynamic_slice_in_dim(vw_pad, start, Q_BLOCK + WIN, axis=2)
        kpos = start - WIN + jnp.arange(Q_BLOCK + WIN)
        diff = t[:, None] - kpos[None, :]
        m_w = (diff >= 0) & (diff < WIN) & (kpos[None, :] >= 0)
        s_w = jnp.einsum('bghqd,bgkd->bghqk', qi, kwi)
        p_w = masked_softmax(s_w, m_w)
        o_w = jnp.einsum('bghqk,bgkd->bghqd', p_w.astype(vwi.dtype), vwi)

        o = (gi[..., 0:1] * o_c.astype(jnp.float32) + gi[..., 1:2] * o_s.astype(jnp.float32)
             + gi[..., 2:3] * o_w.astype(jnp.float32))
        return o.astype(q.dtype)

    out = lax.map(block, jnp.arange(t_len // Q_BLOCK))
    return out.transpose(1, 0, 4, 2, 3, 5).reshape(bsz, t_len, B_WIDTH)


def hybrid_mixer(hn, w_in, g_v, w_s, b_s, pe_k, w1_k, w2_k, pe_v, w1_v, w2_v, g_out_a, g_out_b, w_o):
    bsz, t_len, _ = hn.shape
    z = hn @ w_in
    zu = jax.nn.gelu(z[..., :2 * A_WIDTH])
    u, v = zu[..., :A_WIDTH], zu[..., A_WIDTH:]
    pos = jnp.arange(t_len)
    q = rope(z[..., OFF_Q:OFF_KV].reshape(bsz, t_len, B_HEADS, HEAD_DIM), pos) * (HEAD_DIM ** -0.5)
    kv = [z[..., OFF_KV + j * KV_W:OFF_KV + (j + 1) * KV_W].reshape(bsz, t_len, B_KV, HEAD_DIM)
          for j in range(6)]
    k_cmp, v_cmp, k_slc, v_slc, k_win, v_win = kv
    k_cmp, k_slc, k_win = rope(k_cmp, pos), rope(k_slc, pos), rope(k_win, pos)
    gates = jax.nn.sigmoid(z[..., OFF_GATE:D_IN].astype(jnp.float32)).reshape(bsz, t_len, B_HEADS, 3)

    oa = chunked_gmlp(u, v, g_v, w_s, b_s)
    ob = nsa_attention(q, k_cmp, v_cmp, k_slc, v_slc, k_win, v_win, gates,
                       pe_k, w1_k, w2_k, pe_v, w1_v, w2_v)
    cat = jnp.concatenate([rmsnorm(oa, g_out_a), rmsnorm(ob, g_out_b)], axis=-1)
    return cat @ w_o


def hier_moe(hn, r_group, r_expert, w_gate, w_up, w_down):
    bsz, t_len, d = hn.shape
    xt = hn.reshape(-1, d)
    pg = jax.nn.softmax((xt @ r_group).astype(jnp.float32), axis=-1)
    pg_top, g_sel = lax.top_k(pg, 1)
    le = (xt @ r_expert).astype(jnp.float32).reshape(-1, N_GROUPS, EXPERTS_PER_GROUP)
    le_sel = jnp.take_along_axis(le, g_sel[:, :, None], axis=1)[:, 0]
    pe = jax.nn.softmax(le_sel, axis=-1)
    pe_top, e_sel = lax.top_k(pe, TOP_K_IN_GROUP)
    w = pg_top * pe_top / jnp.sum(pe_top, axis=-1, keepdims=True)
    eid = g_sel * EXPERTS_PER_GROUP + e_sel
    comb = jnp.einsum('nk,nke->ne', w, jax.nn.one_hot(eid, N_EXPERTS, dtype=jnp.float32))
    y = jnp.zeros(xt.shape, jnp.float32)
    for e in range(N_EXPERTS):
        hid = jax.nn.silu(xt @ w_gate[e]) * (xt @ w_up[e])
        y = y + comb[:, e:e + 1] * (hid @ w_down[e]).astype(jnp.float32)
    return y.astype(hn.dtype).reshape(bsz, t_len, d)


def setup_inputs(seed: int = 0) -> dict:
    key = jax.random.key(seed)
    ks = jax.random.split(key, 26)
    f = jnp.float32
    nrm = lambda k, shape, s: jax.random.normal(k, shape, f) * s
    gain = lambda k, shape: 1.0 + 0.02 * jax.random.normal(k, shape, f)
    return {
        'x': nrm(ks[0], (BATCH, SEQ, D_MODEL), 1.0),
        'p': nrm(ks[1], (DEPTH, BATCH, SEQ, D_PLE), 1.0),
        'norm_mix': gain(ks[2], (DEPTH, D_MODEL)),
        'w_in': nrm(ks[3], (DEPTH, D_MODEL, D_IN), D_MODEL ** -0.5),
        'gmlp_v_norm': gain(ks[4], (DEPTH, A_WIDTH)),
        'gmlp_w_s': nrm(ks[5], (DEPTH, A_HEADS, CHUNK, CHUNK), CHUNK ** -0.5),
        'gmlp_b_s': gain(ks[6], (DEPTH, A_HEADS, CHUNK)),
        'cmp_pe_k': nrm(ks[7], (DEPTH, L_CMP, HEAD_DIM), 0.02),
        'cmp_w1_k': nrm(ks[8], (DEPTH, L_CMP * HEAD_DIM, CMP_HIDDEN), (L_CMP * HEAD_DIM) ** -0.5),
        'cmp_w2_k': nrm(ks[9], (DEPTH, CMP_HIDDEN, HEAD_DIM), CMP_HIDDEN ** -0.5),
        'cmp_pe_v': nrm(ks[10], (DEPTH, L_CMP, HEAD_DIM), 0.02),
        'cmp_w1_v': nrm(ks[11], (DEPTH, L_CMP * HEAD_DIM, CMP_HIDDEN), (L_CMP * HEAD_DIM) ** -0.5),
        'cmp_w2_v': nrm(ks[12], (DEPTH, CMP_HIDDEN, HEAD_DIM), CMP_HIDDEN ** -0.5),
        'out_norm_a': gain(ks[13], (DEPTH, A_WIDTH)),
        'out_norm_b': gain(ks[14], (DEPTH, B_WIDTH)),
        'w_o': nrm(ks[15], (DEPTH, A_WIDTH + B_WIDTH, D_MODEL), (A_WIDTH + B_WIDTH) ** -0.5),
        'norm_moe': gain(ks[16], (DEPTH, D_MODEL)),
        'router_group': nrm(ks[17], (DEPTH, D_MODEL, N_GROUPS), D_MODEL ** -0.5),
        'router_expert': nrm(ks[18], (DEPTH, D_MODEL, N_EXPERTS), D_MODEL ** -0.5),
        'moe_w_gate': nrm(ks[19], (DEPTH, N_EXPERTS, D_MODEL, D_FF_EXPERT), D_MODEL ** -0.5),
        'moe_w_up': nrm(ks[20], (DEPTH, N_EXPERTS, D_MODEL, D_FF_EXPERT), D_MODEL ** -0.5),
        'moe_w_down': nrm(ks[21], (DEPTH, N_EXPERTS, D_FF_EXPERT, D_MODEL), D_FF_EXPERT ** -0.5),
        'norm_ple': gain(ks[22], (DEPTH, D_MODEL)),
        'w_ple_proj': nrm(ks[23], (DEPTH, D_PLE, D_MODEL), D_PLE ** -0.5),
        'w_ple_gate': nrm(ks[24], (DEPTH, D_MODEL, D_MODEL), D_MODEL ** -0.5),
        'norm_final': gain(ks[25], (D_MODEL,)),
    }


def reference(x, p, norm_mix, w_in, gmlp_v_norm, gmlp_w_s, gmlp_b_s,
              cmp_pe_k, cmp_w1_k, cmp_w2_k, cmp_pe_v, cmp_w1_v, cmp_w2_v,
              out_norm_a, out_norm_b, w_o, norm_moe, router_group, router_expert,
              moe_w_gate, moe_w_up, moe_w_down, norm_ple, w_ple_proj, w_ple_gate, norm_final):
    h = x
    for i in range(DEPTH):
        hn = rmsnorm(h, norm_mix[i])
        h = h + hybrid_mixer(hn, w_in[i], gmlp_v_norm[i], gmlp_w_s[i], gmlp_b_s[i],
                             cmp_pe_k[i], cmp_w1_k[i], cmp_w2_k[i],
                             cmp_pe_v[i], cmp_w1_v[i], cmp_w2_v[i],
                             out_norm_a[i], out_norm_b[i], w_o[i])
        h = h + hier_moe(rmsnorm(h, norm_moe[i]), router_group[i], router_expert[i],
                         moe_w_gate[i], moe_w_up[i], moe_w_down[i])
        gate = jax.nn.sigmoid(rmsnorm(h, norm_ple[i]) @ w_ple_gate[i])
        h = h + (p[i] @ w_ple_proj[i]) * gate
    return rmsnorm(h, norm_final)
```

```python
from contextlib import ExitStack
import types
import numpy as np
import concourse.bass as bass
import concourse.mybir as mybir
from concourse.bass_utils import run_bass_kernel_spmd

F32 = mybir.dt.float32
BF16 = mybir.dt.bfloat16
AF = mybir.ActivationFunctionType
ALU = mybir.AluOpType
AX = mybir.AxisListType

T = 8192
D = 1024
NT = 64
NOWN = 16
EPS = 1e-6
FORCE = 1e6
ENG = ("pe", "act", "dve", "pool", "sp")
SKIP = set()


def _freeze(fn):
    if fn.__closure__ is None:
        return fn
    cells = []
    for c in fn.__closure__:
        try:
            cells.append(types.CellType(c.cell_contents))
        except ValueError:
            cells.append(c)
    return types.FunctionType(fn.__code__, fn.__globals__, fn.__name__, fn.__defaults__, tuple(cells))


class Prog:
    def __init__(self, nc):
        self.nc = nc
        self.ops = []
        self.last_w = {}
        self.readers = {}
        self.stack = ExitStack()
        self.group_keys = set()
        self.pending_alias = {}
        self.ranges = {}
        self._ovl_cache = {}

    def sb(self, name, shape, dt):
        return self.stack.enter_context(self.nc.sbuf_tensor(name, list(shape), dt))

    def ps(self, name, shape, dt):
        return self.stack.enter_context(self.nc.psum_tensor(name, list(shape), dt))

    def reg(self, keys, lo, hi):
        for k in keys:
            if k in self.ranges:
                a, b = self.ranges[k]
                lo2, hi2 = min(a, lo), max(b, hi)
            else:
                lo2, hi2 = lo, hi
            self.ranges[k] = (lo2, hi2)
        self._ovl_cache = {}

    def overlaps(self, k):
        if k not in self.ranges:
            return ()
        if k not in self._ovl_cache:
            lo, hi = self.ranges[k]
            self._ovl_cache[k] = [k2 for k2, (a, b) in self.ranges.items() if k2 != k and a < hi and lo < b]
        return self._ovl_cache[k]

    def alias(self, newkey, oldkeys):
        self.pending_alias.setdefault(newkey, []).extend(oldkeys)

    @staticmethod
    def _is_psum(k):
        return isinstance(k, str) and len(k) == 2 and k[0] == "B" and k[1].isdigit()

    def op(self, eng, fn, r=(), w=(), dma_key=None):
        i = len(self.ops)
        deps = set()
        w = list(w)
        r = list(r)
        xw = [k for k in r if self._is_psum(k) and k not in w]
        extra = []
        for k in w:
            if k in self.pending_alias:
                extra.extend(self.pending_alias.pop(k))
            extra.extend(self.overlaps(k))
        for k in r:
            if k in self.last_w:
                deps.add(self.last_w[k])
        for k in w + extra + xw:
            if k in self.last_w:
                deps.add(self.last_w[k])
            for j in self.readers.get(k, ()):
                deps.add(j)
        deps.discard(i)
        self.ops.append(dict(eng=eng, fn=_freeze(fn), deps=deps, dma=dma_key, r=tuple(r), w=tuple(w)))
        for k in r:
            self.readers.setdefault(k, []).append(i)
        for k in w:
            self.last_w[k] = i
            self.readers[k] = []
        for k in xw:
            self.last_w[k] = i
            self.readers[k] = []
        return i

    def dma(self, eng, out, in_, r=(), w=(), key=None, group=False, **kw):
        if key is None:
            key = w[0] if len(w) else r[0]
        if group:
            self.group_keys.add(key)
        return self.op(eng, lambda e: e.dma_start(out=out, in_=in_, **kw), r=r, w=w, dma_key=key)

    def finalize(self, final_wait_eng="sp"):
        nc = self.nc
        ops = self.ops
        n = len(ops)
        needed = [False] * n
        for i, o in enumerate(ops):
            keep = set()
            for d in o["deps"]:
                od = ops[d]
                if od["dma"] is None and o["dma"] is None and od["eng"] == o["eng"]:
                    if o["eng"] == "pe":
                        continue
                    if not (set(od["w"]) & (set(o["r"]) | set(o["w"]))):
                        continue
                keep.add(d)
            o["deps"] = keep
            for d in keep:
                needed[d] = True
        final_dma = [i for i, o in enumerate(ops) if o["dma"] is not None and not needed[i]]
        for i in final_dma:
            needed[i] = True
        sems = {}
        cnt = {}
        names = {}

        def sem_name(key):
            if key not in names:
                names[key] = "d%d" % len(names)
            return names[key]

        for i, o in enumerate(ops):
            if not needed[i]:
                o["sig"] = None
                continue
            if o["dma"] is not None:
                sname = sem_name(o["dma"])
                inc = 16
            else:
                sname = "e_" + o["eng"]
                inc = 1
            cnt[sname] = cnt.get(sname, 0) + inc
            o["sig"] = (sname, inc, cnt[sname])
            if sname not in sems:
                sems[sname] = self.stack.enter_context(nc.semaphore(sname))
        gfinal = {sem_name(k): cnt.get(sem_name(k), 0) for k in self.group_keys}
        self.n_sems = len(sems)
        plan = {e: [] for e in ENG}
        waited = {e: {} for e in ENG}
        for i, o in enumerate(ops):
            e = o["eng"]
            waits = {}
            for d in o["deps"]:
                sname, inc, val = ops[d]["sig"]
                if sname in gfinal:
                    val = gfinal[sname]
                if waited[e].get(sname, 0) >= val:
                    continue
                waits[sname] = max(waits.get(sname, 0), val)
            for sname, val in waits.items():
                waited[e][sname] = val
            plan[e].append((o, waits))
        final_waits = {}
        for i in final_dma:
            sname, inc, val = ops[i]["sig"]
            final_waits[sname] = max(final_waits.get(sname, 0), val)
        blk = self.stack.enter_context(nc.Block())

        def emit(engobj, ename):
            for o, waits in plan[ename]:
                for sname, val in waits.items():
                    engobj.wait_ge(sems[sname], val)
                ins = o["fn"](engobj)
                if o["sig"] is not None:
                    ins.then_inc(sems[o["sig"][0]], o["sig"][1])
            if ename == final_wait_eng:
                for sname, val in final_waits.items():
                    engobj.wait_ge(sems[sname], val)

        @blk.tensor
        def _(e):
            emit(e, "pe")

        @blk.scalar
        def _(e):
            emit(e, "act")

        @blk.vector
        def _(e):
            emit(e, "dve")

        @blk.gpsimd
        def _(e):
            emit(e, "pool")

        @blk.sync
        def _(e):
            emit(e, "sp")

        self.stack.close()
        return nc


def build_program(dbg=False, stop_after=None):
    nc = bass.Bass("TRN2", target_bir_lowering=False)
    P = Prog(nc)

    in_names = []
    late = stop_after in (None, "B2")

    def din(name, shape, big=False):
        if big and not late:
            shape = [1] * len(shape)
        in_names.append(name)
        return nc.dram_tensor(name, list(shape), F32, kind="ExternalInput").ap()

    def dout(name, shape):
        return nc.dram_tensor(name, list(shape), F32, kind="ExternalOutput").ap()

    xT_d = din("xT", [D, T])
    xTo_d = din("xTo", [NOWN, D, 128])
    xo_d = din("xo", [NOWN, 128, D], big=True)
    pTo_d = din("pTo", [256, NOWN * 128], big=True)
    cs_d = din("cs", [T, 64])
    cso_d = din("cso", [NOWN * 128, 64])
    wkv_d = din("wkv", [D, 768])
    wown_d = din("wown", [D, 1560])
    gmix_d = din("gmix", [128, 8])
    wsT_d = din("wsT", [128, 8 * 128])
    tri_d = din("tri", [128, 128])
    bsT_d = din("bsT", [128, 8])
    gv_d = din("gv", [128, 512])
    ga_d = din("ga", [128, 512])
    gb_d = din("gb", [128, 512])
    gmoe_d = din("gmoe", [128, D], big=True)
    gple_d = din("gple", [128, D], big=True)
    gfin_d = din("gfin", [128, D], big=True)
    w1_d = [din("w1k", [64, 32 * 256]), din("w1v", [64, 32 * 256])]
    peT_d = [din("peTk", [64, 32]), din("peTv", [64, 32])]
    w2_d = [din("w2k", [256, 64]), din("w2v", [256, 64])]
    wo_d = din("wo", [D, D], big=True)
    rcat_d = din("rcat", [D, 20])
    wg_d = din("moe_wg", [16, D, 512], big=True)
    wu_d = din("moe_wu", [16, D, 512], big=True)
    wd_d = din("moe_wd", [16, 512, D], big=True)
    wpp_d = din("wpp", [256, D], big=True)
    wpg_d = din("wpg", [D, D], big=True)
    identf_d = din("identf", [128, 128])
    ewide_d = din("ewide", [128, 64 * 128])
    ovl_d = din("ovl", [512, 128])
    mC_d = din("mC", [NOWN, 128, 2 * 128])
    m12_d = din("m12", [NOWN, 128, 256])
    dmwm_d = din("dmwm", [128, 12 * 128])
    out_d = dout("out", [NOWN, 128, D])
    dbg_d = {}

    def dbg_out(name, shape):
        if dbg:
            dbg_d[name] = dout("dbg_" + name, shape)
        return dbg_d.get(name)

    KB = 1024
    ARENA = 192 * KB
    arena = P.sb("arena", [128, ARENA // 2], BF16)

    def view(off, shape, dt, keys=None):
        esz = 2 if dt == BF16 else 4
        nel = int(np.prod(shape[1:]))
        assert off % 4 == 0 and off + nel * esz <= ARENA, (off, shape)
        if keys is not None:
            P.reg(keys, off, off + nel * esz)
        a = arena[:, off // 2: off // 2 + nel * esz // 2]
        if dt != BF16:
            a = a.bitcast(dt)
        if len(shape) == 2:
            return a
        nm = " ".join("d%d" % i for i in range(1, len(shape)))
        kw = {"d%d" % i: shape[i] for i in range(2, len(shape))}
        return a.rearrange("p (%s) -> p %s" % (nm, nm), **kw)

    KVc = view(0, [128, 2, T], BF16, ["KVc"])
    Wown = view(0, [128, 8, 1560], BF16, ["Wown"])
    Wo = view(0, [128, 8, D], BF16, ["Wo"])
    Wkv = view(130 * KB + 48 * KB, [128, 8, 768], BF16, ["Wkv"])
    W1 = [view(32 * KB, [128, 32, 256], BF16, [("W1", 0)]), view(48 * KB, [128, 32, 256], BF16, [("W1", 1)])]
    catT = view(32 * KB, [128, 8, NOWN * 128], BF16, [("catT", i_) for i_ in range(NOWN)])
    KVs = view(64 * KB, [128, 2, T], BF16, ["KVs"])
    VTM_B = 64 * 2 * 65 * 2
    Vtm = [view(96 * KB, [128, 64, 2, 65], BF16, ["Vtm", "Vtm_one0"]), view(96 * KB + VTM_B, [128, 64, 2, 65], BF16, ["Vtm", "Vtm_one1"])]
    h1 = view(64 * KB, [128, NOWN, D], F32)
    for i_ in range(NOWN):
        P.reg([("h1", i_)], 64 * KB + i_ * 4096, 64 * KB + (i_ + 1) * 4096)
    h1 = h1
    TB = 130 * KB
    hn2T = view(130 * KB, [128, 8, NOWN * 128], BF16, [("hn2T", i_) for i_ in range(NOWN)])
    Wexp = [(view(0, [128, 8, 512], BF16, [("Wg", 0)]), view(8 * KB, [128, 8, 512], BF16, [("Wu", 0)]), view(16 * KB, [128, 4, D], BF16, [("Wd", 0)])),
            (view(32 * KB, [128, 8, 512], BF16, [("Wg", 1)]), view(40 * KB, [128, 8, 512], BF16, [("Wu", 1)]), view(48 * KB, [128, 4, D], BF16, [("Wd", 1)]))]
    Wpg = view(130 * KB, [128, 8, D], BF16, ["Wpg"])
    Wpp = view(146 * KB, [128, 2, D], BF16, ["Wpp"])

    identb = P.sb("identb", [128, 128], BF16)
    identf = P.sb("identf_s", [128, 128], F32)
    ones = P.sb("ones", [128, 16], BF16)
    trib = P.sb("trib", [128, 128], BF16)
    wsT = P.sb("wsT_s", [128, 8, 128], BF16)
    bsT = P.sb("bsT_s", [128, 8], F32)
    gmix = P.sb("gmix_s", [128, 8], F32)
    gv = P.sb("gv_s", [128, 512], F32)
    ga = P.sb("ga_s", [128, 512], F32)
    gb = P.sb("gb_s", [128, 512], F32)
    kcT = P.sb("kcT", [128, 512], BF16)
    vcx = P.sb("vcx", [128, 4, 2, 194], BF16)
    w2 = [P.sb("w2k_s", [128, 2, 64], BF16), P.sb("w2v_s", [128, 2, 64], BF16)]
    peT = [P.sb("peTk_s", [64, 32], BF16), P.sb("peTv_s", [64, 32], BF16)]
    cbias = P.sb("cbias", [128, 4], F32)
    rcat = P.sb("rcat_s", [128, 8, 20], F32)
    comb = P.sb("comb", [128, NOWN, 16], F32)
    pad8 = P.sb("pad8", [128, 8], F32)

    PS = P.ps("PS", [128, 4096], F32)

    def bank(k, lo=0, hi=512):
        return PS[:, k * 512 + lo: k * 512 + hi]

    def bankb(k):
        return PS[:, k * 512:(k + 1) * 512].bitcast(BF16)

    def bk(k, slots=None):
        return ["B%d" % k]

    def bc_mid(ap2d, n):
        return ap2d.unsqueeze(1).to_broadcast([128, n, ap2d.shape[-1]])

    def bc_last(ap2d, n):
        return ap2d.unsqueeze(2).to_broadcast([128, ap2d.shape[-1], n])

    def cload(eng, dst, src, key):
        P.dma(eng, dst, src, w=[key], key="const_" + eng, group=True)

    cload("pool", identb[:], identf_d, "identb")
    cload("sp", identf[:], identf_d, "identf")
    cload("pool", trib[:], tri_d, "trib")
    cload("pool", wsT[:], wsT_d.rearrange("p (g t) -> p g t", t=128), "wsT")
    cload("sp", bsT[:], bsT_d, "bsT")
    cload("sp", gmix[:], gmix_d, "gmix")
    cload("sp", gv[:], gv_d, "gv")
    cload("sp", ga[:], ga_d, "ga")
    cload("sp", gb[:], gb_d, "gb")
    for kv in range(2):
        cload("pool", w2[kv][:], w2_d[kv].rearrange("(h p) d -> p h d", p=128), "w2%d" % kv)
        cload("pool", peT[kv][:], peT_d[kv], "peT%d" % kv)
    cload("sp", rcat[:], rcat_d.rearrange("(c p) n -> p c n", p=128), "rcat")
    P.op("pool", lambda e: e.memset(ones[:], 1.0), w=["ones"])
    P.op("pool", lambda e: e.memset(pad8[:], -1e30), w=["pad8"])
    P.op("pool", lambda e: e.memset(vcx[:].rearrange("p a g d -> p (a g d)"), 1.0), w=["vcx_one"])
    P.op("pool", lambda e: e.memset(Vtm[0].rearrange("p a g d -> p (a g d)"), 1.0), w=["Vtm_one0"])
    P.op("pool", lambda e: e.memset(Vtm[1].rearrange("p a g d -> p (a g d)"), 1.0), w=["Vtm_one1"])
    for g in range(2):
        for ct in range(4):
            P.dma("pool", vcx[:, ct, g, 66:194], ovl_d[ct * 128:(ct + 1) * 128, :], r=["vcx_one"], w=["vcx_ovl"], key="vcx_ovl")
    if "wst" not in SKIP:
        P.op("dve", lambda e: e.tensor_tensor(out=wsT[:], in0=wsT[:], in1=bc_mid(trib[:, :], 8), op=ALU.mult),
             r=["wsT", "trib"], w=["wsT"])

    P.dma("pool", Wkv, wkv_d.rearrange("(c p) n -> p c n", p=128), w=["Wkv"])
    for dc in range(8 if "wkvs" not in SKIP else 0):
        P.op("dve", lambda e, dc=dc: e.tensor_scalar(out=Wkv[:, dc, :], in0=Wkv[:, dc, :], scalar1=gmix[:, dc:dc + 1],
                                                      scalar2=None, op0=ALU.mult), r=["Wkv", "gmix"], w=["Wkv"])

    if stop_after == "const":
        P.finalize()
        nc.in_names = in_names
        return nc

    def rstd_from_ssq(ssq_ap, n, dst, rkeys, wkey):
        P.op("act", lambda e: e.activation(out=dst, in_=ssq_ap, func=AF.Sqrt, scale=1.0 / n, bias=EPS), r=rkeys, w=[wkey])
        P.op("dve", lambda e: e.reciprocal(out=dst, in_=dst), r=[wkey], w=[wkey])

    def rope_tm(src_ps, nblk, csr, dst_bf, tmp, rkeys, tkey, wkey):
        s4 = src_ps.rearrange("p (b h d) -> p b h d", h=2, d=32)
        d4 = dst_bf.rearrange("p (b h d) -> p b h d", h=2, d=32)
        cr = csr[:, 0:32].unsqueeze(1).to_broadcast([128, nblk, 32])
        sr = csr[:, 32:64].unsqueeze(1).to_broadcast([128, nblk, 32])
        tv = [tmp[:, k, :].rearrange("p (b d) -> p b d", d=32) for k in range(4)]
        P.op("dve", lambda e: e.tensor_tensor(out=tv[0], in0=s4[:, :, 0, :], in1=cr, op=ALU.mult), r=rkeys, w=[tkey + "0"])
        P.op("dve", lambda e: e.tensor_tensor(out=tv[1], in0=s4[:, :, 1, :], in1=sr, op=ALU.mult), r=rkeys, w=[tkey + "1"])
        P.op("dve", lambda e: e.tensor_tensor(out=tv[2], in0=s4[:, :, 0, :], in1=sr, op=ALU.mult), r=rkeys, w=[tkey + "2"])
        P.op("dve", lambda e: e.tensor_tensor(out=tv[3], in0=s4[:, :, 1, :], in1=cr, op=ALU.mult), r=rkeys, w=[tkey + "3"])
        P.op("pool", lambda e: e.tensor_tensor(out=d4[:, :, 0, :], in0=tv[0], in1=tv[1], op=ALU.subtract),
             r=[tkey + "0", tkey + "1"], w=[wkey + "a"])
        P.op("pool", lambda e: e.tensor_tensor(out=d4[:, :, 1, :], in0=tv[2], in1=tv[3], op=ALU.add),
             r=[tkey + "2", tkey + "3"], w=[wkey + "b"])

    A_xTb = [view(TB + 0, [128, 8, 256], BF16, [("xTb", 0)]), view(TB + 4 * KB, [128, 8, 256], BF16, [("xTb", 1)])]
    A_xf = [view(TB + 32 * KB, [128, 8, 256], F32, [("xf", 0)]), view(TB + 40 * KB, [128, 8, 256], F32, [("xf", 1)])]
    A_cs = [view(TB + 16 * KB, [128, 2, 64], F32, [("Acs", 0)]), view(TB + 17 * KB, [128, 2, 64], F32, [("Acs", 1)])]
    A_sq = [view(TB + 18 * KB, [128, 8, 128], BF16, [("Asq", 0)]), view(TB + 20 * KB, [128, 8, 128], BF16, [("Asq", 1)])]
    A_tmp = [view(TB + 22 * KB, [128, 4, 192], F32, ["Atmp0%d" % k_ for k_ in range(4)]), view(TB + 25 * KB, [128, 4, 192], F32, ["Atmp1%d" % k_ for k_ in range(4)])]
    A_ktm = [view(TB + 28 * KB, [128, 512], BF16, ["Aktm0a", "Aktm0b", "Aktm0v"]), view(TB + 29 * KB, [128, 512], BF16, ["Aktm1a", "Aktm1b", "Aktm1v"])]
    A_csr = [view(TB + 30 * KB, [128, 64], F32, [("Acsr", 0)]), view(TB + 30 * KB + 256, [128, 64], F32, [("Acsr", 1)])]
    A_rs = view(TB + 31 * KB, [128, 8], F32, [("Ars", 0), ("Ars", 1)])
    A_u = [view(TB + 8 * KB, [128, 384], F32, [("Au", 0, 0), ("Au", 0, 1)]), view(TB + 10 * KB, [128, 384], F32, [("Au", 1, 0), ("Au", 1, 1)])]

    for kv in range(2):
        for half in range(2):
            P.dma("pool", W1[kv][half * 64:(half + 1) * 64], w1_d[kv].rearrange("p (l h) -> p l h", h=256), w=[("W1", kv)])
    n_chunks = 32
    TPC = 2

    def A_front(ck, tl):
        cb = ck % 2
        tile = ck * TPC + tl
        tb = tile % 2
        xs = A_xTb[cb][:, :, tl * 128:(tl + 1) * 128]
        P.op("act", lambda e: e.activation(out=A_sq[tb], in_=xs, func=AF.Square), r=[("xTb", cb)], w=[("Asq", tb)])
        zb = 0 if tb == 0 else 2
        for dc in range(8):
            P.op("pe", lambda e, dc=dc: e.matmul(bank(4 + tb, 0, 1), lhsT=A_sq[tb][:, dc, :], rhs=ones[:, 0:1],
                                                 start=(dc == 0), stop=(dc == 7)), r=[("Asq", tb), "ones"], w=bk(4 + tb))
        for dc in range(8):
            P.op("pe", lambda e, dc=dc: e.matmul(bank(zb), lhsT=xs[:, dc, :], rhs=Wkv[:, dc, 0:512],
                                                 start=(dc == 0), stop=(dc == 7)), r=[("xTb", cb), "Wkv"], w=bk(zb))
        for dc in range(8):
            P.op("pe", lambda e, dc=dc: e.matmul(bank(zb + 1, 0, 256), lhsT=xs[:, dc, :], rhs=Wkv[:, dc, 512:768],
                                                 start=(dc == 0), stop=(dc == 7)), r=[("xTb", cb), "Wkv"], w=bk(zb + 1))

    def A_back(ck, tl):
        cb = ck % 2
        tile = ck * TPC + tl
        tb = tile % 2
        zb = 0 if tb == 0 else 2
        rs = A_rs[:, tb:tb + 1]
        s4 = bank(zb, 0, 384).rearrange("p (b h d) -> p b h d", h=2, d=32)
        cr = A_cs[cb][:, tl, 0:32].unsqueeze(1).to_broadcast([128, 6, 32])
        sr = A_cs[cb][:, tl, 32:64].unsqueeze(1).to_broadcast([128, 6, 32])
        tv = [A_tmp[tb][:, k, :].rearrange("p (b d) -> p b d", d=32) for k in range(4)]
        tk = "Atmp%d" % tb
        P.op("dve", lambda e: e.tensor_tensor(out=tv[0], in0=s4[:, :, 0, :], in1=cr, op=ALU.mult), r=bk(zb) + [("Acs", cb)], w=[tk + "0"])
        P.op("dve", lambda e: e.tensor_tensor(out=tv[1], in0=s4[:, :, 1, :], in1=sr, op=ALU.mult), r=bk(zb) + [("Acs", cb)], w=[tk + "1"])
        P.op("dve", lambda e: e.tensor_tensor(out=tv[2], in0=s4[:, :, 0, :], in1=sr, op=ALU.mult), r=bk(zb) + [("Acs", cb)], w=[tk + "2"])
        P.op("dve", lambda e: e.tensor_tensor(out=tv[3], in0=s4[:, :, 1, :], in1=cr, op=ALU.mult), r=bk(zb) + [("Acs", cb)], w=[tk + "3"])
        u4 = A_u[tb].rearrange("p (b h d) -> p b h d", h=2, d=32)
        P.op("pool", lambda e: e.tensor_tensor(out=u4[:, :, 0, :], in0=tv[0], in1=tv[1], op=ALU.subtract), r=[tk + "0", tk + "1"], w=[("Au", tb, 0)])
        P.op("pool", lambda e: e.tensor_tensor(out=u4[:, :, 1, :], in0=tv[2], in1=tv[3], op=ALU.add), r=[tk + "2", tk + "3"], w=[("Au", tb, 1)])
        rstd_from_ssq(bank(4 + tb, 0, 1), D, rs, bk(4 + tb), ("Ars", tb))
        P.op("act", lambda e: e.activation(out=A_ktm[tb][:, 0:384], in_=A_u[tb], func=AF.Copy, scale=rs),
             r=[("Au", tb, 0), ("Au", tb, 1), ("Ars", tb)], w=["Aktm%da" % tb, "Aktm%db" % tb])
        P.op("act", lambda e: e.activation(out=A_ktm[tb][:, 384:512], in_=bank(zb, 384, 512), func=AF.Copy, scale=rs),
             r=bk(zb) + [("Ars", tb)], w=["Aktm%dv" % tb])
        for j in range(2):
            P.op("act", lambda e, j=j: e.activation(
                out=Vtm[j][:, tile, :, 0:64], in_=bank(zb + 1, j * 128, (j + 1) * 128).rearrange("p (g d) -> p g d", d=64),
                func=AF.Copy, scale=rs), r=bk(zb + 1) + [("Ars", tb)], w=["Vtm"])
        tp = bankb(6 + tb)[:, 0:512].rearrange("p (a t) -> p a t", t=128)
        src_order = [0, 3, 1, 2]
        for a_, sblk in enumerate(src_order):
            P.op("pe", lambda e, a_=a_, sblk=sblk: e.transpose(tp[:, a_, :], A_ktm[tb][:, sblk * 128:(sblk + 1) * 128], identb[:]),
                 r=["Aktm%da" % tb, "Aktm%db" % tb, "Aktm%dv" % tb, "identb"], w=bk(6 + tb))
        P.op("dve", lambda e: e.tensor_copy(out=KVc[:, :, tile * 128:(tile + 1) * 128], in_=tp[:, 0:2, :]),
             r=bk(6 + tb), w=["KVc"])
        P.op("act", lambda e: e.activation(out=KVs[:, :, tile * 128:(tile + 1) * 128], in_=tp[:, 2:4, :], func=AF.Copy),
             r=bk(6 + tb), w=["KVs"])

    prev = None
    for ck in range(n_chunks):
        cb = ck % 2
        CT = 128 * TPC
        P.dma("sp", A_xf[cb], xT_d[:, ck * CT:(ck + 1) * CT].rearrange("(c p) t -> p c t", p=128), w=[("xf", cb)])
        P.dma("sp", A_cs[cb], cs_d[ck * CT:(ck + 1) * CT, :].rearrange("(a p) n -> p a n", p=128), w=[("Acs", cb)])
        P.op("pool", lambda e: e.tensor_copy(out=A_xTb[cb], in_=A_xf[cb]), r=[("xf", cb)], w=[("xTb", cb)])
        for tl in range(TPC):
            A_front(ck, tl)
            if prev is not None:
                A_back(*prev)
            prev = (ck, tl)
    A_back(*prev)

    if dbg:
        d_kvs = dbg_out("KVs", [128, 2 * T])
        d_kvc = dbg_out("KVc", [128, 2 * T])
        d_vtm = dbg_out("Vtm0", [128, 64 * 130])
        stg = view(TB + 32 * KB, [128, T // 2], F32, ["stg"])
        HT = T // 2
        for hh in range(2):
            for qq in range(2):
                P.op("dve", lambda e, hh=hh, qq=qq: e.tensor_copy(out=stg, in_=KVs[:, hh, qq * HT:(qq + 1) * HT]), r=["KVs"], w=["stg"])
                P.dma("sp", d_kvs[:, hh * T + qq * HT:hh * T + (qq + 1) * HT], stg, r=["stg"], key=("dbgo", 1))
                P.op("dve", lambda e, hh=hh, qq=qq: e.tensor_copy(out=stg, in_=KVc[:, hh, qq * HT:(qq + 1) * HT]), r=["KVc"], w=["stg"])
                P.dma("sp", d_kvc[:, hh * T + qq * HT:hh * T + (qq + 1) * HT], stg, r=["stg"], key=("dbgo", 2))
        for qq in range(4):
            P.op("dve", lambda e, qq=qq: e.tensor_copy(out=stg[:, 0:16 * 130], in_=Vtm[0][:, qq * 16:(qq + 1) * 16].rearrange("p a g d -> p (a g d)")), r=["Vtm", "Vtm_one0"], w=["stg"])
            P.dma("sp", d_vtm[:, qq * 16 * 130:(qq + 1) * 16 * 130], stg[:, 0:16 * 130], r=["stg"], key=("dbgo", 3))
    if stop_after == "A":
        P.finalize()
        nc.in_names = in_names
        return nc

    for kv in range(2):
        for half in range(2):
            col = kv * 2 + half
            for l in range(32):
                P.op("pe", lambda e, kv=kv, half=half, l=l, col=col: e.matmul(
                    bank(4, 8 + col, 9 + col), lhsT=W1[kv][0:64, l, half * 128:(half + 1) * 128], rhs=peT[kv][:, l:l + 1],
                    start=(l == 0), stop=(l == 31)), r=[("W1", kv), "peT%d" % kv], w=bk(4))
    P.op("dve", lambda e: e.tensor_copy(out=cbias[:], in_=bank(4, 8, 12)), r=bk(4), w=["cbias"])
    Ap_hid = [view(TB + 0, [128, 2, 512], BF16, ["hid0"]), view(TB + 2 * KB, [128, 2, 512], BF16, ["hid1"])]
    KVd = [view(TB + 12 * KB, [128, 16, 512], BF16, [("KVd", 0)]), view(TB + 28 * KB, [128, 16, 512], BF16, [("KVd", 1)])]
    P.op("dve", lambda e: e.tensor_copy(out=KVd[0], in_=KVc[:, 0, :].rearrange("p (m b) -> p b m", b=16)), r=["KVc"], w=[("KVd", 0)])
    P.op("pool", lambda e: e.tensor_copy(out=KVd[1], in_=KVc[:, 1, :].rearrange("p (m b) -> p b m", b=16)), r=["KVc"], w=[("KVd", 1)])
    P.alias("hid0", [("xTb", 0)])
    P.alias("hid1", [("xTb", 0)])
    P.op("pool", lambda e: e.memset(Ap_hid[0], 0.0), w=["hid0"])
    P.op("pool", lambda e: e.memset(Ap_hid[1], 0.0), w=["hid1"])
    it = 0
    for kv in range(2):
        for g in range(2):
            hb = it % 2
            it += 1
            hid = Ap_hid[hb]
            for half in range(2):
                hbank = half
                for l in range(32):
                    P.op("pe", lambda e, kv=kv, g=g, half=half, l=l, hbank=hbank: e.matmul(
                        bank(hbank, 0, 511), lhsT=W1[kv][g * 64:(g + 1) * 64, l, half * 128:(half + 1) * 128],
                        rhs=KVd[kv][g * 64:(g + 1) * 64, l % 16, (l // 16):(l // 16) + 511], start=(l == 0), stop=(l == 31)),
                        r=[("W1", kv), ("KVd", kv)], w=bk(hbank))
                P.op("act", lambda e, kv=kv, half=half, hbank=hbank, hid=hid: e.activation(
                    out=hid[:, half, 0:511], in_=bank(hbank, 0, 511), func=AF.Gelu_apprx_tanh, bias=cbias[:, kv * 2 + half:kv * 2 + half + 1]),
                    r=bk(hbank) + ["cbias"], w=["hid%d" % hb])
            if kv == 0:
                for half in range(2):
                    P.op("pe", lambda e, g=g, half=half, hid=hid: e.matmul(PS[g * 64:(g + 1) * 64, 2 * 512:3 * 512], lhsT=w2[0][:, half, :],
                                                                          rhs=hid[:, half, :], start=(half == 0), stop=(half == 1)),
                         r=["hid%d" % hb, "w20"], w=bk(2))
                P.op("dve", lambda e, g=g: e.tensor_copy(out=kcT[g * 64:(g + 1) * 64, :], in_=PS[g * 64:(g + 1) * 64, 2 * 512:3 * 512]),
                     r=bk(2), w=["kcT"])
            else:
                for ct in range(4):
                    for half in range(2):
                        P.op("pe", lambda e, ct=ct, half=half, hid=hid: e.matmul(bank(3, ct * 64, (ct + 1) * 64), lhsT=hid[:, half, ct * 128:(ct + 1) * 128],
                                                                                rhs=w2[1][:, half, :], start=(half == 0), stop=(half == 1)),
                             r=["hid%d" % hb, "w21"], w=bk(3))
                P.op("dve", lambda e, g=g: e.tensor_copy(out=vcx[:, :, g, 0:64], in_=bank(3, 0, 256).rearrange("p (c d) -> p c d", d=64)),
                     r=bk(3) + ["vcx_one"], w=["vcx_v%d" % g])
    VCX = ["vcx_ovl", "vcx_one", "vcx_v0", "vcx_v1"]

    if dbg:
        d_kct = dbg_out("kcT", [128, 512])
        d_vcx = dbg_out("vcx", [128, 4 * 2 * 194])
        stg = view(TB + 32 * KB, [128, 4 * 2 * 194], F32, ["stg"])
        P.op("dve", lambda e: e.tensor_copy(out=stg[:, 0:512], in_=kcT[:]), r=["kcT"], w=["stg"])
        P.dma("sp", d_kct, stg[:, 0:512], r=["stg"], key=("dbgo", 4))
        P.op("dve", lambda e: e.tensor_copy(out=stg, in_=vcx[:].rearrange("p a g d -> p (a g d)")), r=VCX, w=["stg"])
        P.dma("sp", d_vcx, stg, r=["stg"], key=("dbgo", 5))
    if stop_after == "Ap":
        P.finalize()
        nc.in_names = in_names
        return nc

    P.alias("Wown", ["KVc"])
    P.dma("pool", Wown, wown_d.rearrange("(c p) n -> p c n", p=128), w=["Wown"])
    for dc in range(8):
        P.op("dve", lambda e, dc=dc: e.tensor_scalar(out=Wown[:, dc, :], in0=Wown[:, dc, :], scalar1=gmix[:, dc:dc + 1],
                                                      scalar2=None, op0=ALU.mult), r=["Wown", "gmix"], w=["Wown"])
    Ewide = view(TB + 0, [128, 64, 128], BF16, ["Ewide"])
    P.alias("Ewide", [("xTb", 0), ("xTb", 1), "hid0", "hid1"])
    P.dma("pool", Ewide, ewide_d.rearrange("p (k q) -> p k q", q=128), w=["Ewide"])
    o = TB + 16 * KB
    B_xTo = [view(o, [128, 8, 128], BF16, [("xTo", 0)]), view(o + 2 * KB, [128, 8, 128], BF16, [("xTo", 1)])]; o += 4 * KB
    B_sq = view(o, [128, 8, 128], BF16, ["Bsq"]); o += 2 * KB
    B_cso = [view(o, [128, 64], F32, [("cso", 0)]), view(o + 256, [128, 64], F32, [("cso", 1)])]; o += 512
    B_mC = [view(o, [128, 2, 128], BF16, [("mC", 0)]), view(o + 512, [128, 2, 128], BF16, [("mC", 1)])]; o += 1 * KB
    B_m12 = [view(o, [128, 256], F32, [("m12", 0)]), view(o + KB, [128, 256], F32, [("m12", 1)])]; o += 2 * KB
    B_Ocs = view(o, [128, 4 * 194], F32, ["Ocs"])
    B_uv = view(o, [128, 1024], F32, [("uv", 0), ("uv", 1)]); o += 4 * KB
    B_gates = view(o, [128, 24], F32, ["gates"]); o += 128
    B_csr = view(o, [128, 64], F32, ["Bcsr"]); o += 256
    B_OTs = view(o, [128, 512], F32, ["OTs"])
    B_tmp = view(o, [128, 4, 256], F32, ["Btmp%d" % k_ for k_ in range(4)]); o += 4 * KB
    B_qtm = view(o, [128, 512], BF16, ["Bqtma", "Bqtmb"]); o += KB
    B_QZ = [view(o, [128, 512], BF16, [("QZ", 0)]), view(o + KB, [128, 512], BF16, [("QZ", 1)])]; o += 2 * KB
    B_vn = view(o, [128, 512], BF16, ["vn"]); o += KB
    B_oa = view(o, [128, 512], F32, ["oa"]); o += 2 * KB
    B_junk = view(o, [128, 1024], BF16, ["junk"]); o += 2 * KB
    B_cat = view(o, [128, 1024], BF16, ["cat_a", "cat_b"]); o += 2 * KB
    B_Pt = [view(o + k * KB, [128, 512], BF16, [("Pt", k)]) for k in range(3)]; o += 3 * KB
    B_imp = view(o, [128, 128], F32, ["imp"]); o += 512
    B_score = view(o, [128, 128], F32, ["score"]); o += 512
    B_wk = view(o, [128, 128], F32, ["wk"]); o += 512
    B_m8 = view(o, [128, 16], F32, ["m8a", "m8b"]); o += 64
    B_sel = view(o, [128, 128], BF16, ["sel"]); o += 256
    B_selT = view(o, [128, 4, 128], BF16, ["selT"]); o += KB
    B_rd = view(o, [128, 16], F32, ["rdc", "rds", "rdw"]); o += 64
    B_fac = view(o, [128, 16], F32, ["fac"]); o += 64
    B_ob = view(o, [128, 512], F32, [("ob", 0), ("ob", 1)]); o += 2 * KB
    B_t2 = view(o, [128, 256], F32, ["t2"]); o += KB
    B_rs = view(o, [128, 8], F32, ["Brs", "Brsv", "Brsa", "Brsb"]); o += 32
    dmwm = view(o, [128, 12, 128], BF16, ["dmwm"]); o += 3 * KB
    P.dma("pool", dmwm, dmwm_d.rearrange("p (r q) -> p r q", q=128), w=["dmwm"])
    P.op("pool", lambda e: e.memset(B_QZ[0], 0.0), w=[("QZ", 0)])
    P.op("pool", lambda e: e.memset(B_QZ[1], 0.0), w=[("QZ", 1)])
    assert o <= ARENA, o

    d_oa = dbg_out("oa", [128, 512])
    d_ob = dbg_out("ob", [128, 512])
    d_score = dbg_out("score", [128, 256])
    d_imp = dbg_out("imp", [128, 256])
    d_oc = dbg_out("Oc", [128, 2 * 776])
    d_os = dbg_out("Os", [128, 1024])
    d_ow = dbg_out("Ow", [128, 1024])
    d_gates = dbg_out("gates", [128, 24])
    DBG_TILE = 1

    pcount = [0]
    scount = [0]

    LA = 3
    SBANKS = [0, 1, 2, 7]
    pipe = []

    def pipe_step(s1, later):
        s1()
        pipe.append(later)
        if len(pipe) > LA:
            pipe.pop(0)()

    def pipe_flush():
        while pipe:
            pipe.pop(0)()

    def nsa_group(i, g):
        Qg = B_QZ[g]
        mb = i % 2
        OcK = bk(3) + bk(4)
        OsK = bk(5)
        OwK = bk(6)
        Oc = PS[:, 3 * 512:5 * 512].rearrange("p (j x) -> p j x", x=256)
        Os = bank(5).rearrange("p (j x) -> p j x", x=128)
        Ow = bank(6).rearrange("p (j x) -> p j x", x=128)
        Ocs = B_Ocs.rearrange("p (j x) -> p j x", x=194)

        def score_step(lhsT, lkeys, masks):
            sbk = SBANKS[scount[0] % len(SBANKS)]
            scount[0] += 1

            def s1():
                P.op("pe", lambda e: e.matmul(bank(sbk), lhsT=lhsT, rhs=Qg, start=True, stop=(len(masks) == 0), skip_group_check=True),
                     r=lkeys + [("QZ", g)], w=bk(sbk))
                for mi_, (ml, mr, mk) in enumerate(masks):
                    if mr.shape[-1] == 512:
                        last = (mi_ == len(masks) - 1)
                        P.op("pe", lambda e, ml=ml, mr=mr, last=last: e.matmul(bank(sbk), lhsT=ml, rhs=mr, start=False, stop=last, skip_group_check=True),
                             r=mk, w=bk(sbk))
                        continue
                    for j in range(4):
                        last = (mi_ == len(masks) - 1) and j == 3
                        P.op("pe", lambda e, ml=ml, mr=mr, j=j, last=last: e.matmul(bank(sbk, j * 128, (j + 1) * 128), lhsT=ml, rhs=mr,
                                                                                  start=False, stop=last, skip_group_check=True),
                             r=mk, w=bk(sbk))
            return sbk, s1

        def exp_pv(sbk, rhs_of_j, rkeys, outs, okeys, first, lastf, post=None):
            def later():
                pb = pcount[0] % 3
                pcount[0] += 1
                P.op("act", lambda e: e.activation(out=B_Pt[pb], in_=bank(sbk), func=AF.Exp), r=bk(sbk), w=[("Pt", pb)])
                for j in range(4):
                    P.op("pe", lambda e, j=j: e.matmul(outs[j], lhsT=B_Pt[pb][:, j * 128:(j + 1) * 128], rhs=rhs_of_j,
                                                       start=(first and j in okeys[1]), stop=lastf, skip_group_check=True),
                         r=[("Pt", pb)] + rkeys, w=okeys[0])
                if post is not None:
                    post()
            return later

        def exp_pvT(sbk, vext, rkeys, obank, okey, first, lastf, post=None):
            def later():
                pb = pcount[0] % 3
                pcount[0] += 1
                P.op("act", lambda e: e.activation(out=B_Pt[pb], in_=bank(sbk), func=AF.Exp), r=bk(sbk), w=[("Pt", pb)])
                P.op("pe", lambda e: e.matmul(PS[0:65, obank * 512:(obank + 1) * 512], lhsT=vext, rhs=B_Pt[pb], start=first, stop=lastf, skip_group_check=True),
                     r=[("Pt", pb)] + rkeys, w=okey)
                if post is not None:
                    post()
            return later

        def untranspose(obank, okey):
            P.op("act", lambda e: e.activation(out=B_OTs[0:65, :], in_=PS[0:65, obank * 512:(obank + 1) * 512], func=AF.Copy), r=okey, w=["OTs"])
            for j in range(4):
                P.op("pe", lambda e, j=j: e.transpose(bank(obank, j * 128, j * 128 + 65), B_OTs[0:65, j * 128:(j + 1) * 128], identf[0:65, 0:65]),
                     r=["OTs", "identf"], w=okey)

        n_ct = (32 * i + 30) // 128 + 1
        oc_outs = [PS[:, 3 * 512 + j * 256: 3 * 512 + j * 256 + 194] for j in range(4)]

        def post_cmp():
            P.op("act", lambda e: e.activation(out=Ocs, in_=Oc[:, :, 0:194], func=AF.Copy), r=OcK, w=["Ocs"])
            P.op("dve", lambda e: e.tensor_scalar(out=B_rd[:, 0:4], in0=Ocs[:, :, 64], scalar1=1e-30, scalar2=None, op0=ALU.max), r=["Ocs"], w=["rdc"])
            P.op("dve", lambda e: e.reciprocal(out=B_rd[:, 0:4], in_=B_rd[:, 0:4]), r=["rdc"], w=["rdc"])
            P.op("dve", lambda e: e.tensor_scalar(out=B_imp, in0=Ocs[:, 0, 66:194], scalar1=B_rd[:, 0:1], scalar2=None, op0=ALU.mult), r=["Ocs", "rdc"], w=["imp"])
            for j in range(1, 4):
                P.op("dve", lambda e, j=j: e.scalar_tensor_tensor(out=B_imp, in0=Ocs[:, j, 66:194], scalar=B_rd[:, j:j + 1], in1=B_imp, op0=ALU.mult, op1=ALU.add),
                     r=["Ocs", "rdc", "imp"], w=["imp"])
            P.op("dve", lambda e: e.tensor_tensor(out=B_score, in0=B_imp, in1=B_m12[mb][:, 0:128], op=ALU.mult), r=["imp", ("m12", mb)], w=["score"])
            P.op("dve", lambda e: e.tensor_tensor(out=B_score, in0=B_score, in1=B_m12[mb][:, 128:256], op=ALU.add), r=["score", ("m12", mb)], w=["score"])
            if dbg and i == DBG_TILE:
                P.dma("sp", d_score[:, g * 128:(g + 1) * 128], B_score, r=["score"], key=("dbgo", 200 + g))
                P.dma("sp", d_imp[:, g * 128:(g + 1) * 128], B_imp, r=["imp"], key=("dbgo", 202 + g))
                P.dma("sp", d_oc[:, g * 776:(g + 1) * 776], B_Ocs, r=["Ocs"], key=("dbgo", 204 + g))
            P.op("dve", lambda e: e.max(out=B_m8[:, 0:8], in_=B_score), r=["score"], w=["m8a"])
            P.op("dve", lambda e: e.match_replace(out=B_wk, in_to_replace=B_m8[:, 0:8], in_values=B_score, imm_value=-1e30), r=["score", "m8a"], w=["wk"])
            P.op("dve", lambda e: e.max(out=B_m8[:, 8:16], in_=B_wk), r=["wk"], w=["m8b"])
            P.op("dve", lambda e: e.tensor_scalar(out=B_sel, in0=B_score, scalar1=B_m8[:, 15:16], scalar2=None, op0=ALU.is_ge), r=["score", "m8b"], w=["sel"])
            P.op("dve", lambda e: e.tensor_scalar(out=B_sel, in0=B_sel, scalar1=-1.0, scalar2=30000.0, op0=ALU.add, op1=ALU.mult), r=["sel"], w=["sel"])
            tpv = bankb(3)[:, 0:128]
            P.op("pe", lambda e: e.transpose(tpv, B_sel, identb[:]), r=["sel", "identb"], w=bk(3))
            P.op("dve", lambda e: e.tensor_copy(out=B_selT, in_=bc_mid(tpv, 4)), r=bk(3), w=["selT"])

        for ct in range(n_ct):
            masks = []
            if ct >= n_ct - 2:
                mi = ct - (n_ct - 2)
                masks.append((identb[:], B_mC[mb][:, mi, :], ["identb", ("mC", mb)]))
            sbk, s1 = score_step(kcT[:, ct * 128:(ct + 1) * 128], ["kcT"], masks)
            pipe_step(s1, exp_pv(sbk, vcx[:, ct, g, :], VCX, oc_outs, (OcK, (0, 2)), ct == 0, ct == n_ct - 1,
                                 post=(post_cmp if ct == n_ct - 1 else None)))

        ow_outs = [bank(6, j * 128, j * 128 + 65) for j in range(4)]
        rlist = [r_ for r_ in range(8) if 4 * i - 4 + r_ >= 0]
        for r_ in rlist:
            kt = 4 * i - 4 + r_
            masks = [(identb[:], dmwm[:, 4 + r_, :], ["identb", "dmwm"])]
            sbk, s1 = score_step(KVs[:, 1, kt * 128:(kt + 1) * 128], ["KVs"], masks)
            pipe_step(s1, exp_pv(sbk, Vtm[1][:, kt, g, :], ["Vtm", "Vtm_one1"], ow_outs, (OwK, (0,)), r_ == rlist[0], r_ == rlist[-1]))

        os_outs = [bank(5, j * 128, j * 128 + 65) for j in range(4)]
        n_kt = 4 * i + 4

        def post_group():
            if dbg and i == DBG_TILE:
                stg = view(TB + 58 * KB, [128, 1024], F32, ["stg2"])
                P.op("dve", lambda e: e.memset(stg, 0.0), w=["stg2"])
                P.op("dve", lambda e: e.tensor_copy(out=stg[:, 0:512].rearrange("p (j x) -> p j x", x=128)[:, :, 0:65], in_=Os[:, :, 0:65]), r=OsK + ["stg2"], w=["stg2"])
                P.dma("sp", d_os[:, g * 512:(g + 1) * 512], stg[:, 0:512], r=["stg2"], key=("dbgo", 102 + g))
                P.op("dve", lambda e: e.tensor_copy(out=stg[:, 0:512].rearrange("p (j x) -> p j x", x=128)[:, :, 0:65], in_=Ow[:, :, 0:65]), r=OwK, w=["stg2"])
                P.dma("sp", d_ow[:, g * 512:(g + 1) * 512], stg[:, 0:512], r=["stg2"], key=("dbgo", 104 + g))
            P.op("dve", lambda e: e.reciprocal(out=B_rd[:, 4:8], in_=Os[:, :, 64]), r=OsK, w=["rds"])
            P.op("dve", lambda e: e.reciprocal(out=B_rd[:, 8:12], in_=Ow[:, :, 64]), r=OwK, w=["rdw"])
            gsl = B_gates[:, g * 12:(g + 1) * 12].rearrange("p (h b) -> p b h", b=3)
            P.op("dve", lambda e: e.tensor_tensor(out=B_fac[:, 0:12].rearrange("p (b h) -> p b h", h=4), in0=B_rd[:, 0:12].rearrange("p (b h) -> p b h", h=4),
                                                  in1=gsl, op=ALU.mult), r=["rdc", "rds", "rdw", "gates"], w=["fac"])
            obg = B_ob[:, g * 256:(g + 1) * 256].rearrange("p (j d) -> p j d", d=64)
            t2 = B_t2.rearrange("p (j d) -> p j d", d=64)
            P.op("pool", lambda e: e.tensor_tensor(out=obg, in0=Ocs[:, :, 0:64], in1=bc_last(B_fac[:, 0:4], 64), op=ALU.mult), r=["Ocs", "fac"], w=[("ob", g)])
            P.op("dve", lambda e: e.tensor_tensor(out=t2, in0=Os[:, :, 0:64], in1=bc_last(B_fac[:, 4:8], 64), op=ALU.mult), r=OsK + ["fac"], w=["t2"])
            P.op("pool", lambda e: e.tensor_tensor(out=obg, in0=obg, in1=t2, op=ALU.add), r=[("ob", g), "t2"], w=[("ob", g)])
            P.op("dve", lambda e: e.tensor_tensor(out=t2, in0=Ow[:, :, 0:64], in1=bc_last(B_fac[:, 8:12], 64), op=ALU.mult), r=OwK + ["fac"], w=["t2"])
            P.op("pool", lambda e: e.tensor_tensor(out=obg, in0=obg, in1=t2, op=ALU.add), r=[("ob", g), "t2"], w=[("ob", g)])

        for kt in range(n_kt):
            masks = [(Ewide[:, kt, :], B_selT.rearrange("p j q -> p (j q)"), ["Ewide", "selT"])]
            if kt >= 4 * i:
                masks.append((identb[:], dmwm[:, kt - 4 * i, :], ["identb", "dmwm"]))
            sbk, s1 = score_step(KVs[:, 0, kt * 128:(kt + 1) * 128], ["KVs"], masks)

            def post_sel():
                untranspose(5, OsK)
                post_group()
            pipe_step(s1, exp_pv(sbk, Vtm[0][:, kt, g, :], ["Vtm", "Vtm_one0"], os_outs, (OsK, (0,)), kt == 0, kt == n_kt - 1,
                                 post=(post_group if kt == n_kt - 1 else None)))

    n_own = NOWN if stop_after not in ("B1x",) else 2
    for i in range(n_own):
        ib = i % 2
        P.dma("pool", B_xTo[ib], xTo_d[i].rearrange("(c p) t -> p c t", p=128), w=[("xTo", ib)])
        P.dma("sp", B_cso[ib], cso_d[i * 128:(i + 1) * 128, :], w=[("cso", ib)])
        P.dma("pool", B_mC[ib], mC_d[i].rearrange("p (k q) -> p k q", q=128), w=[("mC", ib)])
        P.dma("sp", B_m12[ib], m12_d[i], w=[("m12", ib)])
        xs = B_xTo[ib]
        P.op("act", lambda e, xs=xs: e.activation(out=B_sq, in_=xs, func=AF.Square), r=[("xTo", ib)], w=["Bsq"])
        for dc in range(8):
            P.op("pe", lambda e, dc=dc: e.matmul(bank(6, 0, 1), lhsT=B_sq[:, dc, :], rhs=ones[:, 0:1], start=(dc == 0), stop=(dc == 7)),
                 r=["Bsq", "ones"], w=bk(6))
        for half in range(2):
            for dc in range(8):
                P.op("pe", lambda e, dc=dc, half=half, xs=xs: e.matmul(bank(half), lhsT=xs[:, dc, :], rhs=Wown[:, dc, half * 512:(half + 1) * 512],
                                                                     start=(dc == 0), stop=(dc == 7)), r=[("xTo", ib), "Wown"], w=bk(half))
        for dc in range(8):
            P.op("pe", lambda e, dc=dc, xs=xs: e.matmul(bank(2), lhsT=xs[:, dc, :], rhs=Wown[:, dc, 1024:1536], start=(dc == 0), stop=(dc == 7)),
                 r=[("xTo", ib), "Wown"], w=bk(2))
        for dc in range(8):
            P.op("pe", lambda e, dc=dc, xs=xs: e.matmul(bank(5, 0, 24), lhsT=xs[:, dc, :], rhs=Wown[:, dc, 1536:1560], start=(dc == 0), stop=(dc == 7)),
                 r=[("xTo", ib), "Wown"], w=bk(5))
        rs = B_rs[:, 0:1]
        rstd_from_ssq(bank(6, 0, 1), D, rs, bk(6), "Brs")
        for half in range(2):
            P.op("act", lambda e, half=half: e.activation(out=B_uv[:, half * 512:(half + 1) * 512], in_=bank(half), func=AF.Gelu_apprx_tanh, scale=rs),
                 r=bk(half) + ["Brs"], w=[("uv", half)])
        P.op("act", lambda e: e.activation(out=B_gates, in_=bank(5, 0, 24), func=AF.Sigmoid, scale=rs), r=bk(5) + ["Brs"], w=["gates"])
        P.op("dve", lambda e, ib=ib: e.tensor_scalar(out=B_csr, in0=B_cso[ib], scalar1=rs, scalar2=0.125, op0=ALU.mult, op1=ALU.mult),
             r=[("cso", ib), "Brs"], w=["Bcsr"])
        rope_tm(bank(2), 8, B_csr, B_qtm, B_tmp, bk(2) + ["Bcsr"], "Btmp", "Bqtm")
        tq = bankb(7)[:, 0:512].rearrange("p (j t) -> p j t", t=128)
        for j in range(4):
            P.op("pe", lambda e, j=j: e.transpose(tq[:, j, :], B_qtm[:, j * 128:(j + 1) * 128], identb[:]), r=["Bqtma", "Bqtmb", "identb"], w=bk(7, (0, 1)))
        for gq in range(2):
            P.op("act", lambda e, gq=gq: e.activation(out=B_QZ[gq][gq * 64:(gq + 1) * 64].rearrange("p (j q) -> p j q", q=128),
                                                      in_=tq[gq * 64:(gq + 1) * 64], func=AF.Copy), r=bk(7), w=[("QZ", gq)])
        P.op("act", lambda e: e.activation(out=B_junk[:, 0:512], in_=B_uv[:, 512:1024], func=AF.Square, accum_out=B_rs[:, 1:2]), r=[("uv", 1)], w=["junk", "Brsv"])
        rstd_from_ssq(B_rs[:, 1:2], 512, B_rs[:, 1:2], ["Brsv"], "Brsv")
        P.op("dve", lambda e: e.scalar_tensor_tensor(out=B_vn, in0=B_uv[:, 512:1024], scalar=B_rs[:, 1:2], in1=gv[:], op0=ALU.mult, op1=ALU.mult),
             r=[("uv", 1), "Brsv", "gv"], w=["vn"])
        for g8 in range(8):
            P.op("pe", lambda e, g8=g8: e.matmul(bank(2, g8 * 64, (g8 + 1) * 64), lhsT=wsT[:, g8, :], rhs=B_vn[:, g8 * 64:(g8 + 1) * 64], start=True, stop=True),
                 r=["wsT", "vn"], w=bk(2))
        oa3 = B_oa.rearrange("p (g d) -> p g d", d=64)
        P.op("dve", lambda e: e.tensor_tensor(out=oa3, in0=bank(2).rearrange("p (g d) -> p g d", d=64), in1=bc_last(bsT[:, :], 64), op=ALU.add),
             r=bk(2) + ["bsT"], w=["oa"])
        P.op("dve", lambda e: e.tensor_tensor(out=B_oa, in0=B_oa, in1=B_uv[:, 0:512], op=ALU.mult), r=["oa", ("uv", 0)], w=["oa"])
        P.op("act", lambda e: e.activation(out=B_junk[:, 0:512], in_=B_oa, func=AF.Square, accum_out=B_rs[:, 2:3]), r=["oa"], w=["junk", "Brsa"])
        rstd_from_ssq(B_rs[:, 2:3], 512, B_rs[:, 2:3], ["Brsa"], "Brsa")
        P.op("dve", lambda e: e.scalar_tensor_tensor(out=B_cat[:, 0:512], in0=B_oa, scalar=B_rs[:, 2:3], in1=ga[:], op0=ALU.mult, op1=ALU.mult),
             r=["oa", "Brsa", "ga"], w=["cat_a"])
        if dbg and i == DBG_TILE:
            stg = view(TB + 58 * KB, [128, 1024], F32, ["stg2"])
            P.dma("sp", d_oa, B_oa, r=["oa"], key=("dbgo", 12))
            P.dma("sp", d_gates, B_gates, r=["gates"], key=("dbgo", 13))
        for g in range(2):
            nsa_group(i, g)
        pipe_flush()
        if dbg and i == DBG_TILE:
            P.dma("sp", d_ob, B_ob, r=[("ob", 0), ("ob", 1)], key=("dbgo", 14))
        P.op("act", lambda e: e.activation(out=B_junk[:, 0:512], in_=B_ob, func=AF.Square, accum_out=B_rs[:, 3:4]), r=[("ob", 0), ("ob", 1)], w=["junk", "Brsb"])
        rstd_from_ssq(B_rs[:, 3:4], 512, B_rs[:, 3:4], ["Brsb"], "Brsb")
        P.op("dve", lambda e: e.scalar_tensor_tensor(out=B_cat[:, 512:1024], in0=B_ob, scalar=B_rs[:, 3:4], in1=gb[:], op0=ALU.mult, op1=ALU.mult),
             r=[("ob", 0), ("ob", 1), "Brsb", "gb"], w=["cat_b"])
        tc = bankb(7).rearrange("p (k t) -> p k t", t=128)
        for kc in range(8):
            P.op("pe", lambda e, kc=kc: e.transpose(tc[:, kc, :], B_cat[:, kc * 128:(kc + 1) * 128], identb[:]), r=["cat_a", "cat_b", "identb"], w=bk(7))
        if i == 0:
            P.alias(("catT", 0), [("W1", 0), ("W1", 1)])
        P.op("act", lambda e, i=i: e.activation(out=catT[:, :, i * 128:(i + 1) * 128], in_=tc, func=AF.Copy), r=bk(7), w=[("catT", i)])

    if stop_after in ("B1", "B1x"):
        if dbg:
            d_catT = dbg_out("catT", [128, 8 * NOWN * 128])
            for kc in range(8):
                stg = view(TB + 58 * KB, [128, 1024], F32, ["stg2"])
                for hh in range(2):
                    P.op("dve", lambda e, kc=kc, hh=hh: e.tensor_copy(out=stg, in_=catT[:, kc, hh * 1024:(hh + 1) * 1024]),
                         r=[("catT", i) for i in range(n_own)], w=["stg2"])
                    P.dma("sp", d_catT[:, kc * 2048 + hh * 1024: kc * 2048 + (hh + 1) * 1024], stg, r=["stg2"], key=("dbgo", 15))
        P.finalize()
        nc.in_names = in_names
        return nc

    P.dma("pool", Wo, wo_d.rearrange("(c p) n -> p c n", p=128), w=["Wo"])
    C_gmoe = view(16 * KB, [128, D], F32, ["gmoe"])
    C_hn2 = [view(20 * KB, [128, D], F32, [("hn2", 0)]), view(24 * KB, [128, D], F32, [("hn2", 1)])]
    o = TB + 32 * KB
    C_xo = [view(o, [128, D], F32, [("xo", 0)]), view(o + 4 * KB, [128, D], F32, [("xo", 1)])]; o += 8 * KB
    C_hn2Tf = [view(o, [128, 8, 128], F32, [("hn2Tf", 0)]), view(o + 4 * KB, [128, 8, 128], F32, [("hn2Tf", 1)])]; o += 8 * KB
    C_junk = view(o, [128, D], BF16, ["Cjunk"]); o += 2 * KB
    RK2 = ["q_mg", "q_sg", "q_l1", "q_l2", "q_e2", "rstd2"] + [("ssq2", i_) for i_ in range(NOWN)]
    C_lgall = view(o, [128, NOWN, 20], F32, [("lg", i_) for i_ in range(NOWN)]); o += 1280
    C_r = view(o, [128, 12, NOWN * 4], F32, ["r_ohg", "r_dg", "r_les", "r_eq1", "r_x2", "r_sel2", "r_ee"]); o += 3072
    C_q = view(o, [128, 12, NOWN], F32, RK2); o += 768
    C_t44 = view(o, [128, NOWN * 16], F32, ["t44"]); o += 1024
    assert o <= ARENA
    P.dma("sp", C_gmoe, gmoe_d, w=["gmoe"])


    def B2_front(i):
        ib = i % 2
        hb = 0 if i % 2 == 0 else 2
        P.dma("sp", C_xo[ib], xo_d[i], w=[("xo", ib)])
        for half in range(2):
            for kc in range(8):
                P.op("pe", lambda e, kc=kc, half=half: e.matmul(bank(hb + half), lhsT=catT[:, kc, i * 128:(i + 1) * 128], rhs=Wo[:, kc, half * 512:(half + 1) * 512],
                                                              start=(kc == 0), stop=(kc == 7)), r=[("catT", i), "Wo"], w=bk(hb + half))

    def B2_back(i):
        ib = i % 2
        hb = 0 if i % 2 == 0 else 2
        for half in range(2):
            P.op("dve", lambda e, half=half: e.tensor_tensor(out=h1[:, i, half * 512:(half + 1) * 512], in0=bank(hb + half),
                                                             in1=C_xo[ib][:, half * 512:(half + 1) * 512], op=ALU.add),
                 r=bk(hb + half) + [("xo", ib)], w=[("h1", i)])
        P.op("act", lambda e: e.activation(out=C_junk, in_=h1[:, i, :], func=AF.Square, accum_out=C_q[:, 0, i:i + 1]), r=[("h1", i)], w=["Cjunk", ("ssq2", i)])

    for i in range(NOWN):
        B2_front(i)
        if i > 0:
            B2_back(i - 1)
    B2_back(NOWN - 1)
    SS2 = [("ssq2", i) for i in range(NOWN)]
    P.op("act", lambda e: e.activation(out=C_q[:, 1, :], in_=C_q[:, 0, :], func=AF.Sqrt, scale=1.0 / D, bias=EPS), r=SS2, w=["rstd2"])
    P.op("dve", lambda e: e.reciprocal(out=C_q[:, 1, :], in_=C_q[:, 1, :]), r=["rstd2"], w=["rstd2"])

    def B2c_front(i):
        ib = i % 2
        P.op("dve", lambda e: e.scalar_tensor_tensor(out=C_hn2[ib], in0=h1[:, i, :], scalar=C_q[:, 1, i:i + 1], in1=C_gmoe, op0=ALU.mult, op1=ALU.mult),
             r=[("h1", i), "rstd2", "gmoe"], w=[("hn2", ib)])

    def B2c_back(i):
        ib = i % 2
        tb_ = 4 if i % 2 == 0 else 6
        tf = PS[:, tb_ * 512:(tb_ + 2) * 512].rearrange("p (c t) -> p c t", t=128)
        for dc in range(8):
            P.op("pe", lambda e, dc=dc: e.transpose(tf[:, dc, :], C_hn2[ib][:, dc * 128:(dc + 1) * 128], identf[:]), r=[("hn2", ib), "identf"], w=bk(tb_) + bk(tb_ + 1))
        P.op("act", lambda e: e.activation(out=C_hn2Tf[ib], in_=tf, func=AF.Copy), r=bk(tb_) + bk(tb_ + 1), w=[("hn2Tf", ib)])
        P.op("dve", lambda e: e.tensor_copy(out=hn2T[:, :, i * 128:(i + 1) * 128], in_=tf), r=bk(tb_) + bk(tb_ + 1), w=[("hn2T", i)])
        for dc in range(8):
            P.op("pe", lambda e, dc=dc: e.matmul(bank(tb_, 0, 20), lhsT=C_hn2Tf[ib][:, dc, :], rhs=rcat[:, dc, :], start=(dc == 0), stop=(dc == 7)),
                 r=[("hn2Tf", ib), "rcat"], w=bk(tb_))
        P.op("act", lambda e: e.activation(out=C_lgall[:, i, :], in_=bank(tb_, 0, 20), func=AF.Copy), r=bk(tb_), w=[("lg", i)])

    for i in range(NOWN):
        B2c_front(i)
        if i > 0:
            B2c_back(i - 1)
    B2c_back(NOWN - 1)

    LGA = [("lg", i) for i in range(NOWN)]
    G3 = C_lgall[:, :, 0:4]
    E4 = C_lgall[:, :, 4:20].rearrange("p t (g e) -> p t g e", e=4)
    R3 = lambda k: C_r[:, k, :].rearrange("p (t x) -> p t x", x=4)
    Q = lambda k: C_q[:, k, :]
    bq = lambda k: C_q[:, k, :].unsqueeze(2).to_broadcast([128, NOWN, 4])
    P.op("dve", lambda e: e.tensor_reduce(out=Q(2), in_=G3, axis=AX.X, op=ALU.max), r=LGA, w=["q_mg"])
    P.op("dve", lambda e: e.tensor_tensor(out=R3(0), in0=G3, in1=bq(2), op=ALU.is_equal), r=LGA + ["q_mg"], w=["r_ohg"])
    P.op("dve", lambda e: e.tensor_tensor(out=R3(1), in0=G3, in1=bq(2), op=ALU.subtract), r=LGA + ["q_mg"], w=["r_dg"])
    P.op("act", lambda e: e.activation(out=R3(1), in_=R3(1), func=AF.Exp), r=["r_dg"], w=["r_dg"])
    P.op("dve", lambda e: e.tensor_reduce(out=Q(3), in_=R3(1), axis=AX.X, op=ALU.add), r=["r_dg"], w=["q_sg"])
    P.op("dve", lambda e: e.reciprocal(out=Q(3), in_=Q(3)), r=["q_sg"], w=["q_sg"])
    t44 = C_t44.rearrange("p (t g e) -> p t g e", g=4, e=4)
    P.op("dve", lambda e: e.tensor_tensor(out=t44, in0=E4, in1=R3(0).unsqueeze(3).to_broadcast([128, NOWN, 4, 4]), op=ALU.mult), r=LGA + ["r_ohg"], w=["t44"])
    P.op("dve", lambda e: e.tensor_reduce(out=R3(2), in_=C_t44.rearrange("p (t g e) -> p t e g", g=4, e=4), axis=AX.X, op=ALU.add), r=["t44"], w=["r_les"])
    P.op("dve", lambda e: e.tensor_reduce(out=Q(4), in_=R3(2), axis=AX.X, op=ALU.max), r=["r_les"], w=["q_l1"])
    P.op("dve", lambda e: e.tensor_tensor(out=R3(3), in0=R3(2), in1=bq(4), op=ALU.is_equal), r=["r_les", "q_l1"], w=["r_eq1"])
    P.op("dve", lambda e: e.scalar_tensor_tensor(out=C_r[:, 4, :], in0=C_r[:, 3, :], scalar=-1e30, in1=C_r[:, 2, :], op0=ALU.mult, op1=ALU.add), r=["r_eq1", "r_les"], w=["r_x2"])
    P.op("dve", lambda e: e.tensor_reduce(out=Q(5), in_=R3(4), axis=AX.X, op=ALU.max), r=["r_x2"], w=["q_l2"])
    P.op("dve", lambda e: e.tensor_tensor(out=R3(5), in0=R3(2), in1=bq(5), op=ALU.is_ge), r=["r_les", "q_l2"], w=["r_sel2"])
    P.op("dve", lambda e: e.tensor_tensor(out=R3(6), in0=R3(2), in1=bq(4), op=ALU.subtract), r=["r_les", "q_l1"], w=["r_ee"])
    P.op("act", lambda e: e.activation(out=R3(6), in_=R3(6), func=AF.Exp), r=["r_ee"], w=["r_ee"])
    P.op("dve", lambda e: e.tensor_tensor(out=Q(6), in0=Q(5), in1=Q(4), op=ALU.subtract), r=["q_l2", "q_l1"], w=["q_e2"])
    P.op("act", lambda e: e.activation(out=Q(6), in_=Q(6), func=AF.Exp), r=["q_e2"], w=["q_e2"])
    P.op("dve", lambda e: e.tensor_scalar(out=Q(6), in0=Q(6), scalar1=1.0, scalar2=None, op0=ALU.add), r=["q_e2"], w=["q_e2"])
    P.op("dve", lambda e: e.reciprocal(out=Q(6), in_=Q(6)), r=["q_e2"], w=["q_e2"])
    P.op("dve", lambda e: e.tensor_tensor(out=Q(6), in0=Q(6), in1=Q(3), op=ALU.mult), r=["q_e2", "q_sg"], w=["q_e2"])
    P.op("dve", lambda e: e.tensor_tensor(out=R3(6), in0=R3(6), in1=R3(5), op=ALU.mult), r=["r_ee", "r_sel2"], w=["r_ee"])
    P.op("dve", lambda e: e.tensor_tensor(out=R3(6), in0=R3(6), in1=bq(6), op=ALU.mult), r=["r_ee", "q_e2"], w=["r_ee"])
    P.op("dve", lambda e: e.tensor_tensor(out=comb[:].rearrange("p t (g e) -> p t g e", e=4), in0=R3(0).unsqueeze(3).to_broadcast([128, NOWN, 4, 4]),
                                          in1=R3(6).unsqueeze(2).to_broadcast([128, NOWN, 4, 4]), op=ALU.mult),
         r=["r_ohg", "r_ee"], w=[("comb", i) for i in range(NOWN)])

    d_h1 = dbg_out("h1", [128, NOWN * D])
    d_comb = dbg_out("comb", [128, NOWN * 16])
    if dbg:
        for i in range(NOWN):
            P.dma("sp", d_h1[:, i * D:(i + 1) * D], h1[:, i, :], r=[("h1", i)], key=("dbgo", 16, i))
        P.dma("sp", d_comb, comb[:].rearrange("p a b -> p (a b)"), r=[("comb", i) for i in range(NOWN)], key=("dbgo", 17))
    if stop_after == "B2":
        P.finalize()
        nc.in_names = in_names
        return nc

    o = TB + 32 * KB
    M_sg = [view(o, [128, 512], BF16, [("Msg", 0)]), view(o + KB, [128, 512], BF16, [("Msg", 1)])]; o += 2 * KB
    M_hid = [view(o, [128, 4, 512], BF16, [("Mhid", 0)]), view(o + 4 * KB, [128, 4, 512], BF16, [("Mhid", 1)])]; o += 8 * KB
    first_c = True
    HN2T_ALL = [("hn2T", i) for i in range(NOWN)]
    gcount = 0
    ycount = 0
    for ex in range(16):
        eb = ex % 2
        Wg_, Wu_, Wd_ = Wexp[eb]
        if ex == 0:
            P.alias(("Wg", 0), ["Wo"])
            P.alias(("Wu", 0), ["Wo"])
            P.alias(("Wd", 0), ["Wo", "Wown"])
        if ex == 1:
            cts = [("catT", i) for i in range(NOWN)]
            P.alias(("Wg", 1), cts)
            P.alias(("Wu", 1), cts)
            P.alias(("Wd", 1), cts)
        P.dma("pool", Wg_, wg_d[ex].rearrange("(c p) n -> p c n", p=128), w=[("Wg", eb)])
        P.dma("pool", Wu_, wu_d[ex].rearrange("(c p) n -> p c n", p=128), w=[("Wu", eb)])
        P.dma("pool", Wd_, wd_d[ex].rearrange("(c p) n -> p c n", p=128), w=[("Wd", eb)])
        for grp in range(4):
            hb_ = (ex * 4 + grp) % 2
            hid = M_hid[hb_]
            if first_c:
                pass
                pass
                P.alias(("Msg", 0), [("xo", 0), ("xo", 1)])
                P.alias(("Msg", 1), [("xo", 0), ("xo", 1)])
                first_c = False
            rk = [("hn2T", 4 * grp + t_) for t_ in range(4)]
            for fc in range(4):
                gb_ = 0 if gcount % 2 == 0 else 2
                sgb = gcount % 2
                gcount += 1
                for dc in range(8):
                    P.op("pe", lambda e, dc=dc, fc=fc, grp=grp, gb_=gb_, Wg_=Wg_: e.matmul(bank(gb_), lhsT=Wg_[:, dc, fc * 128:(fc + 1) * 128],
                                                                                         rhs=hn2T[:, dc, grp * 512:(grp + 1) * 512], start=(dc == 0), stop=(dc == 7)),
                         r=[("Wg", eb)] + rk, w=bk(gb_))
                for dc in range(8):
                    P.op("pe", lambda e, dc=dc, fc=fc, grp=grp, gb_=gb_, Wu_=Wu_: e.matmul(bank(gb_ + 1), lhsT=Wu_[:, dc, fc * 128:(fc + 1) * 128],
                                                                                         rhs=hn2T[:, dc, grp * 512:(grp + 1) * 512], start=(dc == 0), stop=(dc == 7)),
                         r=[("Wu", eb)] + rk, w=bk(gb_ + 1))
                P.op("act", lambda e, gb_=gb_, sgb=sgb: e.activation(out=M_sg[sgb], in_=bank(gb_), func=AF.Silu), r=bk(gb_), w=[("Msg", sgb)])
                P.op("dve", lambda e, gb_=gb_, sgb=sgb, fc=fc, hid=hid: e.tensor_tensor(out=hid[:, fc, :], in0=bank(gb_ + 1), in1=M_sg[sgb], op=ALU.mult),
                     r=bk(gb_ + 1) + [("Msg", sgb)], w=[("Mhid", hb_)])
            for tl in range(4):
                tile = 4 * grp + tl
                for half in range(2):
                    yb = 4 + ycount % 4
                    ycount += 1
                    for fc in range(4):
                        P.op("pe", lambda e, fc=fc, tl=tl, half=half, yb=yb, hid=hid, Wd_=Wd_: e.matmul(bank(yb), lhsT=hid[:, fc, tl * 128:(tl + 1) * 128],
                                                                                                      rhs=Wd_[:, fc, half * 512:(half + 1) * 512], start=(fc == 0), stop=(fc == 3)),
                             r=[("Mhid", hb_), ("Wd", eb)], w=bk(yb))
                    P.op("dve", lambda e, tile=tile, half=half, yb=yb, ex=ex: e.scalar_tensor_tensor(
                        out=h1[:, tile, half * 512:(half + 1) * 512], in0=bank(yb), scalar=comb[:, tile, ex:ex + 1],
                        in1=h1[:, tile, half * 512:(half + 1) * 512], op0=ALU.mult, op1=ALU.add),
                        r=bk(yb) + [("comb", tile), ("h1", tile)], w=[("h1", tile)])

    d_h2 = dbg_out("h2", [128, NOWN * D])
    if dbg:
        for i in range(NOWN):
            P.dma("sp", d_h2[:, i * D:(i + 1) * D], h1[:, i, :], r=[("h1", i)], key=("dbgo", 18, i))

    P.dma("pool", Wpg, wpg_d.rearrange("(c p) n -> p c n", p=128), w=["Wpg"])
    P.dma("pool", Wpp, wpp_d.rearrange("(c p) n -> p c n", p=128), w=["Wpp"])
    o = TB + 20 * KB
    D_pT = [view(o, [128, 2, 128], BF16, [("pT", 0)]), view(o + 512, [128, 2, 128], BF16, [("pT", 1)])]; o += KB
    D_gple = view(o, [128, D], F32, ["gple"]); o += 4 * KB
    D_gfin = view(o, [128, D], F32, ["gfin"]); o += 4 * KB
    D_hn3 = [view(o, [128, D], BF16, [("hn3", 0)]), view(o + 2 * KB, [128, D], BF16, [("hn3", 1)])]; o += 4 * KB
    D_hn3T = [view(o, [128, 8, 128], BF16, [("hn3T", 0)]), view(o + 2 * KB, [128, 8, 128], BF16, [("hn3T", 1)])]; o += 4 * KB
    D_sgt = [view(o, [128, D], F32, [("sgt", 0, 0), ("sgt", 0, 1)]), view(o + 4 * KB, [128, D], F32, [("sgt", 1, 0), ("sgt", 1, 1)])]; o += 8 * KB
    D_junk = view(o, [128, D], BF16, ["Djunk"]); o += 2 * KB
    D_out = [view(o, [128, D], F32, [("Dout", 0)]), view(o + 4 * KB, [128, D], F32, [("Dout", 1)])]; o += 8 * KB
    D_s = view(o, [128, 8], F32, [("Drs", 0), ("Drs", 1), ("Drs2", 0), ("Drs2", 1)]); o += 32
    assert o <= ARENA
    P.dma("sp", D_gple, gple_d, w=["gple"])
    P.dma("sp", D_gfin, gfin_d, w=["gfin"])

    D_q = view(o, [128, 4, NOWN], F32, ["rstd3", "rstd4"] + [("ssq3", i_) for i_ in range(NOWN)] + [("ssq4", i_) for i_ in range(NOWN)]); o += 256
    assert o <= ARENA
    for i in range(NOWN):
        P.op("act", lambda e, i=i: e.activation(out=D_junk, in_=h1[:, i, :], func=AF.Square, accum_out=D_q[:, 0, i:i + 1]), r=[("h1", i)], w=["Djunk", ("ssq3", i)])
    P.op("act", lambda e: e.activation(out=D_q[:, 1, :], in_=D_q[:, 0, :], func=AF.Sqrt, scale=1.0 / D, bias=EPS), r=[("ssq3", i) for i in range(NOWN)], w=["rstd3"])
    P.op("dve", lambda e: e.reciprocal(out=D_q[:, 1, :], in_=D_q[:, 1, :]), r=["rstd3"], w=["rstd3"])

    def D_front(i):
        ib = i % 2
        gbk = 0 if ib == 0 else 4
        trb = 6 + ib
        P.dma("pool", D_pT[ib], pTo_d[:, i * 128:(i + 1) * 128].rearrange("(c p) t -> p c t", p=128), w=[("pT", ib)])
        P.op("dve", lambda e: e.scalar_tensor_tensor(out=D_hn3[ib], in0=h1[:, i, :], scalar=D_q[:, 1, i:i + 1], in1=D_gple, op0=ALU.mult, op1=ALU.mult),
             r=[("h1", i), "rstd3", "gple"], w=[("hn3", ib)])
        tc = bankb(trb).rearrange("p (k t) -> p k t", t=128)
        for kc in range(8):
            P.op("pe", lambda e, kc=kc: e.transpose(tc[:, kc, :], D_hn3[ib][:, kc * 128:(kc + 1) * 128], identb[:]), r=[("hn3", ib), "identb"], w=bk(trb))
        P.op("act", lambda e: e.activation(out=D_hn3T[ib], in_=tc, func=AF.Copy), r=bk(trb), w=[("hn3T", ib)])
        for half in range(2):
            for dc in range(8):
                P.op("pe", lambda e, dc=dc, half=half: e.matmul(bank(gbk + half), lhsT=D_hn3T[ib][:, dc, :], rhs=Wpg[:, dc, half * 512:(half + 1) * 512],
                                                              start=(dc == 0), stop=(dc == 7)), r=[("hn3T", ib), "Wpg"], w=bk(gbk + half))

    def D_back(i):
        ib = i % 2
        gbk = 0 if ib == 0 else 4
        sg = D_sgt[ib]
        for half in range(2):
            for kc in range(2):
                P.op("pe", lambda e, kc=kc, half=half: e.matmul(bank(2 + half), lhsT=D_pT[ib][:, kc, :], rhs=Wpp[:, kc, half * 512:(half + 1) * 512],
                                                              start=(kc == 0), stop=(kc == 1)), r=[("pT", ib), "Wpp"], w=bk(2 + half))
        for half in range(2):
            P.op("act", lambda e, half=half: e.activation(out=sg[:, half * 512:(half + 1) * 512], in_=bank(gbk + half), func=AF.Sigmoid), r=bk(gbk + half), w=[("sgt", ib, half)])
            P.op("dve", lambda e, half=half: e.tensor_tensor(out=sg[:, half * 512:(half + 1) * 512], in0=bank(2 + half), in1=sg[:, half * 512:(half + 1) * 512], op=ALU.mult),
                 r=bk(2 + half) + [("sgt", ib, half)], w=[("sgt", ib, half)])
        P.op("pool", lambda e: e.tensor_tensor(out=h1[:, i, :], in0=h1[:, i, :], in1=sg, op=ALU.add), r=[("h1", i), ("sgt", ib, 0), ("sgt", ib, 1)], w=[("h1", i)])
        P.op("act", lambda e: e.activation(out=D_junk, in_=h1[:, i, :], func=AF.Square, accum_out=D_q[:, 2, i:i + 1]), r=[("h1", i)], w=["Djunk", ("ssq4", i)])

    for i in range(NOWN):
        D_front(i)
        if i > 0:
            D_back(i - 1)
    D_back(NOWN - 1)
    P.op("act", lambda e: e.activation(out=D_q[:, 3, :], in_=D_q[:, 2, :], func=AF.Sqrt, scale=1.0 / D, bias=EPS), r=[("ssq4", i) for i in range(NOWN)], w=["rstd4"])
    P.op("dve", lambda e: e.reciprocal(out=D_q[:, 3, :], in_=D_q[:, 3, :]), r=["rstd4"], w=["rstd4"])
    for i in range(NOWN):
        ib = i % 2
        P.op("dve", lambda e, i=i, ib=ib: e.scalar_tensor_tensor(out=D_out[ib], in0=h1[:, i, :], scalar=D_q[:, 3, i:i + 1], in1=D_gfin, op0=ALU.mult, op1=ALU.mult),
             r=[("h1", i), "rstd4", "gfin"], w=[("Dout", ib)])
        P.dma("sp", out_d[i], D_out[ib], r=[("Dout", ib)], key=("outq", ib))

    P.finalize()
    nc.in_names = in_names
    return nc


def host_inputs(inp):
    f = np.float32
    x = np.asarray(inp["x"], f)
    p = np.asarray(inp["p"], f)[0]
    w_in = np.asarray(inp["w_in"], f)[0]
    HD = 64
    OFF_Q, OFF_KV, OFF_GATE = 1024, 1536, 2304
    kvc = [w_in[:, OFF_KV + j * 128: OFF_KV + (j + 1) * 128] for j in range(6)]
    wkv = np.concatenate([kvc[0], kvc[2], kvc[4], kvc[1], kvc[3], kvc[5]], axis=1)
    wq = w_in[:, OFF_Q:OFF_KV].reshape(D, 8, HD)
    wq_r = np.concatenate([np.concatenate([wq[:, j], wq[:, 4 + j]], axis=1) for j in range(4)], axis=1)
    wown = np.concatenate([w_in[:, 0:1024], wq_r, w_in[:, OFF_GATE:OFF_GATE + 24]], axis=1)
    shared = {
        "wkv": np.ascontiguousarray(wkv), "wown": np.ascontiguousarray(wown),
        "gmix": np.ascontiguousarray(np.asarray(inp["norm_mix"], f)[0].reshape(8, 128).T),
        "wsT": np.ascontiguousarray(np.asarray(inp["gmlp_w_s"], f)[0].transpose(2, 0, 1).reshape(128, 8 * 128)),
        "tri": np.triu(np.ones((128, 128), f)),
        "bsT": np.ascontiguousarray(np.asarray(inp["gmlp_b_s"], f)[0].T),
        "gv": np.ascontiguousarray(np.broadcast_to(np.asarray(inp["gmlp_v_norm"], f)[0][None, :], (128, 512))),
        "ga": np.ascontiguousarray(np.broadcast_to(np.asarray(inp["out_norm_a"], f)[0][None, :], (128, 512))),
        "gb": np.ascontiguousarray(np.broadcast_to(np.asarray(inp["out_norm_b"], f)[0][None, :], (128, 512))),
        "gmoe": np.ascontiguousarray(np.broadcast_to(np.asarray(inp["norm_moe"], f)[0][None, :], (128, D))),
        "gple": np.ascontiguousarray(np.broadcast_to(np.asarray(inp["norm_ple"], f)[0][None, :], (128, D))),
        "gfin": np.ascontiguousarray(np.broadcast_to(np.asarray(inp["norm_final"], f)[None, :], (128, D))),
        "w1k": np.ascontiguousarray(np.asarray(inp["cmp_w1_k"], f)[0].reshape(32, 64, 256).transpose(1, 0, 2).reshape(64, 32 * 256)),
        "w1v": np.ascontiguousarray(np.asarray(inp["cmp_w1_v"], f)[0].reshape(32, 64, 256).transpose(1, 0, 2).reshape(64, 32 * 256)),
        "peTk": np.ascontiguousarray(np.asarray(inp["cmp_pe_k"], f)[0].T),
        "peTv": np.ascontiguousarray(np.asarray(inp["cmp_pe_v"], f)[0].T),
        "w2k": np.ascontiguousarray(np.asarray(inp["cmp_w2_k"], f)[0]),
        "w2v": np.ascontiguousarray(np.asarray(inp["cmp_w2_v"], f)[0]),
        "wo": np.ascontiguousarray(np.asarray(inp["w_o"], f)[0]),
        "rcat": np.ascontiguousarray(np.concatenate([np.asarray(inp["router_group"], f)[0], np.asarray(inp["router_expert"], f)[0]], axis=1)),
        "moe_wg": np.ascontiguousarray(np.asarray(inp["moe_w_gate"], f)[0]),
        "moe_wu": np.ascontiguousarray(np.asarray(inp["moe_w_up"], f)[0]),
        "moe_wd": np.ascontiguousarray(np.asarray(inp["moe_w_down"], f)[0]),
        "wpp": np.ascontiguousarray(np.asarray(inp["w_ple_proj"], f)[0]),
        "wpg": np.ascontiguousarray(np.asarray(inp["w_ple_gate"], f)[0]),
        "identf": np.eye(128, dtype=f),
    }
    half = 32
    inv = (1.0 / (np.float32(10000.0) ** (np.arange(half, dtype=f) / np.float32(half)))).astype(f)
    ang = (np.arange(T, dtype=f)[:, None] * inv[None, :]).astype(f)
    cs = np.concatenate([np.cos(ang), np.sin(ang)], axis=1).astype(f)
    shared["cs"] = cs
    ew = np.zeros((128, 64, 128), f)
    for kt in range(64):
        ew[2 * kt, kt, 0:64] = 1.0
        ew[2 * kt + 1, kt, 64:128] = 1.0
    shared["ewide"] = ew.reshape(128, 64 * 128)
    cidx = np.arange(512)
    sidx = np.arange(128)
    ovl = ((cidx[:, None] * 16 < sidx[None, :] * 64 + 64) & (cidx[:, None] * 16 + 32 > sidx[None, :] * 64)).astype(f)
    ovl[511, :] = 0.0
    shared["ovl"] = ovl
    kk = np.arange(128)[:, None]
    tt = np.arange(128)[None, :]
    tri_m = (kk <= tt).astype(f)
    ntri_m = (kk > tt).astype(f)
    onesm = np.ones((128, 128), f)
    zerom = np.zeros((128, 128), f)

    maps = []
    for core in range(8):
        b, c = core // 4, core % 4
        own_tiles = [4 * i + c for i in range(NOWN)]
        tok = np.concatenate([np.arange(128 * qt, 128 * qt + 128) for qt in own_tiles])
        m = dict(shared)
        m["xT"] = np.ascontiguousarray(x[b].T)
        m["xo"] = np.ascontiguousarray(x[b][tok].reshape(NOWN, 128, D))
        m["xTo"] = np.ascontiguousarray(x[b][tok].reshape(NOWN, 128, D).transpose(0, 2, 1))
        m["pTo"] = np.ascontiguousarray(p[b][tok].T)
        m["cso"] = np.ascontiguousarray(cs[tok])
        dm = []
        for r in range(4):
            dm.append(onesm if r < c else (tri_m if r == c else zerom))
        for r in range(8):
            dlt = r - 4 - c
            dm.append(ntri_m if dlt == -4 else (onesm if -3 <= dlt <= -1 else (tri_m if dlt == 0 else zerom)))
        m["dmwm"] = np.ascontiguousarray(((np.stack(dm, axis=1) - 1.0) * 30000.0).astype(f).reshape(128, 12 * 128))
        mC = np.zeros((NOWN, 128, 2, 128), f)
        m12 = np.zeros((NOWN, 128, 256), f)
        for i, qt in enumerate(own_tiles):
            n_ct = (32 * i + 30) // 128 + 1
            t = 128 * qt + np.arange(128)
            for k in range(2):
                ct = n_ct - 2 + k
                if ct < 0:
                    continue
                cc = ct * 128 + np.arange(128)
                mC[i, :, k, :] = ((16 * cc[:, None] + 31) <= t[None, :]).astype(f)
            cur = (t // 64)[:, None]
            s = np.arange(128)[None, :]
            forced = (s == 0) | (s == cur) | (s == cur - 1)
            valid = (s * 64) <= t[:, None]
            m12[i, :, 0:128] = np.where(forced, 0.0, np.where(valid, 1.0, 0.0))
            m12[i, :, 128:256] = np.where(forced, FORCE, np.where(valid, 0.0, -FORCE))
        m["mC"] = np.ascontiguousarray(((mC - 1.0) * 30000.0).astype(f).reshape(NOWN, 128, 256))
        m["m12"] = m12
        maps.append(m)
    return maps


_NC_CACHE = {}


def kernel(**inputs):
    maps = host_inputs(inputs)
    if "nc" not in _NC_CACHE:
        _NC_CACHE["nc"] = build_program()
    nc = _NC_CACHE["nc"]
    res = run_bass_kernel_spmd(nc, maps, core_ids=list(range(8)))
    out = np.zeros((2, T, D), np.float32)
    for core in range(8):
        b, c = core // 4, core % 4
        o = np.asarray(res.results[core]["out"], np.float32)
        for i in range(NOWN):
            qt = 4 * i + c
            out[b, 128 * qt:128 * (qt + 1), :] = o[i]
    return out
```

```python
from contextlib import ExitStack
import types
import numpy as np
import concourse.bass as bass
import concourse.mybir as mybir
from concourse.bass_utils import run_bass_kernel_spmd

F32 = mybir.dt.float32
BF16 = mybir.dt.bfloat16
AF = mybir.ActivationFunctionType
ALU = mybir.AluOpType
AX = mybir.AxisListType

T = 8192
D = 1024
NT = 64
NOWN = 16
EPS = 1e-6
FORCE = 1e6
ENG = ("pe", "act", "dve", "pool", "sp")
SKIP = set()


def _freeze(fn):
    if fn.__closure__ is None:
        return fn
    cells = []
    for c in fn.__closure__:
        try:
            cells.append(types.CellType(c.cell_contents))
        except ValueError:
            cells.append(c)
    return types.FunctionType(fn.__code__, fn.__globals__, fn.__name__, fn.__defaults__, tuple(cells))


class Prog:
    def __init__(self, nc):
        self.nc = nc
        self.ops = []
        self.last_w = {}
        self.readers = {}
        self.stack = ExitStack()
        self.group_keys = set()
        self.pending_alias = {}
        self.ranges = {}
        self._ovl_cache = {}

    def sb(self, name, shape, dt):
        return self.stack.enter_context(self.nc.sbuf_tensor(name, list(shape), dt))

    def ps(self, name, shape, dt):
        return self.stack.enter_context(self.nc.psum_tensor(name, list(shape), dt))

    def reg(self, keys, lo, hi):
        for k in keys:
            if k in self.ranges:
                a, b = self.ranges[k]
                lo2, hi2 = min(a, lo), max(b, hi)
            else:
                lo2, hi2 = lo, hi
            self.ranges[k] = (lo2, hi2)
        self._ovl_cache = {}

    def overlaps(self, k):
        if k not in self.ranges:
            return ()
        if k not in self._ovl_cache:
            lo, hi = self.ranges[k]
            self._ovl_cache[k] = [k2 for k2, (a, b) in self.ranges.items() if k2 != k and a < hi and lo < b]
        return self._ovl_cache[k]

    def alias(self, newkey, oldkeys):
        self.pending_alias.setdefault(newkey, []).extend(oldkeys)

    @staticmethod
    def _is_psum(k):
        return isinstance(k, str) and len(k) == 2 and k[0] == "B" and k[1].isdigit()

    def op(self, eng, fn, r=(), w=(), dma_key=None):
        i = len(self.ops)
        deps = set()
        w = list(w)
        r = list(r)
        xw = [k for k in r if self._is_psum(k) and k not in w]
        extra = []
        for k in w:
            if k in self.pending_alias:
                extra.extend(self.pending_alias.pop(k))
            extra.extend(self.overlaps(k))
        for k in r:
            if k in self.last_w:
                deps.add(self.last_w[k])
        for k in w + extra + xw:
            if k in self.last_w:
                deps.add(self.last_w[k])
            for j in self.readers.get(k, ()):
                deps.add(j)
        deps.discard(i)
        self.ops.append(dict(eng=eng, fn=_freeze(fn), deps=deps, dma=dma_key, r=tuple(r), w=tuple(w)))
        for k in r:
            self.readers.setdefault(k, []).append(i)
        for k in w:
            self.last_w[k] = i
            self.readers[k] = []
        for k in xw:
            self.last_w[k] = i
            self.readers[k] = []
        return i

    def dma(self, eng, out, in_, r=(), w=(), key=None, group=False, **kw):
        if key is None:
            key = w[0] if len(w) else r[0]
        if group:
            self.group_keys.add(key)
        return self.op(eng, lambda e: e.dma_start(out=out, in_=in_, **kw), r=r, w=w, dma_key=key)

    def finalize(self, final_wait_eng="sp"):
        nc = self.nc
        ops = self.ops
        n = len(ops)
        needed = [False] * n
        for i, o in enumerate(ops):
            keep = set()
            for d in o["deps"]:
                od = ops[d]
                if od["dma"] is None and o["dma"] is None and od["eng"] == o["eng"]:
                    if o["eng"] == "pe":
                        continue
                    if not (set(od["w"]) & (set(o["r"]) | set(o["w"]))):
                        continue
                keep.add(d)
            o["deps"] = keep
            for d in keep:
                needed[d] = True
        final_dma = [i for i, o in enumerate(ops) if o["dma"] is not None and not needed[i]]
        for i in final_dma:
            needed[i] = True
        sems = {}
        cnt = {}
        names = {}

        def sem_name(key):
            if key not in names:
                names[key] = "d%d" % len(names)
            return names[key]

        for i, o in enumerate(ops):
            if not needed[i]:
                o["sig"] = None
                continue
            if o["dma"] is not None:
                sname = sem_name(o["dma"])
                inc = 16
            else:
                sname = "e_" + o["eng"]
                inc = 1
            cnt[sname] = cnt.get(sname, 0) + inc
            o["sig"] = (sname, inc, cnt[sname])
            if sname not in sems:
                sems[sname] = self.stack.enter_context(nc.semaphore(sname))
        gfinal = {sem_name(k): cnt.get(sem_name(k), 0) for k in self.group_keys}
        self.n_sems = len(sems)
        plan = {e: [] for e in ENG}
        waited = {e: {} for e in ENG}
        for i, o in enumerate(ops):
            e = o["eng"]
            waits = {}
            for d in o["deps"]:
                sname, inc, val = ops[d]["sig"]
                if sname in gfinal:
                    val = gfinal[sname]
                if waited[e].get(sname, 0) >= val:
                    continue
                waits[sname] = max(waits.get(sname, 0), val)
            for sname, val in waits.items():
                waited[e][sname] = val
            plan[e].append((o, waits))
        final_waits = {}
        for i in final_dma:
            sname, inc, val = ops[i]["sig"]
            final_waits[sname] = max(final_waits.get(sname, 0), val)
        blk = self.stack.enter_context(nc.Block())

        def emit(engobj, ename):
            for o, waits in plan[ename]:
                for sname, val in waits.items():
                    engobj.wait_ge(sems[sname], val)
                ins = o["fn"](engobj)
                if o["sig"] is not None:
                    ins.then_inc(sems[o["sig"][0]], o["sig"][1])
            if ename == final_wait_eng:
                for sname, val in final_waits.items():
                    engobj.wait_ge(sems[sname], val)

        @blk.tensor
        def _(e):
            emit(e, "pe")

        @blk.scalar
        def _(e):
            emit(e, "act")

        @blk.vector
        def _(e):
            emit(e, "dve")

        @blk.gpsimd
        def _(e):
            emit(e, "pool")

        @blk.sync
        def _(e):
            emit(e, "sp")

        self.stack.close()
        return nc


def build_program(dbg=False, stop_after=None):
    nc = bass.Bass("TRN2", target_bir_lowering=False)
    P = Prog(nc)

    in_names = []
    late = stop_after in (None, "B2")

    def din(name, shape, big=False):
        if big and not late:
            shape = [1] * len(shape)
        in_names.append(name)
        return nc.dram_tensor(name, list(shape), F32, kind="ExternalInput").ap()

    def dout(name, shape):
        return nc.dram_tensor(name, list(shape), F32, kind="ExternalOutput").ap()

    xT_d = din("xT", [D, T])
    xTo_d = din("xTo", [NOWN, D, 128])
    xo_d = din("xo", [NOWN, 128, D], big=True)
    pTo_d = din("pTo", [256, NOWN * 128], big=True)
    cs_d = din("cs", [T, 64])
    cso_d = din("cso", [NOWN * 128, 64])
    wkv_d = din("wkv", [D, 768])
    wown_d = din("wown", [D, 1560])
    gmix_d = din("gmix", [128, 8])
    wsT_d = din("wsT", [128, 8 * 128])
    tri_d = din("tri", [128, 128])
    bsT_d = din("bsT", [128, 8])
    gv_d = din("gv", [128, 512])
    ga_d = din("ga", [128, 512])
    gb_d = din("gb", [128, 512])
    gmoe_d = din("gmoe", [128, D], big=True)
    gple_d = din("gple", [128, D], big=True)
    gfin_d = din("gfin", [128, D], big=True)
    w1_d = [din("w1k", [64, 32 * 256]), din("w1v", [64, 32 * 256])]
    peT_d = [din("peTk", [64, 32]), din("peTv", [64, 32])]
    w2_d = [din("w2k", [256, 64]), din("w2v", [256, 64])]
    wo_d = din("wo", [D, D], big=True)
    rcat_d = din("rcat", [D, 20])
    wg_d = din("moe_wg", [16, D, 512], big=True)
    wu_d = din("moe_wu", [16, D, 512], big=True)
    wd_d = din("moe_wd", [16, 512, D], big=True)
    wpp_d = din("wpp", [256, D], big=True)
    wpg_d = din("wpg", [D, D], big=True)
    identf_d = din("identf", [128, 128])
    ewide_d = din("ewide", [128, 64 * 128])
    ovl_d = din("ovl", [512, 128])
    mC_d = din("mC", [NOWN, 128, 2 * 128])
    m12_d = din("m12", [NOWN, 128, 256])
    dmwm_d = din("dmwm", [128, 12 * 128])
    out_d = dout("out", [NOWN, 128, D])
    dbg_d = {}

    def dbg_out(name, shape):
        if dbg:
            dbg_d[name] = dout("dbg_" + name, shape)
        return dbg_d.get(name)

    KB = 1024
    ARENA = 192 * KB
    arena = P.sb("arena", [128, ARENA // 2], BF16)

    def view(off, shape, dt, keys=None):
        esz = 2 if dt == BF16 else 4
        nel = int(np.prod(shape[1:]))
        assert off % 4 == 0 and off + nel * esz <= ARENA, (off, shape)
        if keys is not None:
            P.reg(keys, off, off + nel * esz)
        a = arena[:, off // 2: off // 2 + nel * esz // 2]
        if dt != BF16:
            a = a.bitcast(dt)
        if len(shape) == 2:
            return a
        nm = " ".join("d%d" % i for i in range(1, len(shape)))
        kw = {"d%d" % i: shape[i] for i in range(2, len(shape))}
        return a.rearrange("p (%s) -> p %s" % (nm, nm), **kw)

    KVc = view(0, [128, 2, T], BF16, ["KVc"])
    Wown = view(0, [128, 8, 1560], BF16, ["Wown"])
    Wo = view(0, [128, 8, D], BF16, ["Wo"])
    Wkv = view(130 * KB + 48 * KB, [128, 8, 768], BF16, ["Wkv"])
    W1 = [view(32 * KB, [128, 32, 256], BF16, [("W1", 0)]), view(48 * KB, [128, 32, 256], BF16, [("W1", 1)])]
    catT = view(32 * KB, [128, 8, NOWN * 128], BF16, [("catT", i_) for i_ in range(NOWN)])
    KVs = view(64 * KB, [128, 2, T], BF16, ["KVs"])
    VTM_B = 64 * 2 * 65 * 2
    Vtm = [view(96 * KB, [128, 64, 2, 65], BF16, ["Vtm", "Vtm_one0"]), view(96 * KB + VTM_B, [128, 64, 2, 65], BF16, ["Vtm", "Vtm_one1"])]
    h1 = view(64 * KB, [128, NOWN, D], F32)
    for i_ in range(NOWN):
        P.reg([("h1", i_)], 64 * KB + i_ * 4096, 64 * KB + (i_ + 1) * 4096)
    h1 = h1
    TB = 130 * KB
    hn2T = view(130 * KB, [128, 8, NOWN * 128], BF16, [("hn2T", i_) for i_ in range(NOWN)])
    Wexp = [(view(0, [128, 8, 512], BF16, [("Wg", 0)]), view(8 * KB, [128, 8, 512], BF16, [("Wu", 0)]), view(16 * KB, [128, 4, D], BF16, [("Wd", 0)])),
            (view(32 * KB, [128, 8, 512], BF16, [("Wg", 1)]), view(40 * KB, [128, 8, 512], BF16, [("Wu", 1)]), view(48 * KB, [128, 4, D], BF16, [("Wd", 1)]))]
    Wpg = view(130 * KB, [128, 8, D], BF16, ["Wpg"])
    Wpp = view(146 * KB, [128, 2, D], BF16, ["Wpp"])

    identb = P.sb("identb", [128, 128], BF16)
    identf = P.sb("identf_s", [128, 128], F32)
    ones = P.sb("ones", [128, 16], BF16)
    trib = P.sb("trib", [128, 128], BF16)
    wsT = P.sb("wsT_s", [128, 8, 128], BF16)
    bsT = P.sb("bsT_s", [128, 8], F32)
    gmix = P.sb("gmix_s", [128, 8], F32)
    gv = P.sb("gv_s", [128, 512], F32)
    ga = P.sb("ga_s", [128, 512], F32)
    gb = P.sb("gb_s", [128, 512], F32)
    kcT = P.sb("kcT", [128, 512], BF16)
    vcx = P.sb("vcx", [128, 4, 2, 194], BF16)
    w2 = [P.sb("w2k_s", [128, 2, 64], BF16), P.sb("w2v_s", [128, 2, 64], BF16)]
    peT = [P.sb("peTk_s", [64, 32], BF16), P.sb("peTv_s", [64, 32], BF16)]
    cbias = P.sb("cbias", [128, 4], F32)
    rcat = P.sb("rcat_s", [128, 8, 20], F32)
    comb = P.sb("comb", [128, NOWN, 16], F32)
    pad8 = P.sb("pad8", [128, 8], F32)

    PS = P.ps("PS", [128, 4096], F32)

    def bank(k, lo=0, hi=512):
        return PS[:, k * 512 + lo: k * 512 + hi]

    def bankb(k):
        return PS[:, k * 512:(k + 1) * 512].bitcast(BF16)

    def bk(k, slots=None):
        return ["B%d" % k]

    def bc_mid(ap2d, n):
        return ap2d.unsqueeze(1).to_broadcast([128, n, ap2d.shape[-1]])

    def bc_last(ap2d, n):
        return ap2d.unsqueeze(2).to_broadcast([128, ap2d.shape[-1], n])

    def cload(eng, dst, src, key):
        P.dma(eng, dst, src, w=[key], key="const_" + eng, group=True)

    cload("pool", identb[:], identf_d, "identb")
    cload("sp", identf[:], identf_d, "identf")
    cload("pool", trib[:], tri_d, "trib")
    cload("pool", wsT[:], wsT_d.rearrange("p (g t) -> p g t", t=128), "wsT")
    cload("sp", bsT[:], bsT_d, "bsT")
    cload("sp", gmix[:], gmix_d, "gmix")
    cload("sp", gv[:], gv_d, "gv")
    cload("sp", ga[:], ga_d, "ga")
    cload("sp", gb[:], gb_d, "gb")
    for kv in range(2):
        cload("pool", w2[kv][:], w2_d[kv].rearrange("(h p) d -> p h d", p=128), "w2%d" % kv)
        cload("pool", peT[kv][:], peT_d[kv], "peT%d" % kv)
    cload("sp", rcat[:], rcat_d.rearrange("(c p) n -> p c n", p=128), "rcat")
    P.op("pool", lambda e: e.memset(ones[:], 1.0), w=["ones"])
    P.op("pool", lambda e: e.memset(pad8[:], -1e30), w=["pad8"])
    P.op("pool", lambda e: e.memset(vcx[:].rearrange("p a g d -> p (a g d)"), 1.0), w=["vcx_one"])
    P.op("pool", lambda e: e.memset(Vtm[0].rearrange("p a g d -> p (a g d)"), 1.0), w=["Vtm_one0"])
    P.op("pool", lambda e: e.memset(Vtm[1].rearrange("p a g d -> p (a g d)"), 1.0), w=["Vtm_one1"])
    for g in range(2):
        for ct in range(4):
            P.dma("pool", vcx[:, ct, g, 66:194], ovl_d[ct * 128:(ct + 1) * 128, :], r=["vcx_one"], w=["vcx_ovl"], key="vcx_ovl")
    if "wst" not in SKIP:
        P.op("dve", lambda e: e.tensor_tensor(out=wsT[:], in0=wsT[:], in1=bc_mid(trib[:, :], 8), op=ALU.mult),
             r=["wsT", "trib"], w=["wsT"])

    P.dma("pool", Wkv, wkv_d.rearrange("(c p) n -> p c n", p=128), w=["Wkv"])
    for dc in range(8 if "wkvs" not in SKIP else 0):
        P.op("dve", lambda e, dc=dc: e.tensor_scalar(out=Wkv[:, dc, :], in0=Wkv[:, dc, :], scalar1=gmix[:, dc:dc + 1],
                                                      scalar2=None, op0=ALU.mult), r=["Wkv", "gmix"], w=["Wkv"])

    if stop_after == "const":
        P.finalize()
        nc.in_names = in_names
        return nc

    def rstd_from_ssq(ssq_ap, n, dst, rkeys, wkey):
        P.op("act", lambda e: e.activation(out=dst, in_=ssq_ap, func=AF.Sqrt, scale=1.0 / n, bias=EPS), r=rkeys, w=[wkey])
        P.op("dve", lambda e: e.reciprocal(out=dst, in_=dst), r=[wkey], w=[wkey])

    def rope_tm(src_ps, nblk, csr, dst_bf, tmp, rkeys, tkey, wkey):
        s4 = src_ps.rearrange("p (b h d) -> p b h d", h=2, d=32)
        d4 = dst_bf.rearrange("p (b h d) -> p b h d", h=2, d=32)
        cr = csr[:, 0:32].unsqueeze(1).to_broadcast([128, nblk, 32])
        sr = csr[:, 32:64].unsqueeze(1).to_broadcast([128, nblk, 32])
        tv = [tmp[:, k, :].rearrange("p (b d) -> p b d", d=32) for k in range(4)]
        P.op("dve", lambda e: e.tensor_tensor(out=tv[0], in0=s4[:, :, 0, :], in1=cr, op=ALU.mult), r=rkeys, w=[tkey + "0"])
        P.op("dve", lambda e: e.tensor_tensor(out=tv[1], in0=s4[:, :, 1, :], in1=sr, op=ALU.mult), r=rkeys, w=[tkey + "1"])
        P.op("dve", lambda e: e.tensor_tensor(out=tv[2], in0=s4[:, :, 0, :], in1=sr, op=ALU.mult), r=rkeys, w=[tkey + "2"])
        P.op("dve", lambda e: e.tensor_tensor(out=tv[3], in0=s4[:, :, 1, :], in1=cr, op=ALU.mult), r=rkeys, w=[tkey + "3"])
        P.op("pool", lambda e: e.tensor_tensor(out=d4[:, :, 0, :], in0=tv[0], in1=tv[1], op=ALU.subtract),
             r=[tkey + "0", tkey + "1"], w=[wkey + "a"])
        P.op("pool", lambda e: e.tensor_tensor(out=d4[:, :, 1, :], in0=tv[2], in1=tv[3], op=ALU.add),
             r=[tkey + "2", tkey + "3"], w=[wkey + "b"])

    A_xTb = [view(TB + 0, [128, 8, 256], BF16, [("xTb", 0)]), view(TB + 4 * KB, [128, 8, 256], BF16, [("xTb", 1)])]
    A_xf = [view(TB + 32 * KB, [128, 8, 256], F32, [("xf", 0)]), view(TB + 40 * KB, [128, 8, 256], F32, [("xf", 1)])]
    A_cs = [view(TB + 16 * KB, [128, 2, 64], F32, [("Acs", 0)]), view(TB + 17 * KB, [128, 2, 64], F32, [("Acs", 1)])]
    A_sq = [view(TB + 18 * KB, [128, 8, 128], BF16, [("Asq", 0)]), view(TB + 20 * KB, [128, 8, 128], BF16, [("Asq", 1)])]
    A_tmp = [view(TB + 22 * KB, [128, 4, 192], F32, ["Atmp0%d" % k_ for k_ in range(4)]), view(TB + 25 * KB, [128, 4, 192], F32, ["Atmp1%d" % k_ for k_ in range(4)])]
    A_ktm = [view(TB + 28 * KB, [128, 512], BF16, ["Aktm0a", "Aktm0b", "Aktm0v"]), view(TB + 29 * KB, [128, 512], BF16, ["Aktm1a", "Aktm1b", "Aktm1v"])]
    A_csr = [view(TB + 30 * KB, [128, 64], F32, [("Acsr", 0)]), view(TB + 30 * KB + 256, [128, 64], F32, [("Acsr", 1)])]
    A_rs = view(TB + 31 * KB, [128, 8], F32, [("Ars", 0), ("Ars", 1)])
    A_u = [view(TB + 8 * KB, [128, 384], F32, [("Au", 0, 0), ("Au", 0, 1)]), view(TB + 10 * KB, [128, 384], F32, [("Au", 1, 0), ("Au", 1, 1)])]

    for kv in range(2):
        for half in range(2):
            P.dma("pool", W1[kv][half * 64:(half + 1) * 64], w1_d[kv].rearrange("p (l h) -> p l h", h=256), w=[("W1", kv)])
    n_chunks = 32
    TPC = 2

    def A_front(ck, tl):
        cb = ck % 2
        tile = ck * TPC + tl
        tb = tile % 2
        xs = A_xTb[cb][:, :, tl * 128:(tl + 1) * 128]
        P.op("act", lambda e: e.activation(out=A_sq[tb], in_=xs, func=AF.Square), r=[("xTb", cb)], w=[("Asq", tb)])
        zb = 2 * (tile % 3)
        for dc in range(8):
            P.op("pe", lambda e, dc=dc: e.matmul(bank(zb + 1, 300, 301), lhsT=A_sq[tb][:, dc, :], rhs=ones[:, 0:1],
                                                 start=(dc == 0), stop=(dc == 7)), r=[("Asq", tb), "ones"], w=bk(zb + 1))
        for dc in range(8):
            P.op("pe", lambda e, dc=dc: e.matmul(bank(zb), lhsT=xs[:, dc, :], rhs=Wkv[:, dc, 0:512],
                                                 start=(dc == 0), stop=(dc == 7)), r=[("xTb", cb), "Wkv"], w=bk(zb))
        for dc in range(8):
            P.op("pe", lambda e, dc=dc: e.matmul(bank(zb + 1, 0, 256), lhsT=xs[:, dc, :], rhs=Wkv[:, dc, 512:768],
                                                 start=(dc == 0), stop=(dc == 7)), r=[("xTb", cb), "Wkv"], w=bk(zb + 1))

    def A_back(ck, tl):
        cb = ck % 2
        tile = ck * TPC + tl
        tb = tile % 2
        zb = 2 * (tile % 3)
        rs = A_rs[:, tb:tb + 1]
        s4 = bank(zb, 0, 384).rearrange("p (b h d) -> p b h d", h=2, d=32)
        cr = A_cs[cb][:, tl, 0:32].unsqueeze(1).to_broadcast([128, 6, 32])
        sr = A_cs[cb][:, tl, 32:64].unsqueeze(1).to_broadcast([128, 6, 32])
        tv = [A_tmp[tb][:, k, :].rearrange("p (b d) -> p b d", d=32) for k in range(4)]
        tk = "Atmp%d" % tb
        P.op("dve", lambda e: e.tensor_tensor(out=tv[0], in0=s4[:, :, 0, :], in1=cr, op=ALU.mult), r=bk(zb) + [("Acs", cb)], w=[tk + "0"])
        P.op("dve", lambda e: e.tensor_tensor(out=tv[1], in0=s4[:, :, 1, :], in1=sr, op=ALU.mult), r=bk(zb) + [("Acs", cb)], w=[tk + "1"])
        P.op("dve", lambda e: e.tensor_tensor(out=tv[2], in0=s4[:, :, 0, :], in1=sr, op=ALU.mult), r=bk(zb) + [("Acs", cb)], w=[tk + "2"])
        P.op("dve", lambda e: e.tensor_tensor(out=tv[3], in0=s4[:, :, 1, :], in1=cr, op=ALU.mult), r=bk(zb) + [("Acs", cb)], w=[tk + "3"])
        u4 = A_u[tb].rearrange("p (b h d) -> p b h d", h=2, d=32)
        P.op("pool", lambda e: e.tensor_tensor(out=u4[:, :, 0, :], in0=tv[0], in1=tv[1], op=ALU.subtract), r=[tk + "0", tk + "1"], w=[("Au", tb, 0)])
        P.op("pool", lambda e: e.tensor_tensor(out=u4[:, :, 1, :], in0=tv[2], in1=tv[3], op=ALU.add), r=[tk + "2", tk + "3"], w=[("Au", tb, 1)])
        rstd_from_ssq(bank(zb + 1, 300, 301), D, rs, bk(zb + 1), ("Ars", tb))
        P.op("act", lambda e: e.activation(out=A_ktm[tb][:, 0:384], in_=A_u[tb], func=AF.Copy, scale=rs),
             r=[("Au", tb, 0), ("Au", tb, 1), ("Ars", tb)], w=["Aktm%da" % tb, "Aktm%db" % tb])
        P.op("act", lambda e: e.activation(out=A_ktm[tb][:, 384:512], in_=bank(zb, 384, 512), func=AF.Copy, scale=rs),
             r=bk(zb) + [("Ars", tb)], w=["Aktm%dv" % tb])
        for j in range(2):
            P.op("act", lambda e, j=j: e.activation(
                out=Vtm[j][:, tile, :, 0:64], in_=bank(zb + 1, j * 128, (j + 1) * 128).rearrange("p (g d) -> p g d", d=64),
                func=AF.Copy, scale=rs), r=bk(zb + 1) + [("Ars", tb)], w=["Vtm"])
        tp = bankb(6 + tb)[:, 0:512].rearrange("p (a t) -> p a t", t=128)
        src_order = [0, 3, 1, 2]
        for a_, sblk in enumerate(src_order):
            P.op("pe", lambda e, a_=a_, sblk=sblk: e.transpose(tp[:, a_, :], A_ktm[tb][:, sblk * 128:(sblk + 1) * 128], identb[:]),
                 r=["Aktm%da" % tb, "Aktm%db" % tb, "Aktm%dv" % tb, "identb"], w=bk(6 + tb))
        P.op("dve", lambda e: e.tensor_copy(out=KVc[:, :, tile * 128:(tile + 1) * 128], in_=tp[:, 0:2, :]),
             r=bk(6 + tb), w=["KVc"])
        P.op("act", lambda e: e.activation(out=KVs[:, :, tile * 128:(tile + 1) * 128], in_=tp[:, 2:4, :], func=AF.Copy),
             r=bk(6 + tb), w=["KVs"])

    pend = []
    for ck in range(n_chunks):
        cb = ck % 2
        CT = 128 * TPC
        P.dma("sp", A_xf[cb], xT_d[:, ck * CT:(ck + 1) * CT].rearrange("(c p) t -> p c t", p=128), w=[("xf", cb)])
        P.dma("sp", A_cs[cb], cs_d[ck * CT:(ck + 1) * CT, :].rearrange("(a p) n -> p a n", p=128), w=[("Acs", cb)])
        P.op("pool", lambda e: e.tensor_copy(out=A_xTb[cb], in_=A_xf[cb]), r=[("xf", cb)], w=[("xTb", cb)])
        for tl in range(TPC):
            A_front(ck, tl)
            pend.append((ck, tl))
            if len(pend) > 2:
                A_back(*pend.pop(0))
    while pend:
        A_back(*pend.pop(0))

    if dbg:
        d_kvs = dbg_out("KVs", [128, 2 * T])
        d_kvc = dbg_out("KVc", [128, 2 * T])
        d_vtm = dbg_out("Vtm0", [128, 64 * 130])
        stg = view(TB + 32 * KB, [128, T // 2], F32, ["stg"])
        HT = T // 2
        for hh in range(2):
            for qq in range(2):
                P.op("dve", lambda e, hh=hh, qq=qq: e.tensor_copy(out=stg, in_=KVs[:, hh, qq * HT:(qq + 1) * HT]), r=["KVs"], w=["stg"])
                P.dma("sp", d_kvs[:, hh * T + qq * HT:hh * T + (qq + 1) * HT], stg, r=["stg"], key=("dbgo", 1))
                P.op("dve", lambda e, hh=hh, qq=qq: e.tensor_copy(out=stg, in_=KVc[:, hh, qq * HT:(qq + 1) * HT]), r=["KVc"], w=["stg"])
                P.dma("sp", d_kvc[:, hh * T + qq * HT:hh * T + (qq + 1) * HT], stg, r=["stg"], key=("dbgo", 2))
        for qq in range(4):
            P.op("dve", lambda e, qq=qq: e.tensor_copy(out=stg[:, 0:16 * 130], in_=Vtm[0][:, qq * 16:(qq + 1) * 16].rearrange("p a g d -> p (a g d)")), r=["Vtm", "Vtm_one0"], w=["stg"])
            P.dma("sp", d_vtm[:, qq * 16 * 130:(qq + 1) * 16 * 130], stg[:, 0:16 * 130], r=["stg"], key=("dbgo", 3))
    if stop_after == "A":
        P.finalize()
        nc.in_names = in_names
        return nc

    for kv in range(2):
        for half in range(2):
            col = kv * 2 + half
            for l in range(32):
                P.op("pe", lambda e, kv=kv, half=half, l=l, col=col: e.matmul(
                    bank(4, 8 + col, 9 + col), lhsT=W1[kv][0:64, l, half * 128:(half + 1) * 128], rhs=peT[kv][:, l:l + 1],
                    start=(l == 0), stop=(l == 31)), r=[("W1", kv), "peT%d" % kv], w=bk(4))
    P.op("dve", lambda e: e.tensor_copy(out=cbias[:], in_=bank(4, 8, 12)), r=bk(4), w=["cbias"])
    Ap_hid = [view(TB + 0, [128, 2, 512], BF16, ["hid0"]), view(TB + 2 * KB, [128, 2, 512], BF16, ["hid1"])]
    KVd = [view(TB + 12 * KB, [128, 16, 512], BF16, [("KVd", 0)]), view(TB + 28 * KB, [128, 16, 512], BF16, [("KVd", 1)])]
    P.op("dve", lambda e: e.tensor_copy(out=KVd[0], in_=KVc[:, 0, :].rearrange("p (m b) -> p b m", b=16)), r=["KVc"], w=[("KVd", 0)])
    P.op("pool", lambda e: e.tensor_copy(out=KVd[1], in_=KVc[:, 1, :].rearrange("p (m b) -> p b m", b=16)), r=["KVc"], w=[("KVd", 1)])
    P.alias("hid0", [("xTb", 0)])
    P.alias("hid1", [("xTb", 0)])
    P.op("pool", lambda e: e.memset(Ap_hid[0], 0.0), w=["hid0"])
    P.op("pool", lambda e: e.memset(Ap_hid[1], 0.0), w=["hid1"])
    it = 0
    for kv in range(2):
        for g in range(2):
            hb = it % 2
            it += 1
            hid = Ap_hid[hb]
            for half in range(2):
                hbank = half
                for l in range(32):
                    P.op("pe", lambda e, kv=kv, g=g, half=half, l=l, hbank=hbank: e.matmul(
                        bank(hbank, 0, 511), lhsT=W1[kv][g * 64:(g + 1) * 64, l, half * 128:(half + 1) * 128],
                        rhs=KVd[kv][g * 64:(g + 1) * 64, l % 16, (l // 16):(l // 16) + 511], start=(l == 0), stop=(l == 31)),
                        r=[("W1", kv), ("KVd", kv)], w=bk(hbank))
                P.op("act", lambda e, kv=kv, half=half, hbank=hbank, hid=hid: e.activation(
                    out=hid[:, half, 0:511], in_=bank(hbank, 0, 511), func=AF.Gelu_apprx_tanh, bias=cbias[:, kv * 2 + half:kv * 2 + half + 1]),
                    r=bk(hbank) + ["cbias"], w=["hid%d" % hb])
            if kv == 0:
                for half in range(2):
                    P.op("pe", lambda e, g=g, half=half, hid=hid: e.matmul(PS[g * 64:(g + 1) * 64, 2 * 512:3 * 512], lhsT=w2[0][:, half, :],
                                                                          rhs=hid[:, half, :], start=(half == 0), stop=(half == 1)),
                         r=["hid%d" % hb, "w20"], w=bk(2))
                P.op("dve", lambda e, g=g: e.tensor_copy(out=kcT[g * 64:(g + 1) * 64, :], in_=PS[g * 64:(g + 1) * 64, 2 * 512:3 * 512]),
                     r=bk(2), w=["kcT"])
            else:
                for ct in range(4):
                    for half in range(2):
                        P.op("pe", lambda e, ct=ct, half=half, hid=hid: e.matmul(bank(3, ct * 64, (ct + 1) * 64), lhsT=hid[:, half, ct * 128:(ct + 1) * 128],
                                                                                rhs=w2[1][:, half, :], start=(half == 0), stop=(half == 1)),
                             r=["hid%d" % hb, "w21"], w=bk(3))
                P.op("dve", lambda e, g=g: e.tensor_copy(out=vcx[:, :, g, 0:64], in_=bank(3, 0, 256).rearrange("p (c d) -> p c d", d=64)),
                     r=bk(3) + ["vcx_one"], w=["vcx_v%d" % g])
    VCX = ["vcx_ovl", "vcx_one", "vcx_v0", "vcx_v1"]

    if dbg:
        d_kct = dbg_out("kcT", [128, 512])
        d_vcx = dbg_out("vcx", [128, 4 * 2 * 194])
        stg = view(TB + 32 * KB, [128, 4 * 2 * 194], F32, ["stg"])
        P.op("dve", lambda e: e.tensor_copy(out=stg[:, 0:512], in_=kcT[:]), r=["kcT"], w=["stg"])
        P.dma("sp", d_kct, stg[:, 0:512], r=["stg"], key=("dbgo", 4))
        P.op("dve", lambda e: e.tensor_copy(out=stg, in_=vcx[:].rearrange("p a g d -> p (a g d)")), r=VCX, w=["stg"])
        P.dma("sp", d_vcx, stg, r=["stg"], key=("dbgo", 5))
    if stop_after == "Ap":
        P.finalize()
        nc.in_names = in_names
        return nc

    P.alias("Wown", ["KVc"])
    P.dma("pool", Wown, wown_d.rearrange("(c p) n -> p c n", p=128), w=["Wown"])
    for dc in range(8):
        P.op("dve", lambda e, dc=dc: e.tensor_scalar(out=Wown[:, dc, :], in0=Wown[:, dc, :], scalar1=gmix[:, dc:dc + 1],
                                                      scalar2=None, op0=ALU.mult), r=["Wown", "gmix"], w=["Wown"])
    Ewide = view(TB + 0, [128, 64, 128], BF16, ["Ewide"])
    P.alias("Ewide", [("xTb", 0), ("xTb", 1), "hid0", "hid1"])
    P.dma("pool", Ewide, ewide_d.rearrange("p (k q) -> p k q", q=128), w=["Ewide"])
    o = TB + 16 * KB
    B_xTo = [view(o, [128, 8, 128], BF16, [("xTo", 0)]), view(o + 2 * KB, [128, 8, 128], BF16, [("xTo", 1)])]; o += 4 * KB
    B_sq = view(o, [128, 8, 128], BF16, ["Bsq"]); o += 2 * KB
    B_cso = [view(o, [128, 64], F32, [("cso", 0)]), view(o + 256, [128, 64], F32, [("cso", 1)])]; o += 512
    B_mC = [view(o, [128, 2, 128], BF16, [("mC", 0)]), view(o + 512, [128, 2, 128], BF16, [("mC", 1)])]; o += 1 * KB
    B_m12 = [view(o, [128, 256], F32, [("m12", 0)]), view(o + KB, [128, 256], F32, [("m12", 1)])]; o += 2 * KB
    B_Ocs = view(o, [128, 4 * 194], F32, ["Ocs"])
    B_uv = view(o, [128, 1024], F32, [("uv", 0), ("uv", 1)]); o += 4 * KB
    B_gates = view(o, [128, 24], F32, ["gates"]); o += 128
    B_csr = view(o, [128, 64], F32, ["Bcsr"]); o += 256
    B_OTs = view(o, [128, 512], F32, ["OTs"])
    B_tmp = view(o, [128, 4, 256], F32, ["Btmp%d" % k_ for k_ in range(4)]); o += 4 * KB
    B_qtm = view(o, [128, 512], BF16, ["Bqtma", "Bqtmb"]); o += KB
    B_QZ = [view(o, [128, 512], BF16, [("QZ", 0)]), view(o + KB, [128, 512], BF16, [("QZ", 1)])]; o += 2 * KB
    B_vn = view(o, [128, 512], BF16, ["vn"]); o += KB
    B_oa = view(o, [128, 512], F32, ["oa"]); o += 2 * KB
    B_junk = view(o, [128, 1024], BF16, ["junk"]); o += 2 * KB
    B_cat = view(o, [128, 1024], BF16, ["cat_a", "cat_b"]); o += 2 * KB
    B_Pt = [view(o + k * KB, [128, 512], BF16, [("Pt", k)]) for k in range(3)]; o += 3 * KB
    B_imp = view(o, [128, 128], F32, ["imp"]); o += 512
    B_score = view(o, [128, 128], F32, ["score"]); o += 512
    B_wk = view(o, [128, 128], F32, ["wk"]); o += 512
    B_m8 = view(o, [128, 16], F32, ["m8a", "m8b"]); o += 64
    B_sel = view(o, [128, 128], BF16, ["sel"]); o += 256
    B_selT = view(o, [128, 4, 128], BF16, ["selT"]); o += KB
    B_rd = view(o, [128, 16], F32, ["rdc", "rds", "rdw"]); o += 64
    B_fac = view(o, [128, 16], F32, ["fac"]); o += 64
    B_ob = view(o, [128, 512], F32, [("ob", 0), ("ob", 1)]); o += 2 * KB
    B_t2 = view(o, [128, 256], F32, ["t2"]); o += KB
    B_rs = view(o, [128, 8], F32, ["Brs", "Brsv", "Brsa", "Brsb"]); o += 32
    dmwm = view(o, [128, 12, 128], BF16, ["dmwm"]); o += 3 * KB
    P.dma("pool", dmwm, dmwm_d.rearrange("p (r q) -> p r q", q=128), w=["dmwm"])
    P.op("pool", lambda e: e.memset(B_QZ[0], 0.0), w=[("QZ", 0)])
    P.op("pool", lambda e: e.memset(B_QZ[1], 0.0), w=[("QZ", 1)])
    assert o <= ARENA, o

    d_oa = dbg_out("oa", [128, 512])
    d_ob = dbg_out("ob", [128, 512])
    d_score = dbg_out("score", [128, 256])
    d_imp = dbg_out("imp", [128, 256])
    d_oc = dbg_out("Oc", [128, 2 * 776])
    d_os = dbg_out("Os", [128, 1024])
    d_ow = dbg_out("Ow", [128, 1024])
    d_gates = dbg_out("gates", [128, 24])
    DBG_TILE = 1

    pcount = [0]
    scount = [0]

    LA = 3
    SBANKS = [0, 1, 2, 7]
    pipe = []

    def pipe_step(s1, later):
        s1()
        pipe.append(later)
        if len(pipe) > LA:
            pipe.pop(0)()

    def pipe_flush():
        while pipe:
            pipe.pop(0)()

    def nsa_group(i, g):
        Qg = B_QZ[g]
        mb = i % 2
        OcK = bk(3) + bk(4)
        OsK = bk(5)
        OwK = bk(6)
        Oc = PS[:, 3 * 512:5 * 512].rearrange("p (j x) -> p j x", x=256)
        Os = bank(5).rearrange("p (j x) -> p j x", x=128)
        Ow = bank(6).rearrange("p (j x) -> p j x", x=128)
        Ocs = B_Ocs.rearrange("p (j x) -> p j x", x=194)

        def score_step(lhsT, lkeys, masks):
            sbk = SBANKS[scount[0] % len(SBANKS)]
            scount[0] += 1

            def s1():
                P.op("pe", lambda e: e.matmul(bank(sbk), lhsT=lhsT, rhs=Qg, start=True, stop=(len(masks) == 0), skip_group_check=True),
                     r=lkeys + [("QZ", g)], w=bk(sbk))
                for mi_, (ml, mr, mk) in enumerate(masks):
                    if mr.shape[-1] == 512:
                        last = (mi_ == len(masks) - 1)
                        P.op("pe", lambda e, ml=ml, mr=mr, last=last: e.matmul(bank(sbk), lhsT=ml, rhs=mr, start=False, stop=last, skip_group_check=True),
                             r=mk, w=bk(sbk))
                        continue
                    for j in range(4):
                        last = (mi_ == len(masks) - 1) and j == 3
                        P.op("pe", lambda e, ml=ml, mr=mr, j=j, last=last: e.matmul(bank(sbk, j * 128, (j + 1) * 128), lhsT=ml, rhs=mr,
                                                                                  start=False, stop=last, skip_group_check=True),
                             r=mk, w=bk(sbk))
            return sbk, s1

        def exp_pv(sbk, rhs_of_j, rkeys, outs, okeys, first, lastf, post=None):
            def later():
                pb = pcount[0] % 3
                pcount[0] += 1
                P.op("act", lambda e: e.activation(out=B_Pt[pb], in_=bank(sbk), func=AF.Exp), r=bk(sbk), w=[("Pt", pb)])
                for j in range(4):
                    P.op("pe", lambda e, j=j: e.matmul(outs[j], lhsT=B_Pt[pb][:, j * 128:(j + 1) * 128], rhs=rhs_of_j,
                                                       start=(first and j in okeys[1]), stop=lastf, skip_group_check=True),
                         r=[("Pt", pb)] + rkeys, w=okeys[0])
                if post is not None:
                    post()
            return later

        def exp_pvT(sbk, vext, rkeys, obank, okey, first, lastf, post=None):
            def later():
                pb = pcount[0] % 3
                pcount[0] += 1
                P.op("act", lambda e: e.activation(out=B_Pt[pb], in_=bank(sbk), func=AF.Exp), r=bk(sbk), w=[("Pt", pb)])
                P.op("pe", lambda e: e.matmul(PS[0:65, obank * 512:(obank + 1) * 512], lhsT=vext, rhs=B_Pt[pb], start=first, stop=lastf, skip_group_check=True),
                     r=[("Pt", pb)] + rkeys, w=okey)
                if post is not None:
                    post()
            return later

        def untranspose(obank, okey):
            P.op("act", lambda e: e.activation(out=B_OTs[0:65, :], in_=PS[0:65, obank * 512:(obank + 1) * 512], func=AF.Copy), r=okey, w=["OTs"])
            for j in range(4):
                P.op("pe", lambda e, j=j: e.transpose(bank(obank, j * 128, j * 128 + 65), B_OTs[0:65, j * 128:(j + 1) * 128], identf[0:65, 0:65]),
                     r=["OTs", "identf"], w=okey)

        n_ct = (32 * i + 30) // 128 + 1
        oc_outs = [PS[:, 3 * 512 + j * 256: 3 * 512 + j * 256 + 194] for j in range(4)]

        def post_cmp():
            P.op("act", lambda e: e.activation(out=Ocs, in_=Oc[:, :, 0:194], func=AF.Copy), r=OcK, w=["Ocs"])
            P.op("dve", lambda e: e.tensor_scalar(out=B_rd[:, 0:4], in0=Ocs[:, :, 64], scalar1=1e-30, scalar2=None, op0=ALU.max), r=["Ocs"], w=["rdc"])
            P.op("dve", lambda e: e.reciprocal(out=B_rd[:, 0:4], in_=B_rd[:, 0:4]), r=["rdc"], w=["rdc"])
            P.op("dve", lambda e: e.tensor_scalar(out=B_imp, in0=Ocs[:, 0, 66:194], scalar1=B_rd[:, 0:1], scalar2=None, op0=ALU.mult), r=["Ocs", "rdc"], w=["imp"])
            for j in range(1, 4):
                P.op("dve", lambda e, j=j: e.scalar_tensor_tensor(out=B_imp, in0=Ocs[:, j, 66:194], scalar=B_rd[:, j:j + 1], in1=B_imp, op0=ALU.mult, op1=ALU.add),
                     r=["Ocs", "rdc", "imp"], w=["imp"])
            P.op("dve", lambda e: e.tensor_tensor(out=B_score, in0=B_imp, in1=B_m12[mb][:, 0:128], op=ALU.mult), r=["imp", ("m12", mb)], w=["score"])
            P.op("dve", lambda e: e.tensor_tensor(out=B_score, in0=B_score, in1=B_m12[mb][:, 128:256], op=ALU.add), r=["score", ("m12", mb)], w=["score"])
            if dbg and i == DBG_TILE:
                P.dma("sp", d_score[:, g * 128:(g + 1) * 128], B_score, r=["score"], key=("dbgo", 200 + g))
                P.dma("sp", d_imp[:, g * 128:(g + 1) * 128], B_imp, r=["imp"], key=("dbgo", 202 + g))
                P.dma("sp", d_oc[:, g * 776:(g + 1) * 776], B_Ocs, r=["Ocs"], key=("dbgo", 204 + g))
            P.op("dve", lambda e: e.max(out=B_m8[:, 0:8], in_=B_score), r=["score"], w=["m8a"])
            P.op("dve", lambda e: e.match_replace(out=B_wk, in_to_replace=B_m8[:, 0:8], in_values=B_score, imm_value=-1e30), r=["score", "m8a"], w=["wk"])
            P.op("dve", lambda e: e.max(out=B_m8[:, 8:16], in_=B_wk), r=["wk"], w=["m8b"])
            P.op("dve", lambda e: e.tensor_scalar(out=B_sel, in0=B_score, scalar1=B_m8[:, 15:16], scalar2=None, op0=ALU.is_ge), r=["score", "m8b"], w=["sel"])
            P.op("dve", lambda e: e.tensor_scalar(out=B_sel, in0=B_sel, scalar1=-1.0, scalar2=30000.0, op0=ALU.add, op1=ALU.mult), r=["sel"], w=["sel"])
            tpv = bankb(3)[:, 0:128]
            P.op("pe", lambda e: e.transpose(tpv, B_sel, identb[:]), r=["sel", "identb"], w=bk(3))
            P.op("dve", lambda e: e.tensor_copy(out=B_selT, in_=bc_mid(tpv, 4)), r=bk(3), w=["selT"])

        for ct in range(n_ct):
            masks = []
            if ct >= n_ct - 2:
                mi = ct - (n_ct - 2)
                masks.append((identb[:], B_mC[mb][:, mi, :], ["identb", ("mC", mb)]))
            sbk, s1 = score_step(kcT[:, ct * 128:(ct + 1) * 128], ["kcT"], masks)
            pipe_step(s1, exp_pv(sbk, vcx[:, ct, g, :], VCX, oc_outs, (OcK, (0, 2)), ct == 0, ct == n_ct - 1,
                                 post=(post_cmp if ct == n_ct - 1 else None)))

        ow_outs = [bank(6, j * 128, j * 128 + 65) for j in range(4)]
        rlist = [r_ for r_ in range(8) if 4 * i - 4 + r_ >= 0]
        for r_ in rlist:
            kt = 4 * i - 4 + r_
            masks = [(identb[:], dmwm[:, 4 + r_, :], ["identb", "dmwm"])]
            sbk, s1 = score_step(KVs[:, 1, kt * 128:(kt + 1) * 128], ["KVs"], masks)
            pipe_step(s1, exp_pv(sbk, Vtm[1][:, kt, g, :], ["Vtm", "Vtm_one1"], ow_outs, (OwK, (0,)), r_ == rlist[0], r_ == rlist[-1]))

        os_outs = [bank(5, j * 128, j * 128 + 65) for j in range(4)]
        n_kt = 4 * i + 4

        def post_group():
            if dbg and i == DBG_TILE:
                stg = view(TB + 58 * KB, [128, 1024], F32, ["stg2"])
                P.op("dve", lambda e: e.memset(stg, 0.0), w=["stg2"])
                P.op("dve", lambda e: e.tensor_copy(out=stg[:, 0:512].rearrange("p (j x) -> p j x", x=128)[:, :, 0:65], in_=Os[:, :, 0:65]), r=OsK + ["stg2"], w=["stg2"])
                P.dma("sp", d_os[:, g * 512:(g + 1) * 512], stg[:, 0:512], r=["stg2"], key=("dbgo", 102 + g))
                P.op("dve", lambda e: e.tensor_copy(out=stg[:, 0:512].rearrange("p (j x) -> p j x", x=128)[:, :, 0:65], in_=Ow[:, :, 0:65]), r=OwK, w=["stg2"])
                P.dma("sp", d_ow[:, g * 512:(g + 1) * 512], stg[:, 0:512], r=["stg2"], key=("dbgo", 104 + g))
            P.op("dve", lambda e: e.reciprocal(out=B_rd[:, 4:8], in_=Os[:, :, 64]), r=OsK, w=["rds"])
            P.op("dve", lambda e: e.reciprocal(out=B_rd[:, 8:12], in_=Ow[:, :, 64]), r=OwK, w=["rdw"])
            gsl = B_gates[:, g * 12:(g + 1) * 12].rearrange("p (h b) -> p b h", b=3)
            P.op("dve", lambda e: e.tensor_tensor(out=B_fac[:, 0:12].rearrange("p (b h) -> p b h", h=4), in0=B_rd[:, 0:12].rearrange("p (b h) -> p b h", h=4),
                                                  in1=gsl, op=ALU.mult), r=["rdc", "rds", "rdw", "gates"], w=["fac"])
            obg = B_ob[:, g * 256:(g + 1) * 256].rearrange("p (j d) -> p j d", d=64)
            t2 = B_t2.rearrange("p (j d) -> p j d", d=64)
            P.op("pool", lambda e: e.tensor_tensor(out=obg, in0=Ocs[:, :, 0:64], in1=bc_last(B_fac[:, 0:4], 64), op=ALU.mult), r=["Ocs", "fac"], w=[("ob", g)])
            P.op("dve", lambda e: e.tensor_tensor(out=t2, in0=Os[:, :, 0:64], in1=bc_last(B_fac[:, 4:8], 64), op=ALU.mult), r=OsK + ["fac"], w=["t2"])
            P.op("pool", lambda e: e.tensor_tensor(out=obg, in0=obg, in1=t2, op=ALU.add), r=[("ob", g), "t2"], w=[("ob", g)])
            P.op("dve", lambda e: e.tensor_tensor(out=t2, in0=Ow[:, :, 0:64], in1=bc_last(B_fac[:, 8:12], 64), op=ALU.mult), r=OwK + ["fac"], w=["t2"])
            P.op("pool", lambda e: e.tensor_tensor(out=obg, in0=obg, in1=t2, op=ALU.add), r=[("ob", g), "t2"], w=[("ob", g)])

        for kt in range(n_kt):
            masks = [(Ewide[:, kt, :], B_selT.rearrange("p j q -> p (j q)"), ["Ewide", "selT"])]
            if kt >= 4 * i:
                masks.append((identb[:], dmwm[:, kt - 4 * i, :], ["identb", "dmwm"]))
            sbk, s1 = score_step(KVs[:, 0, kt * 128:(kt + 1) * 128], ["KVs"], masks)

            def post_sel():
                untranspose(5, OsK)
                post_group()
            pipe_step(s1, exp_pv(sbk, Vtm[0][:, kt, g, :], ["Vtm", "Vtm_one0"], os_outs, (OsK, (0,)), kt == 0, kt == n_kt - 1,
                                 post=(post_group if kt == n_kt - 1 else None)))

    n_own = NOWN if stop_after not in ("B1x",) else 2
    for i in range(n_own):
        ib = i % 2
        P.dma("pool", B_xTo[ib], xTo_d[i].rearrange("(c p) t -> p c t", p=128), w=[("xTo", ib)])
        P.dma("sp", B_cso[ib], cso_d[i * 128:(i + 1) * 128, :], w=[("cso", ib)])
        P.dma("pool", B_mC[ib], mC_d[i].rearrange("p (k q) -> p k q", q=128), w=[("mC", ib)])
        P.dma("sp", B_m12[ib], m12_d[i], w=[("m12", ib)])
        xs = B_xTo[ib]
        P.op("act", lambda e, xs=xs: e.activation(out=B_sq, in_=xs, func=AF.Square), r=[("xTo", ib)], w=["Bsq"])
        for dc in range(8):
            P.op("pe", lambda e, dc=dc: e.matmul(bank(6, 0, 1), lhsT=B_sq[:, dc, :], rhs=ones[:, 0:1], start=(dc == 0), stop=(dc == 7)),
                 r=["Bsq", "ones"], w=bk(6))
        for half in range(2):
            for dc in range(8):
                P.op("pe", lambda e, dc=dc, half=half, xs=xs: e.matmul(bank(half), lhsT=xs[:, dc, :], rhs=Wown[:, dc, half * 512:(half + 1) * 512],
                                                                     start=(dc == 0), stop=(dc == 7)), r=[("xTo", ib), "Wown"], w=bk(half))
        for dc in range(8):
            P.op("pe", lambda e, dc=dc, xs=xs: e.matmul(bank(2), lhsT=xs[:, dc, :], rhs=Wown[:, dc, 1024:1536], start=(dc == 0), stop=(dc == 7)),
                 r=[("xTo", ib), "Wown"], w=bk(2))
        for dc in range(8):
            P.op("pe", lambda e, dc=dc, xs=xs: e.matmul(bank(5, 0, 24), lhsT=xs[:, dc, :], rhs=Wown[:, dc, 1536:1560], start=(dc == 0), stop=(dc == 7)),
                 r=[("xTo", ib), "Wown"], w=bk(5))
        rs = B_rs[:, 0:1]
        rstd_from_ssq(bank(6, 0, 1), D, rs, bk(6), "Brs")
        for half in range(2):
            P.op("act", lambda e, half=half: e.activation(out=B_uv[:, half * 512:(half + 1) * 512], in_=bank(half), func=AF.Gelu_apprx_tanh, scale=rs),
                 r=bk(half) + ["Brs"], w=[("uv", half)])
        P.op("act", lambda e: e.activation(out=B_gates, in_=bank(5, 0, 24), func=AF.Sigmoid, scale=rs), r=bk(5) + ["Brs"], w=["gates"])
        P.op("dve", lambda e, ib=ib: e.tensor_scalar(out=B_csr, in0=B_cso[ib], scalar1=rs, scalar2=0.125, op0=ALU.mult, op1=ALU.mult),
             r=[("cso", ib), "Brs"], w=["Bcsr"])
        rope_tm(bank(2), 8, B_csr, B_qtm, B_tmp, bk(2) + ["Bcsr"], "Btmp", "Bqtm")
        tq = bankb(7)[:, 0:512].rearrange("p (j t) -> p j t", t=128)
        for j in range(4):
            P.op("pe", lambda e, j=j: e.transpose(tq[:, j, :], B_qtm[:, j * 128:(j + 1) * 128], identb[:]), r=["Bqtma", "Bqtmb", "identb"], w=bk(7, (0, 1)))
        for gq in range(2):
            P.op("act", lambda e, gq=gq: e.activation(out=B_QZ[gq][gq * 64:(gq + 1) * 64].rearrange("p (j q) -> p j q", q=128),
                                                      in_=tq[gq * 64:(gq + 1) * 64], func=AF.Copy), r=bk(7), w=[("QZ", gq)])
        P.op("act", lambda e: e.activation(out=B_junk[:, 0:512], in_=B_uv[:, 512:1024], func=AF.Square, accum_out=B_rs[:, 1:2]), r=[("uv", 1)], w=["junk", "Brsv"])
        rstd_from_ssq(B_rs[:, 1:2], 512, B_rs[:, 1:2], ["Brsv"], "Brsv")
        P.op("dve", lambda e: e.scalar_tensor_tensor(out=B_vn, in0=B_uv[:, 512:1024], scalar=B_rs[:, 1:2], in1=gv[:], op0=ALU.mult, op1=ALU.mult),
             r=[("uv", 1), "Brsv", "gv"], w=["vn"])
        for g8 in range(8):
            P.op("pe", lambda e, g8=g8: e.matmul(bank(2, g8 * 64, (g8 + 1) * 64), lhsT=wsT[:, g8, :], rhs=B_vn[:, g8 * 64:(g8 + 1) * 64], start=True, stop=True),
                 r=["wsT", "vn"], w=bk(2))
        oa3 = B_oa.rearrange("p (g d) -> p g d", d=64)
        P.op("dve", lambda e: e.tensor_tensor(out=oa3, in0=bank(2).rearrange("p (g d) -> p g d", d=64), in1=bc_last(bsT[:, :], 64), op=ALU.add),
             r=bk(2) + ["bsT"], w=["oa"])
        P.op("dve", lambda e: e.tensor_tensor(out=B_oa, in0=B_oa, in1=B_uv[:, 0:512], op=ALU.mult), r=["oa", ("uv", 0)], w=["oa"])
        P.op("act", lambda e: e.activation(out=B_junk[:, 0:512], in_=B_oa, func=AF.Square, accum_out=B_rs[:, 2:3]), r=["oa"], w=["junk", "Brsa"])
        rstd_from_ssq(B_rs[:, 2:3], 512, B_rs[:, 2:3], ["Brsa"], "Brsa")
        P.op("dve", lambda e: e.scalar_tensor_tensor(out=B_cat[:, 0:512], in0=B_oa, scalar=B_rs[:, 2:3], in1=ga[:], op0=ALU.mult, op1=ALU.mult),
             r=["oa", "Brsa", "ga"], w=["cat_a"])
        if dbg and i == DBG_TILE:
            stg = view(TB + 58 * KB, [128, 1024], F32, ["stg2"])
            P.dma("sp", d_oa, B_oa, r=["oa"], key=("dbgo", 12))
            P.dma("sp", d_gates, B_gates, r=["gates"], key=("dbgo", 13))
        for g in range(2):
            nsa_group(i, g)
        pipe_flush()
        if dbg and i == DBG_TILE:
            P.dma("sp", d_ob, B_ob, r=[("ob", 0), ("ob", 1)], key=("dbgo", 14))
        P.op("act", lambda e: e.activation(out=B_junk[:, 0:512], in_=B_ob, func=AF.Square, accum_out=B_rs[:, 3:4]), r=[("ob", 0), ("ob", 1)], w=["junk", "Brsb"])
        rstd_from_ssq(B_rs[:, 3:4], 512, B_rs[:, 3:4], ["Brsb"], "Brsb")
        P.op("dve", lambda e: e.scalar_tensor_tensor(out=B_cat[:, 512:1024], in0=B_ob, scalar=B_rs[:, 3:4], in1=gb[:], op0=ALU.mult, op1=ALU.mult),
             r=[("ob", 0), ("ob", 1), "Brsb", "gb"], w=["cat_b"])
        tc = bankb(7).rearrange("p (k t) -> p k t", t=128)
        for kc in range(8):
            P.op("pe", lambda e, kc=kc: e.transpose(tc[:, kc, :], B_cat[:, kc * 128:(kc + 1) * 128], identb[:]), r=["cat_a", "cat_b", "identb"], w=bk(7))
        if i == 0:
            P.alias(("catT", 0), [("W1", 0), ("W1", 1)])
        P.op("act", lambda e, i=i: e.activation(out=catT[:, :, i * 128:(i + 1) * 128], in_=tc, func=AF.Copy), r=bk(7), w=[("catT", i)])

    if stop_after in ("B1", "B1x"):
        if dbg:
            d_catT = dbg_out("catT", [128, 8 * NOWN * 128])
            for kc in range(8):
                stg = view(TB + 58 * KB, [128, 1024], F32, ["stg2"])
                for hh in range(2):
                    P.op("dve", lambda e, kc=kc, hh=hh: e.tensor_copy(out=stg, in_=catT[:, kc, hh * 1024:(hh + 1) * 1024]),
                         r=[("catT", i) for i in range(n_own)], w=["stg2"])
                    P.dma("sp", d_catT[:, kc * 2048 + hh * 1024: kc * 2048 + (hh + 1) * 1024], stg, r=["stg2"], key=("dbgo", 15))
        P.finalize()
        nc.in_names = in_names
        return nc

    P.dma("pool", Wo, wo_d.rearrange("(c p) n -> p c n", p=128), w=["Wo"])
    C_gmoe = view(16 * KB, [128, D], F32, ["gmoe"])
    C_hn2 = [view(20 * KB, [128, D], F32, [("hn2", 0)]), view(24 * KB, [128, D], F32, [("hn2", 1)])]
    o = TB + 32 * KB
    C_xo = [view(o, [128, D], F32, [("xo", 0)]), view(o + 4 * KB, [128, D], F32, [("xo", 1)])]; o += 8 * KB
    C_hn2Tf = [view(o, [128, 8, 128], F32, [("hn2Tf", 0)]), view(o + 4 * KB, [128, 8, 128], F32, [("hn2Tf", 1)])]; o += 8 * KB
    C_junk = view(o, [128, D], BF16, ["Cjunk"]); o += 2 * KB
    RK2 = ["q_mg", "q_sg", "q_l1", "q_l2", "q_e2", "rstd2"] + [("ssq2", i_) for i_ in range(NOWN)]
    C_lgall = view(o, [128, NOWN, 20], F32, [("lg", i_) for i_ in range(NOWN)]); o += 1280
    C_r = view(o, [128, 12, NOWN * 4], F32, ["r_ohg", "r_dg", "r_les", "r_eq1", "r_x2", "r_sel2", "r_ee"]); o += 3072
    C_q = view(o, [128, 12, NOWN], F32, RK2); o += 768
    C_t44 = view(o, [128, NOWN * 16], F32, ["t44"]); o += 1024
    assert o <= ARENA
    P.dma("sp", C_gmoe, gmoe_d, w=["gmoe"])


    def B2_front(i):
        ib = i % 2
        hb = 0 if i % 2 == 0 else 2
        P.dma("sp", C_xo[ib], xo_d[i], w=[("xo", ib)])
        for half in range(2):
            for kc in range(8):
                P.op("pe", lambda e, kc=kc, half=half: e.matmul(bank(hb + half), lhsT=catT[:, kc, i * 128:(i + 1) * 128], rhs=Wo[:, kc, half * 512:(half + 1) * 512],
                                                              start=(kc == 0), stop=(kc == 7)), r=[("catT", i), "Wo"], w=bk(hb + half))

    def B2_back(i):
        ib = i % 2
        hb = 0 if i % 2 == 0 else 2
        for half in range(2):
            P.op("dve", lambda e, half=half: e.tensor_tensor(out=h1[:, i, half * 512:(half + 1) * 512], in0=bank(hb + half),
                                                             in1=C_xo[ib][:, half * 512:(half + 1) * 512], op=ALU.add),
                 r=bk(hb + half) + [("xo", ib)], w=[("h1", i)])
        P.op("act", lambda e: e.activation(out=C_junk, in_=h1[:, i, :], func=AF.Square, accum_out=C_q[:, 0, i:i + 1]), r=[("h1", i)], w=["Cjunk", ("ssq2", i)])

    for i in range(NOWN):
        B2_front(i)
        if i > 0:
            B2_back(i - 1)
    B2_back(NOWN - 1)
    SS2 = [("ssq2", i) for i in range(NOWN)]
    P.op("act", lambda e: e.activation(out=C_q[:, 1, :], in_=C_q[:, 0, :], func=AF.Sqrt, scale=1.0 / D, bias=EPS), r=SS2, w=["rstd2"])
    P.op("dve", lambda e: e.reciprocal(out=C_q[:, 1, :], in_=C_q[:, 1, :]), r=["rstd2"], w=["rstd2"])

    def B2c_front(i):
        ib = i % 2
        P.op("dve", lambda e: e.scalar_tensor_tensor(out=C_hn2[ib], in0=h1[:, i, :], scalar=C_q[:, 1, i:i + 1], in1=C_gmoe, op0=ALU.mult, op1=ALU.mult),
             r=[("h1", i), "rstd2", "gmoe"], w=[("hn2", ib)])

    def B2c_back(i):
        ib = i % 2
        tb_ = 4 if i % 2 == 0 else 6
        tf = PS[:, tb_ * 512:(tb_ + 2) * 512].rearrange("p (c t) -> p c t", t=128)
        for dc in range(8):
            P.op("pe", lambda e, dc=dc: e.transpose(tf[:, dc, :], C_hn2[ib][:, dc * 128:(dc + 1) * 128], identf[:]), r=[("hn2", ib), "identf"], w=bk(tb_) + bk(tb_ + 1))
        P.op("act", lambda e: e.activation(out=C_hn2Tf[ib], in_=tf, func=AF.Copy), r=bk(tb_) + bk(tb_ + 1), w=[("hn2Tf", ib)])
        P.op("dve", lambda e: e.tensor_copy(out=hn2T[:, :, i * 128:(i + 1) * 128], in_=tf), r=bk(tb_) + bk(tb_ + 1), w=[("hn2T", i)])
        for dc in range(8):
            P.op("pe", lambda e, dc=dc: e.matmul(bank(tb_, 0, 20), lhsT=C_hn2Tf[ib][:, dc, :], rhs=rcat[:, dc, :], start=(dc == 0), stop=(dc == 7)),
                 r=[("hn2Tf", ib), "rcat"], w=bk(tb_))
        P.op("act", lambda e: e.activation(out=C_lgall[:, i, :], in_=bank(tb_, 0, 20), func=AF.Copy), r=bk(tb_), w=[("lg", i)])

    for i in range(NOWN):
        B2c_front(i)
        if i > 0:
            B2c_back(i - 1)
    B2c_back(NOWN - 1)

    LGA = [("lg", i) for i in range(NOWN)]
    G3 = C_lgall[:, :, 0:4]
    E4 = C_lgall[:, :, 4:20].rearrange("p t (g e) -> p t g e", e=4)
    R3 = lambda k: C_r[:, k, :].rearrange("p (t x) -> p t x", x=4)
    Q = lambda k: C_q[:, k, :]
    bq = lambda k: C_q[:, k, :].unsqueeze(2).to_broadcast([128, NOWN, 4])
    P.op("dve", lambda e: e.tensor_reduce(out=Q(2), in_=G3, axis=AX.X, op=ALU.max), r=LGA, w=["q_mg"])
    P.op("dve", lambda e: e.tensor_tensor(out=R3(0), in0=G3, in1=bq(2), op=ALU.is_equal), r=LGA + ["q_mg"], w=["r_ohg"])
    P.op("dve", lambda e: e.tensor_tensor(out=R3(1), in0=G3, in1=bq(2), op=ALU.subtract), r=LGA + ["q_mg"], w=["r_dg"])
    P.op("act", lambda e: e.activation(out=R3(1), in_=R3(1), func=AF.Exp), r=["r_dg"], w=["r_dg"])
    P.op("dve", lambda e: e.tensor_reduce(out=Q(3), in_=R3(1), axis=AX.X, op=ALU.add), r=["r_dg"], w=["q_sg"])
    P.op("dve", lambda e: e.reciprocal(out=Q(3), in_=Q(3)), r=["q_sg"], w=["q_sg"])
    t44 = C_t44.rearrange("p (t g e) -> p t g e", g=4, e=4)
    P.op("dve", lambda e: e.tensor_tensor(out=t44, in0=E4, in1=R3(0).unsqueeze(3).to_broadcast([128, NOWN, 4, 4]), op=ALU.mult), r=LGA + ["r_ohg"], w=["t44"])
    P.op("dve", lambda e: e.tensor_reduce(out=R3(2), in_=C_t44.rearrange("p (t g e) -> p t e g", g=4, e=4), axis=AX.X, op=ALU.add), r=["t44"], w=["r_les"])
    P.op("dve", lambda e: e.tensor_reduce(out=Q(4), in_=R3(2), axis=AX.X, op=ALU.max), r=["r_les"], w=["q_l1"])
    P.op("dve", lambda e: e.tensor_tensor(out=R3(3), in0=R3(2), in1=bq(4), op=ALU.is_equal), r=["r_les", "q_l1"], w=["r_eq1"])
    P.op("dve", lambda e: e.scalar_tensor_tensor(out=C_r[:, 4, :], in0=C_r[:, 3, :], scalar=-1e30, in1=C_r[:, 2, :], op0=ALU.mult, op1=ALU.add), r=["r_eq1", "r_les"], w=["r_x2"])
    P.op("dve", lambda e: e.tensor_reduce(out=Q(5), in_=R3(4), axis=AX.X, op=ALU.max), r=["r_x2"], w=["q_l2"])
    P.op("dve", lambda e: e.tensor_tensor(out=R3(5), in0=R3(2), in1=bq(5), op=ALU.is_ge), r=["r_les", "q_l2"], w=["r_sel2"])
    P.op("dve", lambda e: e.tensor_tensor(out=R3(6), in0=R3(2), in1=bq(4), op=ALU.subtract), r=["r_les", "q_l1"], w=["r_ee"])
    P.op("act", lambda e: e.activation(out=R3(6), in_=R3(6), func=AF.Exp), r=["r_ee"], w=["r_ee"])
    P.op("dve", lambda e: e.tensor_tensor(out=Q(6), in0=Q(5), in1=Q(4), op=ALU.subtract), r=["q_l2", "q_l1"], w=["q_e2"])
    P.op("act", lambda e: e.activation(out=Q(6), in_=Q(6), func=AF.Exp), r=["q_e2"], w=["q_e2"])
    P.op("dve", lambda e: e.tensor_scalar(out=Q(6), in0=Q(6), scalar1=1.0, scalar2=None, op0=ALU.add), r=["q_e2"], w=["q_e2"])
    P.op("dve", lambda e: e.reciprocal(out=Q(6), in_=Q(6)), r=["q_e2"], w=["q_e2"])
    P.op("dve", lambda e: e.tensor_tensor(out=Q(6), in0=Q(6), in1=Q(3), op=ALU.mult), r=["q_e2", "q_sg"], w=["q_e2"])
    P.op("dve", lambda e: e.tensor_tensor(out=R3(6), in0=R3(6), in1=R3(5), op=ALU.mult), r=["r_ee", "r_sel2"], w=["r_ee"])
    P.op("dve", lambda e: e.tensor_tensor(out=R3(6), in0=R3(6), in1=bq(6), op=ALU.mult), r=["r_ee", "q_e2"], w=["r_ee"])
    P.op("dve", lambda e: e.tensor_tensor(out=comb[:].rearrange("p t (g e) -> p t g e", e=4), in0=R3(0).unsqueeze(3).to_broadcast([128, NOWN, 4, 4]),
                                          in1=R3(6).unsqueeze(2).to_broadcast([128, NOWN, 4, 4]), op=ALU.mult),
         r=["r_ohg", "r_ee"], w=[("comb", i) for i in range(NOWN)])

    d_h1 = dbg_out("h1", [128, NOWN * D])
    d_comb = dbg_out("comb", [128, NOWN * 16])
    if dbg:
        for i in range(NOWN):
            P.dma("sp", d_h1[:, i * D:(i + 1) * D], h1[:, i, :], r=[("h1", i)], key=("dbgo", 16, i))
        P.dma("sp", d_comb, comb[:].rearrange("p a b -> p (a b)"), r=[("comb", i) for i in range(NOWN)], key=("dbgo", 17))
    if stop_after == "B2":
        P.finalize()
        nc.in_names = in_names
        return nc

    o = TB + 32 * KB
    M_sg = [view(o, [128, 512], BF16, [("Msg", 0)]), view(o + KB, [128, 512], BF16, [("Msg", 1)])]; o += 2 * KB
    M_hid = [view(o, [128, 4, 512], BF16, [("Mhid", 0)]), view(o + 4 * KB, [128, 4, 512], BF16, [("Mhid", 1)])]; o += 8 * KB
    first_c = True
    HN2T_ALL = [("hn2T", i) for i in range(NOWN)]
    gcount = 0
    ycount = 0
    for ex in range(16):
        eb = ex % 2
        Wg_, Wu_, Wd_ = Wexp[eb]
        if ex == 0:
            P.alias(("Wg", 0), ["Wo"])
            P.alias(("Wu", 0), ["Wo"])
            P.alias(("Wd", 0), ["Wo", "Wown"])
        if ex == 1:
            cts = [("catT", i) for i in range(NOWN)]
            P.alias(("Wg", 1), cts)
            P.alias(("Wu", 1), cts)
            P.alias(("Wd", 1), cts)
        P.dma("pool", Wg_, wg_d[ex].rearrange("(c p) n -> p c n", p=128), w=[("Wg", eb)])
        P.dma("pool", Wu_, wu_d[ex].rearrange("(c p) n -> p c n", p=128), w=[("Wu", eb)])
        P.dma("pool", Wd_, wd_d[ex].rearrange("(c p) n -> p c n", p=128), w=[("Wd", eb)])
        for grp in range(4):
            hb_ = (ex * 4 + grp) % 2
            hid = M_hid[hb_]
            if first_c:
                pass
                pass
                P.alias(("Msg", 0), [("xo", 0), ("xo", 1)])
                P.alias(("Msg", 1), [("xo", 0), ("xo", 1)])
                first_c = False
            rk = [("hn2T", 4 * grp + t_) for t_ in range(4)]
            for fc in range(4):
                gb_ = 0 if gcount % 2 == 0 else 2
                sgb = gcount % 2
                gcount += 1
                for dc in range(8):
                    P.op("pe", lambda e, dc=dc, fc=fc, grp=grp, gb_=gb_, Wg_=Wg_: e.matmul(bank(gb_), lhsT=Wg_[:, dc, fc * 128:(fc + 1) * 128],
                                                                                         rhs=hn2T[:, dc, grp * 512:(grp + 1) * 512], start=(dc == 0), stop=(dc == 7)),
                         r=[("Wg", eb)] + rk, w=bk(gb_))
                for dc in range(8):
                    P.op("pe", lambda e, dc=dc, fc=fc, grp=grp, gb_=gb_, Wu_=Wu_: e.matmul(bank(gb_ + 1), lhsT=Wu_[:, dc, fc * 128:(fc + 1) * 128],
                                                                                         rhs=hn2T[:, dc, grp * 512:(grp + 1) * 512], start=(dc == 0), stop=(dc == 7)),
                         r=[("Wu", eb)] + rk, w=bk(gb_ + 1))
                P.op("act", lambda e, gb_=gb_, sgb=sgb: e.activation(out=M_sg[sgb], in_=bank(gb_), func=AF.Silu), r=bk(gb_), w=[("Msg", sgb)])
                P.op("dve", lambda e, gb_=gb_, sgb=sgb, fc=fc, hid=hid: e.tensor_tensor(out=hid[:, fc, :], in0=bank(gb_ + 1), in1=M_sg[sgb], op=ALU.mult),
                     r=bk(gb_ + 1) + [("Msg", sgb)], w=[("Mhid", hb_)])
            for tl in range(4):
                tile = 4 * grp + tl
                for half in range(2):
                    yb = 4 + ycount % 4
                    ycount += 1
                    for fc in range(4):
                        P.op("pe", lambda e, fc=fc, tl=tl, half=half, yb=yb, hid=hid, Wd_=Wd_: e.matmul(bank(yb), lhsT=hid[:, fc, tl * 128:(tl + 1) * 128],
                                                                                                      rhs=Wd_[:, fc, half * 512:(half + 1) * 512], start=(fc == 0), stop=(fc == 3)),
                             r=[("Mhid", hb_), ("Wd", eb)], w=bk(yb))
                    P.op("dve", lambda e, tile=tile, half=half, yb=yb, ex=ex: e.scalar_tensor_tensor(
                        out=h1[:, tile, half * 512:(half + 1) * 512], in0=bank(yb), scalar=comb[:, tile, ex:ex + 1],
                        in1=h1[:, tile, half * 512:(half + 1) * 512], op0=ALU.mult, op1=ALU.add),
                        r=bk(yb) + [("comb", tile), ("h1", tile)], w=[("h1", tile)])

    d_h2 = dbg_out("h2", [128, NOWN * D])
    if dbg:
        for i in range(NOWN):
            P.dma("sp", d_h2[:, i * D:(i + 1) * D], h1[:, i, :], r=[("h1", i)], key=("dbgo", 18, i))

    P.dma("pool", Wpg, wpg_d.rearrange("(c p) n -> p c n", p=128), w=["Wpg"])
    P.dma("pool", Wpp, wpp_d.rearrange("(c p) n -> p c n", p=128), w=["Wpp"])
    o = TB + 20 * KB
    D_pT = [view(o, [128, 2, 128], BF16, [("pT", 0)]), view(o + 512, [128, 2, 128], BF16, [("pT", 1)])]; o += KB
    D_gple = view(o, [128, D], F32, ["gple"]); o += 4 * KB
    D_gfin = view(o, [128, D], F32, ["gfin"]); o += 4 * KB
    D_hn3 = [view(o, [128, D], BF16, [("hn3", 0)]), view(o + 2 * KB, [128, D], BF16, [("hn3", 1)])]; o += 4 * KB
    D_hn3T = [view(o, [128, 8, 128], BF16, [("hn3T", 0)]), view(o + 2 * KB, [128, 8, 128], BF16, [("hn3T", 1)])]; o += 4 * KB
    D_sgt = [view(o, [128, D], F32, [("sgt", 0, 0), ("sgt", 0, 1)]), view(o + 4 * KB, [128, D], F32, [("sgt", 1, 0), ("sgt", 1, 1)])]; o += 8 * KB
    D_junk = view(o, [128, D], BF16, ["Djunk"]); o += 2 * KB
    D_out = [view(o, [128, D], F32, [("Dout", 0)]), view(o + 4 * KB, [128, D], F32, [("Dout", 1)])]; o += 8 * KB
    D_s = view(o, [128, 8], F32, [("Drs", 0), ("Drs", 1), ("Drs2", 0), ("Drs2", 1)]); o += 32
    assert o <= ARENA
    P.dma("sp", D_gple, gple_d, w=["gple"])
    P.dma("sp", D_gfin, gfin_d, w=["gfin"])

    D_q = view(o, [128, 4, NOWN], F32, ["rstd3", "rstd4"] + [("ssq3", i_) for i_ in range(NOWN)] + [("ssq4", i_) for i_ in range(NOWN)]); o += 256
    assert o <= ARENA
    for i in range(NOWN):
        P.op("act", lambda e, i=i: e.activation(out=D_junk, in_=h1[:, i, :], func=AF.Square, accum_out=D_q[:, 0, i:i + 1]), r=[("h1", i)], w=["Djunk", ("ssq3", i)])
    P.op("act", lambda e: e.activation(out=D_q[:, 1, :], in_=D_q[:, 0, :], func=AF.Sqrt, scale=1.0 / D, bias=EPS), r=[("ssq3", i) for i in range(NOWN)], w=["rstd3"])
    P.op("dve", lambda e: e.reciprocal(out=D_q[:, 1, :], in_=D_q[:, 1, :]), r=["rstd3"], w=["rstd3"])

    def D_front(i):
        ib = i % 2
        gbk = 0 if ib == 0 else 4
        trb = 6 + ib
        P.dma("pool", D_pT[ib], pTo_d[:, i * 128:(i + 1) * 128].rearrange("(c p) t -> p c t", p=128), w=[("pT", ib)])
        P.op("dve", lambda e: e.scalar_tensor_tensor(out=D_hn3[ib], in0=h1[:, i, :], scalar=D_q[:, 1, i:i + 1], in1=D_gple, op0=ALU.mult, op1=ALU.mult),
             r=[("h1", i), "rstd3", "gple"], w=[("hn3", ib)])
        tc = bankb(trb).rearrange("p (k t) -> p k t", t=128)
        for kc in range(8):
            P.op("pe", lambda e, kc=kc: e.transpose(tc[:, kc, :], D_hn3[ib][:, kc * 128:(kc + 1) * 128], identb[:]), r=[("hn3", ib), "identb"], w=bk(trb))
        P.op("act", lambda e: e.activation(out=D_hn3T[ib], in_=tc, func=AF.Copy), r=bk(trb), w=[("hn3T", ib)])
        for half in range(2):
            for dc in range(8):
                P.op("pe", lambda e, dc=dc, half=half: e.matmul(bank(gbk + half), lhsT=D_hn3T[ib][:, dc, :], rhs=Wpg[:, dc, half * 512:(half + 1) * 512],
                                                              start=(dc == 0), stop=(dc == 7)), r=[("hn3T", ib), "Wpg"], w=bk(gbk + half))

    def D_back(i):
        ib = i % 2
        gbk = 0 if ib == 0 else 4
        sg = D_sgt[ib]
        for half in range(2):
            for kc in range(2):
                P.op("pe", lambda e, kc=kc, half=half: e.matmul(bank(2 + half), lhsT=D_pT[ib][:, kc, :], rhs=Wpp[:, kc, half * 512:(half + 1) * 512],
                                                              start=(kc == 0), stop=(kc == 1)), r=[("pT", ib), "Wpp"], w=bk(2 + half))
        for half in range(2):
            P.op("act", lambda e, half=half: e.activation(out=sg[:, half * 512:(half + 1) * 512], in_=bank(gbk + half), func=AF.Sigmoid), r=bk(gbk + half), w=[("sgt", ib, half)])
            P.op("dve", lambda e, half=half: e.tensor_tensor(out=sg[:, half * 512:(half + 1) * 512], in0=bank(2 + half), in1=sg[:, half * 512:(half + 1) * 512], op=ALU.mult),
                 r=bk(2 + half) + [("sgt", ib, half)], w=[("sgt", ib, half)])
        P.op("pool", lambda e: e.tensor_tensor(out=h1[:, i, :], in0=h1[:, i, :], in1=sg, op=ALU.add), r=[("h1", i), ("sgt", ib, 0), ("sgt", ib, 1)], w=[("h1", i)])
        P.op("act", lambda e: e.activation(out=D_junk, in_=h1[:, i, :], func=AF.Square, accum_out=D_q[:, 2, i:i + 1]), r=[("h1", i)], w=["Djunk", ("ssq4", i)])

    for i in range(NOWN):
        D_front(i)
        if i > 0:
            D_back(i - 1)
    D_back(NOWN - 1)
    P.op("act", lambda e: e.activation(out=D_q[:, 3, :], in_=D_q[:, 2, :], func=AF.Sqrt, scale=1.0 / D, bias=EPS), r=[("ssq4", i) for i in range(NOWN)], w=["rstd4"])
    P.op("dve", lambda e: e.reciprocal(out=D_q[:, 3, :], in_=D_q[:, 3, :]), r=["rstd4"], w=["rstd4"])
    for i in range(NOWN):
        ib = i % 2
        P.op("dve", lambda e, i=i, ib=ib: e.scalar_tensor_tensor(out=D_out[ib], in0=h1[:, i, :], scalar=D_q[:, 3, i:i + 1], in1=D_gfin, op0=ALU.mult, op1=ALU.mult),
             r=[("h1", i), "rstd4", "gfin"], w=[("Dout", ib)])
        P.dma("sp", out_d[i], D_out[ib], r=[("Dout", ib)], key=("outq", ib))

    P.finalize()
    nc.in_names = in_names
    return nc


def host_inputs(inp):
    f = np.float32
    x = np.asarray(inp["x"], f)
    p = np.asarray(inp["p"], f)[0]
    w_in = np.asarray(inp["w_in"], f)[0]
    HD = 64
    OFF_Q, OFF_KV, OFF_GATE = 1024, 1536, 2304
    kvc = [w_in[:, OFF_KV + j * 128: OFF_KV + (j + 1) * 128] for j in range(6)]
    wkv = np.concatenate([kvc[0], kvc[2], kvc[4], kvc[1], kvc[3], kvc[5]], axis=1)
    wq = w_in[:, OFF_Q:OFF_KV].reshape(D, 8, HD)
    wq_r = np.concatenate([np.concatenate([wq[:, j], wq[:, 4 + j]], axis=1) for j in range(4)], axis=1)
    wown = np.concatenate([w_in[:, 0:1024], wq_r, w_in[:, OFF_GATE:OFF_GATE + 24]], axis=1)
    shared = {
        "wkv": np.ascontiguousarray(wkv), "wown": np.ascontiguousarray(wown),
        "gmix": np.ascontiguousarray(np.asarray(inp["norm_mix"], f)[0].reshape(8, 128).T),
        "wsT": np.ascontiguousarray(np.asarray(inp["gmlp_w_s"], f)[0].transpose(2, 0, 1).reshape(128, 8 * 128)),
        "tri": np.triu(np.ones((128, 128), f)),
        "bsT": np.ascontiguousarray(np.asarray(inp["gmlp_b_s"], f)[0].T),
        "gv": np.ascontiguousarray(np.broadcast_to(np.asarray(inp["gmlp_v_norm"], f)[0][None, :], (128, 512))),
        "ga": np.ascontiguousarray(np.broadcast_to(np.asarray(inp["out_norm_a"], f)[0][None, :], (128, 512))),
        "gb": np.ascontiguousarray(np.broadcast_to(np.asarray(inp["out_norm_b"], f)[0][None, :], (128, 512))),
        "gmoe": np.ascontiguousarray(np.broadcast_to(np.asarray(inp["norm_moe"], f)[0][None, :], (128, D))),
        "gple": np.ascontiguousarray(np.broadcast_to(np.asarray(inp["norm_ple"], f)[0][None, :], (128, D))),
        "gfin": np.ascontiguousarray(np.broadcast_to(np.asarray(inp["norm_final"], f)[None, :], (128, D))),
        "w1k": np.ascontiguousarray(np.asarray(inp["cmp_w1_k"], f)[0].reshape(32, 64, 256).transpose(1, 0, 2).reshape(64, 32 * 256)),
        "w1v": np.ascontiguousarray(np.asarray(inp["cmp_w1_v"], f)[0].reshape(32, 64, 256).transpose(1, 0, 2).reshape(64, 32 * 256)),
        "peTk": np.ascontiguousarray(np.asarray(inp["cmp_pe_k"], f)[0].T),
        "peTv": np.ascontiguousarray(np.asarray(inp["cmp_pe_v"], f)[0].T),
        "w2k": np.ascontiguousarray(np.asarray(inp["cmp_w2_k"], f)[0]),
        "w2v": np.ascontiguousarray(np.asarray(inp["cmp_w2_v"], f)[0]),
        "wo": np.ascontiguousarray(np.asarray(inp["w_o"], f)[0]),
        "rcat": np.ascontiguousarray(np.concatenate([np.asarray(inp["router_group"], f)[0], np.asarray(inp["router_expert"], f)[0]], axis=1)),
        "moe_wg": np.ascontiguousarray(np.asarray(inp["moe_w_gate"], f)[0]),
        "moe_wu": np.ascontiguousarray(np.asarray(inp["moe_w_up"], f)[0]),
        "moe_wd": np.ascontiguousarray(np.asarray(inp["moe_w_down"], f)[0]),
        "wpp": np.ascontiguousarray(np.asarray(inp["w_ple_proj"], f)[0]),
        "wpg": np.ascontiguousarray(np.asarray(inp["w_ple_gate"], f)[0]),
        "identf": np.eye(128, dtype=f),
    }
    half = 32
    inv = (1.0 / (np.float32(10000.0) ** (np.arange(half, dtype=f) / np.float32(half)))).astype(f)
    ang = (np.arange(T, dtype=f)[:, None] * inv[None, :]).astype(f)
    cs = np.concatenate([np.cos(ang), np.sin(ang)], axis=1).astype(f)
    shared["cs"] = cs
    ew = np.zeros((128, 64, 128), f)
    for kt in range(64):
        ew[2 * kt, kt, 0:64] = 1.0
        ew[2 * kt + 1, kt, 64:128] = 1.0
    shared["ewide"] = ew.reshape(128, 64 * 128)
    cidx = np.arange(512)
    sidx = np.arange(128)
    ovl = ((cidx[:, None] * 16 < sidx[None, :] * 64 + 64) & (cidx[:, None] * 16 + 32 > sidx[None, :] * 64)).astype(f)
    ovl[511, :] = 0.0
    shared["ovl"] = ovl
    kk = np.arange(128)[:, None]
    tt = np.arange(128)[None, :]
    tri_m = (kk <= tt).astype(f)
    ntri_m = (kk > tt).astype(f)
    onesm = np.ones((128, 128), f)
    zerom = np.zeros((128, 128), f)

    maps = []
    for core in range(8):
        b, c = core // 4, core % 4
        own_tiles = [4 * i + c for i in range(NOWN)]
        tok = np.concatenate([np.arange(128 * qt, 128 * qt + 128) for qt in own_tiles])
        m = dict(shared)
        m["xT"] = np.ascontiguousarray(x[b].T)
        m["xo"] = np.ascontiguousarray(x[b][tok].reshape(NOWN, 128, D))
        m["xTo"] = np.ascontiguousarray(x[b][tok].reshape(NOWN, 128, D).transpose(0, 2, 1))
        m["pTo"] = np.ascontiguousarray(p[b][tok].T)
        m["cso"] = np.ascontiguousarray(cs[tok])
        dm = []
        for r in range(4):
            dm.append(onesm if r < c else (tri_m if r == c else zerom))
        for r in range(8):
            dlt = r - 4 - c
            dm.append(ntri_m if dlt == -4 else (onesm if -3 <= dlt <= -1 else (tri_m if dlt == 0 else zerom)))
        m["dmwm"] = np.ascontiguousarray(((np.stack(dm, axis=1) - 1.0) * 30000.0).astype(f).reshape(128, 12 * 128))
        mC = np.zeros((NOWN, 128, 2, 128), f)
        m12 = np.zeros((NOWN, 128, 256), f)
        for i, qt in enumerate(own_tiles):
            n_ct = (32 * i + 30) // 128 + 1
            t = 128 * qt + np.arange(128)
            for k in range(2):
                ct = n_ct - 2 + k
                if ct < 0:
                    continue
                cc = ct * 128 + np.arange(128)
                mC[i, :, k, :] = ((16 * cc[:, None] + 31) <= t[None, :]).astype(f)
            cur = (t // 64)[:, None]
            s = np.arange(128)[None, :]
            forced = (s == 0) | (s == cur) | (s == cur - 1)
            valid = (s * 64) <= t[:, None]
            m12[i, :, 0:128] = np.where(forced, 0.0, np.where(valid, 1.0, 0.0))
            m12[i, :, 128:256] = np.where(forced, FORCE, np.where(valid, 0.0, -FORCE))
        m["mC"] = np.ascontiguousarray(((mC - 1.0) * 30000.0).astype(f).reshape(NOWN, 128, 256))
        m["m12"] = m12
        maps.append(m)
    return maps


_NC_CACHE = {}


def kernel(**inputs):
    maps = host_inputs(inputs)
    if "nc" not in _NC_CACHE:
        _NC_CACHE["nc"] = build_program()
    nc = _NC_CACHE["nc"]
    res = run_bass_kernel_spmd(nc, maps, core_ids=list(range(8)))
    out = np.zeros((2, T, D), np.float32)
    for core in range(8):
        b, c = core // 4, core % 4
        o = np.asarray(res.results[core]["out"], np.float32)
        for i in range(NOWN):
            qt = 4 * i + c
            out[b, 128 * qt:128 * (qt + 1), :] = o[i]
    return out
```

```python
from contextlib import ExitStack
import types
import numpy as np
import concourse.bass as bass
import concourse.mybir as mybir
from concourse.bass_utils import run_bass_kernel_spmd

F32 = mybir.dt.float32
BF16 = mybir.dt.bfloat16
AF = mybir.ActivationFunctionType
ALU = mybir.AluOpType
AX = mybir.AxisListType

T = 8192
D = 1024
NT = 64
NOWN = 16
EPS = 1e-6
FORCE = 1e6
ENG = ("pe", "act", "dve", "pool", "sp")
SKIP = set()


def _freeze(fn):
    if fn.__closure__ is None:
        return fn
    cells = []
    for c in fn.__closure__:
        try:
            cells.append(types.CellType(c.cell_contents))
        except ValueError:
            cells.append(c)
    return types.FunctionType(fn.__code__, fn.__globals__, fn.__name__, fn.__defaults__, tuple(cells))


class Prog:
    def __init__(self, nc):
        self.nc = nc
        self.ops = []
        self.last_w = {}
        self.readers = {}
        self.stack = ExitStack()
        self.group_keys = set()
        self.pending_alias = {}
        self.ranges = {}
        self._ovl_cache = {}

    def sb(self, name, shape, dt):
        return self.stack.enter_context(self.nc.sbuf_tensor(name, list(shape), dt))

    def ps(self, name, shape, dt):
        return self.stack.enter_context(self.nc.psum_tensor(name, list(shape), dt))

    def reg(self, keys, lo, hi):
        for k in keys:
            if k in self.ranges:
                a, b = self.ranges[k]
                lo2, hi2 = min(a, lo), max(b, hi)
            else:
                lo2, hi2 = lo, hi
            self.ranges[k] = (lo2, hi2)
        self._ovl_cache = {}

    def overlaps(self, k):
        if k not in self.ranges:
            return ()
        if k not in self._ovl_cache:
            lo, hi = self.ranges[k]
            self._ovl_cache[k] = [k2 for k2, (a, b) in self.ranges.items() if k2 != k and a < hi and lo < b]
        return self._ovl_cache[k]

    def alias(self, newkey, oldkeys):
        self.pending_alias.setdefault(newkey, []).extend(oldkeys)

    @staticmethod
    def _is_psum(k):
        return isinstance(k, str) and len(k) == 2 and k[0] == "B" and k[1].isdigit()

    def op(self, eng, fn, r=(), w=(), dma_key=None):
        i = len(self.ops)
        deps = set()
        w = list(w)
        r = list(r)
        xw = [k for k in r if self._is_psum(k) and k not in w]
        extra = []
        for k in w:
            if k in self.pending_alias:
                extra.extend(self.pending_alias.pop(k))
            extra.extend(self.overlaps(k))
        for k in r:
            if k in self.last_w:
                deps.add(self.last_w[k])
        for k in w + extra + xw:
            if k in self.last_w:
                deps.add(self.last_w[k])
            for j in self.readers.get(k, ()):
                deps.add(j)
        deps.discard(i)
        self.ops.append(dict(eng=eng, fn=_freeze(fn), deps=deps, dma=dma_key, r=tuple(r), w=tuple(w)))
        for k in r:
            self.readers.setdefault(k, []).append(i)
        for k in w:
            self.last_w[k] = i
            self.readers[k] = []
        for k in xw:
            self.last_w[k] = i
            self.readers[k] = []
        return i

    def dma(self, eng, out, in_, r=(), w=(), key=None, group=False, **kw):
        if key is None:
            key = w[0] if len(w) else r[0]
        if group:
            self.group_keys.add(key)
        return self.op(eng, lambda e: e.dma_start(out=out, in_=in_, **kw), r=r, w=w, dma_key=key)

    def finalize(self, final_wait_eng="sp"):
        nc = self.nc
        ops = self.ops
        n = len(ops)
        needed = [False] * n
        for i, o in enumerate(ops):
            keep = set()
            for d in o["deps"]:
                od = ops[d]
                if od["dma"] is None and o["dma"] is None and od["eng"] == o["eng"]:
                    if o["eng"] == "pe":
                        continue
                    if not (set(od["w"]) & (set(o["r"]) | set(o["w"]))):
                        continue
                keep.add(d)
            o["deps"] = keep
            for d in keep:
                needed[d] = True
        final_dma = [i for i, o in enumerate(ops) if o["dma"] is not None and not needed[i]]
        for i in final_dma:
            needed[i] = True
        sems = {}
        cnt = {}
        names = {}

        def sem_name(key):
            if key not in names:
                names[key] = "d%d" % len(names)
            return names[key]

        for i, o in enumerate(ops):
            if not needed[i]:
                o["sig"] = None
                continue
            if o["dma"] is not None:
                sname = sem_name(o["dma"])
                inc = 16
            else:
                sname = "e_" + o["eng"]
                inc = 1
            cnt[sname] = cnt.get(sname, 0) + inc
            o["sig"] = (sname, inc, cnt[sname])
            if sname not in sems:
                sems[sname] = self.stack.enter_context(nc.semaphore(sname))
        gfinal = {sem_name(k): cnt.get(sem_name(k), 0) for k in self.group_keys}
        self.n_sems = len(sems)
        plan = {e: [] for e in ENG}
        waited = {e: {} for e in ENG}
        for i, o in enumerate(ops):
            e = o["eng"]
            waits = {}
            for d in o["deps"]:
                sname, inc, val = ops[d]["sig"]
                if sname in gfinal:
                    val = gfinal[sname]
                if waited[e].get(sname, 0) >= val:
                    continue
                waits[sname] = max(waits.get(sname, 0), val)
            for sname, val in waits.items():
                waited[e][sname] = val
            plan[e].append((o, waits))
        final_waits = {}
        for i in final_dma:
            sname, inc, val = ops[i]["sig"]
            final_waits[sname] = max(final_waits.get(sname, 0), val)
        blk = self.stack.enter_context(nc.Block())

        def emit(engobj, ename):
            for o, waits in plan[ename]:
                for sname, val in waits.items():
                    engobj.wait_ge(sems[sname], val)
                ins = o["fn"](engobj)
                if o["sig"] is not None:
                    ins.then_inc(sems[o["sig"][0]], o["sig"][1])
            if ename == final_wait_eng:
                for sname, val in final_waits.items():
                    engobj.wait_ge(sems[sname], val)

        @blk.tensor
        def _(e):
            emit(e, "pe")

        @blk.scalar
        def _(e):
            emit(e, "act")

        @blk.vector
        def _(e):
            emit(e, "dve")

        @blk.gpsimd
        def _(e):
            emit(e, "pool")

        @blk.sync
        def _(e):
            emit(e, "sp")

        self.stack.close()
        return nc


def build_program(dbg=False, stop_after=None):
    nc = bass.Bass("TRN2", target_bir_lowering=False)
    P = Prog(nc)

    in_names = []
    late = stop_after in (None, "B2")

    def din(name, shape, big=False):
        if big and not late:
            shape = [1] * len(shape)
        in_names.append(name)
        return nc.dram_tensor(name, list(shape), F32, kind="ExternalInput").ap()

    def dout(name, shape):
        return nc.dram_tensor(name, list(shape), F32, kind="ExternalOutput").ap()

    xT_d = din("xT", [D, T])
    xTo_d = din("xTo", [NOWN, D, 128])
    xo_d = din("xo", [NOWN, 128, D], big=True)
    pTo_d = din("pTo", [256, NOWN * 128], big=True)
    cs_d = din("cs", [T, 64])
    cso_d = din("cso", [NOWN * 128, 64])
    wkv_d = din("wkv", [D, 768])
    wown_d = din("wown", [D, 1560])
    gmix_d = din("gmix", [128, 8])
    wsT_d = din("wsT", [128, 8 * 128])
    tri_d = din("tri", [128, 128])
    bsT_d = din("bsT", [128, 8])
    gv_d = din("gv", [128, 512])
    ga_d = din("ga", [128, 512])
    gb_d = din("gb", [128, 512])
    gmoe_d = din("gmoe", [128, D], big=True)
    gple_d = din("gple", [128, D], big=True)
    gfin_d = din("gfin", [128, D], big=True)
    w1_d = [din("w1k", [64, 32 * 256]), din("w1v", [64, 32 * 256])]
    peT_d = [din("peTk", [64, 32]), din("peTv", [64, 32])]
    w2_d = [din("w2k", [256, 64]), din("w2v", [256, 64])]
    wo_d = din("wo", [D, D], big=True)
    rcat_d = din("rcat", [D, 20])
    wg_d = din("moe_wg", [16, D, 512], big=True)
    wu_d = din("moe_wu", [16, D, 512], big=True)
    wd_d = din("moe_wd", [16, 512, D], big=True)
    wpp_d = din("wpp", [256, D], big=True)
    wpg_d = din("wpg", [D, D], big=True)
    identf_d = din("identf", [128, 128])
    ewide_d = din("ewide", [128, 64 * 128])
    ovl_d = din("ovl", [512, 128])
    mC_d = din("mC", [NOWN, 128, 2 * 128])
    m12_d = din("m12", [NOWN, 128, 256])
    dmwm_d = din("dmwm", [128, 12 * 128])
    out_d = dout("out", [NOWN, 128, D])
    dbg_d = {}

    def dbg_out(name, shape):
        if dbg:
            dbg_d[name] = dout("dbg_" + name, shape)
        return dbg_d.get(name)

    KB = 1024
    ARENA = 192 * KB
    arena = P.sb("arena", [128, ARENA // 2], BF16)

    def view(off, shape, dt, keys=None):
        esz = 2 if dt == BF16 else 4
        nel = int(np.prod(shape[1:]))
        assert off % 4 == 0 and off + nel * esz <= ARENA, (off, shape)
        if keys is not None:
            P.reg(keys, off, off + nel * esz)
        a = arena[:, off // 2: off // 2 + nel * esz // 2]
        if dt != BF16:
            a = a.bitcast(dt)
        if len(shape) == 2:
            return a
        nm = " ".join("d%d" % i for i in range(1, len(shape)))
        kw = {"d%d" % i: shape[i] for i in range(2, len(shape))}
        return a.rearrange("p (%s) -> p %s" % (nm, nm), **kw)

    KVc = view(0, [128, 2, T], BF16, ["KVc"])
    Wown = view(0, [128, 8, 1560], BF16, ["Wown"])
    Wo = view(0, [128, 8, D], BF16, ["Wo"])
    Wkv = view(130 * KB + 48 * KB, [128, 8, 768], BF16, ["Wkv"])
    W1 = [view(32 * KB, [128, 32, 256], BF16, [("W1", 0)]), view(48 * KB, [128, 32, 256], BF16, [("W1", 1)])]
    catT = view(32 * KB, [128, 8, NOWN * 128], BF16, [("catT", i_) for i_ in range(NOWN)])
    KVs = view(64 * KB, [128, 2, T], BF16, ["KVs"])
    VTM_B = 64 * 2 * 65 * 2
    Vtm = [view(96 * KB, [128, 64, 2, 65], BF16, ["Vtm", "Vtm_one0"]), view(96 * KB + VTM_B, [128, 64, 2, 65], BF16, ["Vtm", "Vtm_one1"])]
    h1 = view(64 * KB, [128, NOWN, D], F32)
    for i_ in range(NOWN):
        P.reg([("h1", i_)], 64 * KB + i_ * 4096, 64 * KB + (i_ + 1) * 4096)
    h1 = h1
    TB = 130 * KB
    hn2T = view(130 * KB, [128, 8, NOWN * 128], BF16, [("hn2T", i_) for i_ in range(NOWN)])
    Wexp = [(view(0, [128, 8, 512], BF16, [("Wg", 0)]), view(8 * KB, [128, 8, 512], BF16, [("Wu", 0)]), view(16 * KB, [128, 4, D], BF16, [("Wd", 0)])),
            (view(32 * KB, [128, 8, 512], BF16, [("Wg", 1)]), view(40 * KB, [128, 8, 512], BF16, [("Wu", 1)]), view(48 * KB, [128, 4, D], BF16, [("Wd", 1)]))]
    Wpg = view(130 * KB, [128, 8, D], BF16, ["Wpg"])
    Wpp = view(146 * KB, [128, 2, D], BF16, ["Wpp"])

    identb = P.sb("identb", [128, 128], BF16)
    identf = P.sb("identf_s", [128, 128], F32)
    ones = P.sb("ones", [128, 16], BF16)
    trib = P.sb("trib", [128, 128], BF16)
    wsT = P.sb("wsT_s", [128, 8, 128], BF16)
    bsT = P.sb("bsT_s", [128, 8], F32)
    gmix = P.sb("gmix_s", [128, 8], F32)
    gv = P.sb("gv_s", [128, 512], F32)
    ga = P.sb("ga_s", [128, 512], F32)
    gb = P.sb("gb_s", [128, 512], F32)
    kcT = P.sb("kcT", [128, 512], BF16)
    vcx = P.sb("vcx", [128, 4, 2, 194], BF16)
    w2 = [P.sb("w2k_s", [128, 2, 64], BF16), P.sb("w2v_s", [128, 2, 64], BF16)]
    peT = [P.sb("peTk_s", [64, 32], BF16), P.sb("peTv_s", [64, 32], BF16)]
    cbias = P.sb("cbias", [128, 4], F32)
    rcat = P.sb("rcat_s", [128, 8, 20], F32)
    comb = P.sb("comb", [128, NOWN, 16], F32)
    pad8 = P.sb("pad8", [128, 8], F32)

    PS = P.ps("PS", [128, 4096], F32)

    def bank(k, lo=0, hi=512):
        return PS[:, k * 512 + lo: k * 512 + hi]

    def bankb(k):
        return PS[:, k * 512:(k + 1) * 512].bitcast(BF16)

    def bk(k, slots=None):
        return ["B%d" % k]

    def bc_mid(ap2d, n):
        return ap2d.unsqueeze(1).to_broadcast([128, n, ap2d.shape[-1]])

    def bc_last(ap2d, n):
        return ap2d.unsqueeze(2).to_broadcast([128, ap2d.shape[-1], n])

    def cload(eng, dst, src, key):
        P.dma(eng, dst, src, w=[key], key="const_" + eng, group=True)

    cload("pool", identb[:], identf_d, "identb")
    cload("sp", identf[:], identf_d, "identf")
    cload("pool", trib[:], tri_d, "trib")
    cload("pool", wsT[:], wsT_d.rearrange("p (g t) -> p g t", t=128), "wsT")
    cload("sp", bsT[:], bsT_d, "bsT")
    cload("sp", gmix[:], gmix_d, "gmix")
    cload("sp", gv[:], gv_d, "gv")
    cload("sp", ga[:], ga_d, "ga")
    cload("sp", gb[:], gb_d, "gb")
    for kv in range(2):
        cload("pool", w2[kv][:], w2_d[kv].rearrange("(h p) d -> p h d", p=128), "w2%d" % kv)
        cload("pool", peT[kv][:], peT_d[kv], "peT%d" % kv)
    cload("sp", rcat[:], rcat_d.rearrange("(c p) n -> p c n", p=128), "rcat")
    P.op("pool", lambda e: e.memset(ones[:], 1.0), w=["ones"])
    P.op("pool", lambda e: e.memset(pad8[:], -1e30), w=["pad8"])
    P.op("pool", lambda e: e.memset(vcx[:].rearrange("p a g d -> p (a g d)"), 1.0), w=["vcx_one"])
    P.op("pool", lambda e: e.memset(Vtm[0].rearrange("p a g d -> p (a g d)"), 1.0), w=["Vtm_one0"])
    P.op("pool", lambda e: e.memset(Vtm[1].rearrange("p a g d -> p (a g d)"), 1.0), w=["Vtm_one1"])
    for g in range(2):
        for ct in range(4):
            P.dma("pool", vcx[:, ct, g, 66:194], ovl_d[ct * 128:(ct + 1) * 128, :], r=["vcx_one"], w=["vcx_ovl"], key="vcx_ovl")
    if "wst" not in SKIP:
        P.op("dve", lambda e: e.tensor_tensor(out=wsT[:], in0=wsT[:], in1=bc_mid(trib[:, :], 8), op=ALU.mult),
             r=["wsT", "trib"], w=["wsT"])

    P.dma("pool", Wkv, wkv_d.rearrange("(c p) n -> p c n", p=128), w=["Wkv"])
    for dc in range(8 if "wkvs" not in SKIP else 0):
        P.op("dve", lambda e, dc=dc: e.tensor_scalar(out=Wkv[:, dc, :], in0=Wkv[:, dc, :], scalar1=gmix[:, dc:dc + 1],
                                                      scalar2=None, op0=ALU.mult), r=["Wkv", "gmix"], w=["Wkv"])

    if stop_after == "const":
        P.finalize()
        nc.in_names = in_names
        return nc

    def rstd_from_ssq(ssq_ap, n, dst, rkeys, wkey):
        P.op("act", lambda e: e.activation(out=dst, in_=ssq_ap, func=AF.Sqrt, scale=1.0 / n, bias=EPS), r=rkeys, w=[wkey])
        P.op("dve", lambda e: e.reciprocal(out=dst, in_=dst), r=[wkey], w=[wkey])

    def rope_tm(src_ps, nblk, csr, dst_bf, tmp, rkeys, tkey, wkey):
        s4 = src_ps.rearrange("p (b h d) -> p b h d", h=2, d=32)
        d4 = dst_bf.rearrange("p (b h d) -> p b h d", h=2, d=32)
        cr = csr[:, 0:32].unsqueeze(1).to_broadcast([128, nblk, 32])
        sr = csr[:, 32:64].unsqueeze(1).to_broadcast([128, nblk, 32])
        tv = [tmp[:, k, :].rearrange("p (b d) -> p b d", d=32) for k in range(4)]
        P.op("dve", lambda e: e.tensor_tensor(out=tv[0], in0=s4[:, :, 0, :], in1=cr, op=ALU.mult), r=rkeys, w=[tkey + "0"])
        P.op("dve", lambda e: e.tensor_tensor(out=tv[1], in0=s4[:, :, 1, :], in1=sr, op=ALU.mult), r=rkeys, w=[tkey + "1"])
        P.op("dve", lambda e: e.tensor_tensor(out=tv[2], in0=s4[:, :, 0, :], in1=sr, op=ALU.mult), r=rkeys, w=[tkey + "2"])
        P.op("dve", lambda e: e.tensor_tensor(out=tv[3], in0=s4[:, :, 1, :], in1=cr, op=ALU.mult), r=rkeys, w=[tkey + "3"])
        P.op("pool", lambda e: e.tensor_tensor(out=d4[:, :, 0, :], in0=tv[0], in1=tv[1], op=ALU.subtract),
             r=[tkey + "0", tkey + "1"], w=[wkey + "a"])
        P.op("pool", lambda e: e.tensor_tensor(out=d4[:, :, 1, :], in0=tv[2], in1=tv[3], op=ALU.add),
             r=[tkey + "2", tkey + "3"], w=[wkey + "b"])

    A_xTb = [view(TB + 0, [128, 8, 256], BF16, [("xTb", 0)]), view(TB + 4 * KB, [128, 8, 256], BF16, [("xTb", 1)])]
    A_xf = [view(TB + 32 * KB, [128, 8, 256], F32, [("xf", 0)]), view(TB + 40 * KB, [128, 8, 256], F32, [("xf", 1)])]
    A_cs = [view(TB + 16 * KB, [128, 2, 64], F32, [("Acs", 0)]), view(TB + 17 * KB, [128, 2, 64], F32, [("Acs", 1)])]
    A_sq = [view(TB + 18 * KB, [128, 8, 128], BF16, [("Asq", 0)]), view(TB + 20 * KB, [128, 8, 128], BF16, [("Asq", 1)])]
    A_tmp = [view(TB + 22 * KB, [128, 4, 192], F32, ["Atmp0%d" % k_ for k_ in range(4)]), view(TB + 25 * KB, [128, 4, 192], F32, ["Atmp1%d" % k_ for k_ in range(4)])]
    A_ktm = [view(TB + 28 * KB, [128, 512], BF16, ["Aktm0a", "Aktm0b", "Aktm0v"]), view(TB + 29 * KB, [128, 512], BF16, ["Aktm1a", "Aktm1b", "Aktm1v"])]
    A_csr = [view(TB + 30 * KB, [128, 64], F32, [("Acsr", 0)]), view(TB + 30 * KB + 256, [128, 64], F32, [("Acsr", 1)])]
    A_rs = view(TB + 31 * KB, [128, 8], F32, [("Ars", 0), ("Ars", 1)])
    A_u = [view(TB + 8 * KB, [128, 384], F32, [("Au", 0, 0), ("Au", 0, 1)]), view(TB + 10 * KB, [128, 384], F32, [("Au", 1, 0), ("Au", 1, 1)])]

    for kv in range(2):
        for half in range(2):
            P.dma("pool", W1[kv][half * 64:(half + 1) * 64], w1_d[kv].rearrange("p (l h) -> p l h", h=256), w=[("W1", kv)])
    n_chunks = 32
    TPC = 2

    def A_front(ck, tl):
        cb = ck % 2
        tile = ck * TPC + tl
        tb = tile % 2
        xs = A_xTb[cb][:, :, tl * 128:(tl + 1) * 128]
        P.op("act", lambda e: e.activation(out=A_sq[tb], in_=xs, func=AF.Square), r=[("xTb", cb)], w=[("Asq", tb)])
        zb = 0 if tb == 0 else 2
        for dc in range(8):
            P.op("pe", lambda e, dc=dc: e.matmul(bank(4 + tb, 0, 1), lhsT=A_sq[tb][:, dc, :], rhs=ones[:, 0:1],
                                                 start=(dc == 0), stop=(dc == 7)), r=[("Asq", tb), "ones"], w=bk(4 + tb))
        for dc in range(8):
            P.op("pe", lambda e, dc=dc: e.matmul(bank(zb), lhsT=xs[:, dc, :], rhs=Wkv[:, dc, 0:512],
                                                 start=(dc == 0), stop=(dc == 7)), r=[("xTb", cb), "Wkv"], w=bk(zb))
        for dc in range(8):
            P.op("pe", lambda e, dc=dc: e.matmul(bank(zb + 1, 0, 256), lhsT=xs[:, dc, :], rhs=Wkv[:, dc, 512:768],
                                                 start=(dc == 0), stop=(dc == 7)), r=[("xTb", cb), "Wkv"], w=bk(zb + 1))

    def A_back(ck, tl):
        cb = ck % 2
        tile = ck * TPC + tl
        tb = tile % 2
        zb = 0 if tb == 0 else 2
        rs = A_rs[:, tb:tb + 1]
        s4 = bank(zb, 0, 384).rearrange("p (b h d) -> p b h d", h=2, d=32)
        cr = A_cs[cb][:, tl, 0:32].unsqueeze(1).to_broadcast([128, 6, 32])
        sr = A_cs[cb][:, tl, 32:64].unsqueeze(1).to_broadcast([128, 6, 32])
        tv = [A_tmp[tb][:, k, :].rearrange("p (b d) -> p b d", d=32) for k in range(4)]
        tk = "Atmp%d" % tb
        P.op("dve", lambda e: e.tensor_tensor(out=tv[0], in0=s4[:, :, 0, :], in1=cr, op=ALU.mult), r=bk(zb) + [("Acs", cb)], w=[tk + "0"])
        P.op("dve", lambda e: e.tensor_tensor(out=tv[1], in0=s4[:, :, 1, :], in1=sr, op=ALU.mult), r=bk(zb) + [("Acs", cb)], w=[tk + "1"])
        P.op("dve", lambda e: e.tensor_tensor(out=tv[2], in0=s4[:, :, 0, :], in1=sr, op=ALU.mult), r=bk(zb) + [("Acs", cb)], w=[tk + "2"])
        P.op("dve", lambda e: e.tensor_tensor(out=tv[3], in0=s4[:, :, 1, :], in1=cr, op=ALU.mult), r=bk(zb) + [("Acs", cb)], w=[tk + "3"])
        u4 = A_u[tb].rearrange("p (b h d) -> p b h d", h=2, d=32)
        P.op("pool", lambda e: e.tensor_tensor(out=u4[:, :, 0, :], in0=tv[0], in1=tv[1], op=ALU.subtract), r=[tk + "0", tk + "1"], w=[("Au", tb, 0)])
        P.op("dve", lambda e: e.tensor_tensor(out=u4[:, :, 1, :], in0=tv[2], in1=tv[3], op=ALU.add), r=[tk + "2", tk + "3"], w=[("Au", tb, 1)])
        rstd_from_ssq(bank(4 + tb, 0, 1), D, rs, bk(4 + tb), ("Ars", tb))
        P.op("act", lambda e: e.activation(out=A_ktm[tb][:, 0:384], in_=A_u[tb], func=AF.Copy, scale=rs),
             r=[("Au", tb, 0), ("Au", tb, 1), ("Ars", tb)], w=["Aktm%da" % tb, "Aktm%db" % tb])
        P.op("act", lambda e: e.activation(out=A_ktm[tb][:, 384:512], in_=bank(zb, 384, 512), func=AF.Copy, scale=rs),
             r=bk(zb) + [("Ars", tb)], w=["Aktm%dv" % tb])
        for j in range(2):
            P.op("act", lambda e, j=j: e.activation(
                out=Vtm[j][:, tile, :, 0:64], in_=bank(zb + 1, j * 128, (j + 1) * 128).rearrange("p (g d) -> p g d", d=64),
                func=AF.Copy, scale=rs), r=bk(zb + 1) + [("Ars", tb)], w=["Vtm"])
        tp = bankb(6 + tb)[:, 0:512].rearrange("p (a t) -> p a t", t=128)
        src_order = [0, 3, 1, 2]
        for a_, sblk in enumerate(src_order):
            P.op("pe", lambda e, a_=a_, sblk=sblk: e.transpose(tp[:, a_, :], A_ktm[tb][:, sblk * 128:(sblk + 1) * 128], identb[:]),
                 r=["Aktm%da" % tb, "Aktm%db" % tb, "Aktm%dv" % tb, "identb"], w=bk(6 + tb))
        P.op("dve", lambda e: e.tensor_copy(out=KVc[:, :, tile * 128:(tile + 1) * 128], in_=tp[:, 0:2, :]),
             r=bk(6 + tb), w=["KVc"])
        P.op("act", lambda e: e.activation(out=KVs[:, :, tile * 128:(tile + 1) * 128], in_=tp[:, 2:4, :], func=AF.Copy),
             r=bk(6 + tb), w=["KVs"])

    prev = None
    for ck in range(n_chunks):
        cb = ck % 2
        CT = 128 * TPC
        P.dma("sp", A_xf[cb], xT_d[:, ck * CT:(ck + 1) * CT].rearrange("(c p) t -> p c t", p=128), w=[("xf", cb)])
        P.dma("sp", A_cs[cb], cs_d[ck * CT:(ck + 1) * CT, :].rearrange("(a p) n -> p a n", p=128), w=[("Acs", cb)])
        P.op("dve", lambda e: e.tensor_copy(out=A_xTb[cb], in_=A_xf[cb]), r=[("xf", cb)], w=[("xTb", cb)])
        for tl in range(TPC):
            A_front(ck, tl)
            if prev is not None:
                A_back(*prev)
            prev = (ck, tl)
    A_back(*prev)

    if dbg:
        d_kvs = dbg_out("KVs", [128, 2 * T])
        d_kvc = dbg_out("KVc", [128, 2 * T])
        d_vtm = dbg_out("Vtm0", [128, 64 * 130])
        stg = view(TB + 32 * KB, [128, T // 2], F32, ["stg"])
        HT = T // 2
        for hh in range(2):
            for qq in range(2):
                P.op("dve", lambda e, hh=hh, qq=qq: e.tensor_copy(out=stg, in_=KVs[:, hh, qq * HT:(qq + 1) * HT]), r=["KVs"], w=["stg"])
                P.dma("sp", d_kvs[:, hh * T + qq * HT:hh * T + (qq + 1) * HT], stg, r=["stg"], key=("dbgo", 1))
                P.op("dve", lambda e, hh=hh, qq=qq: e.tensor_copy(out=stg, in_=KVc[:, hh, qq * HT:(qq + 1) * HT]), r=["KVc"], w=["stg"])
                P.dma("sp", d_kvc[:, hh * T + qq * HT:hh * T + (qq + 1) * HT], stg, r=["stg"], key=("dbgo", 2))
        for qq in range(4):
            P.op("dve", lambda e, qq=qq: e.tensor_copy(out=stg[:, 0:16 * 130], in_=Vtm[0][:, qq * 16:(qq + 1) * 16].rearrange("p a g d -> p (a g d)")), r=["Vtm", "Vtm_one0"], w=["stg"])
            P.dma("sp", d_vtm[:, qq * 16 * 130:(qq + 1) * 16 * 130], stg[:, 0:16 * 130], r=["stg"], key=("dbgo", 3))
    if stop_after == "A":
        P.finalize()
        nc.in_names = in_names
        return nc

    for kv in range(2):
        for half in range(2):
            col = kv * 2 + half
            for l in range(32):
                P.op("pe", lambda e, kv=kv, half=half, l=l, col=col: e.matmul(
                    bank(4, 8 + col, 9 + col), lhsT=W1[kv][0:64, l, half * 128:(half + 1) * 128], rhs=peT[kv][:, l:l + 1],
                    start=(l == 0), stop=(l == 31)), r=[("W1", kv), "peT%d" % kv], w=bk(4))
    P.op("dve", lambda e: e.tensor_copy(out=cbias[:], in_=bank(4, 8, 12)), r=bk(4), w=["cbias"])
    Ap_hid = [view(TB + 0, [128, 2, 512], BF16, ["hid0"]), view(TB + 2 * KB, [128, 2, 512], BF16, ["hid1"])]
    KVd = [view(TB + 12 * KB, [128, 16, 512], BF16, [("KVd", 0)]), view(TB + 28 * KB, [128, 16, 512], BF16, [("KVd", 1)])]
    P.op("dve", lambda e: e.tensor_copy(out=KVd[0], in_=KVc[:, 0, :].rearrange("p (m b) -> p b m", b=16)), r=["KVc"], w=[("KVd", 0)])
    P.op("pool", lambda e: e.tensor_copy(out=KVd[1], in_=KVc[:, 1, :].rearrange("p (m b) -> p b m", b=16)), r=["KVc"], w=[("KVd", 1)])
    P.alias("hid0", [("xTb", 0)])
    P.alias("hid1", [("xTb", 0)])
    P.op("pool", lambda e: e.memset(Ap_hid[0], 0.0), w=["hid0"])
    P.op("pool", lambda e: e.memset(Ap_hid[1], 0.0), w=["hid1"])
    it = 0
    for kv in range(2):
        for g in range(2):
            hb = it % 2
            it += 1
            hid = Ap_hid[hb]
            for half in range(2):
                hbank = half
                for l in range(32):
                    P.op("pe", lambda e, kv=kv, g=g, half=half, l=l, hbank=hbank: e.matmul(
                        bank(hbank, 0, 511), lhsT=W1[kv][g * 64:(g + 1) * 64, l, half * 128:(half + 1) * 128],
                        rhs=KVd[kv][g * 64:(g + 1) * 64, l % 16, (l // 16):(l // 16) + 511], start=(l == 0), stop=(l == 31)),
                        r=[("W1", kv), ("KVd", kv)], w=bk(hbank))
                P.op("act", lambda e, kv=kv, half=half, hbank=hbank, hid=hid: e.activation(
                    out=hid[:, half, 0:511], in_=bank(hbank, 0, 511), func=AF.Gelu_apprx_tanh, bias=cbias[:, kv * 2 + half:kv * 2 + half + 1]),
                    r=bk(hbank) + ["cbias"], w=["hid%d" % hb])
            if kv == 0:
                for half in range(2):
                    P.op("pe", lambda e, g=g, half=half, hid=hid: e.matmul(PS[g * 64:(g + 1) * 64, 2 * 512:3 * 512], lhsT=w2[0][:, half, :],
                                                                          rhs=hid[:, half, :], start=(half == 0), stop=(half == 1)),
                         r=["hid%d" % hb, "w20"], w=bk(2))
                P.op("dve", lambda e, g=g: e.tensor_copy(out=kcT[g * 64:(g + 1) * 64, :], in_=PS[g * 64:(g + 1) * 64, 2 * 512:3 * 512]),
                     r=bk(2), w=["kcT"])
            else:
                for ct in range(4):
                    for half in range(2):
                        P.op("pe", lambda e, ct=ct, half=half, hid=hid: e.matmul(bank(3, ct * 64, (ct + 1) * 64), lhsT=hid[:, half, ct * 128:(ct + 1) * 128],
                                                                                rhs=w2[1][:, half, :], start=(half == 0), stop=(half == 1)),
                             r=["hid%d" % hb, "w21"], w=bk(3))
                P.op("dve", lambda e, g=g: e.tensor_copy(out=vcx[:, :, g, 0:64], in_=bank(3, 0, 256).rearrange("p (c d) -> p c d", d=64)),
                     r=bk(3) + ["vcx_one"], w=["vcx_v%d" % g])
    VCX = ["vcx_ovl", "vcx_one", "vcx_v0", "vcx_v1"]

    if dbg:
        d_kct = dbg_out("kcT", [128, 512])
        d_vcx = dbg_out("vcx", [128, 4 * 2 * 194])
        stg = view(TB + 32 * KB, [128, 4 * 2 * 194], F32, ["stg"])
        P.op("dve", lambda e: e.tensor_copy(out=stg[:, 0:512], in_=kcT[:]), r=["kcT"], w=["stg"])
        P.dma("sp", d_kct, stg[:, 0:512], r=["stg"], key=("dbgo", 4))
        P.op("dve", lambda e: e.tensor_copy(out=stg, in_=vcx[:].rearrange("p a g d -> p (a g d)")), r=VCX, w=["stg"])
        P.dma("sp", d_vcx, stg, r=["stg"], key=("dbgo", 5))
    if stop_after == "Ap":
        P.finalize()
        nc.in_names = in_names
        return nc

    P.alias("Wown", ["KVc"])
    P.dma("pool", Wown, wown_d.rearrange("(c p) n -> p c n", p=128), w=["Wown"])
    for dc in range(8):
        P.op("dve", lambda e, dc=dc: e.tensor_scalar(out=Wown[:, dc, :], in0=Wown[:, dc, :], scalar1=gmix[:, dc:dc + 1],
                                                      scalar2=None, op0=ALU.mult), r=["Wown", "gmix"], w=["Wown"])
    Ewide = view(TB + 0, [128, 64, 128], BF16, ["Ewide"])
    P.alias("Ewide", [("xTb", 0), ("xTb", 1), "hid0", "hid1"])
    P.dma("pool", Ewide, ewide_d.rearrange("p (k q) -> p k q", q=128), w=["Ewide"])
    o = TB + 16 * KB
    B_xTo = [view(o, [128, 8, 128], BF16, [("xTo", 0)]), view(o + 2 * KB, [128, 8, 128], BF16, [("xTo", 1)])]; o += 4 * KB
    B_sq = view(o, [128, 8, 128], BF16, ["Bsq"]); o += 2 * KB
    B_cso = [view(o, [128, 64], F32, [("cso", 0)]), view(o + 256, [128, 64], F32, [("cso", 1)])]; o += 512
    B_mC = [view(o, [128, 2, 128], BF16, [("mC", 0)]), view(o + 512, [128, 2, 128], BF16, [("mC", 1)])]; o += 1 * KB
    B_m12 = [view(o, [128, 256], F32, [("m12", 0)]), view(o + KB, [128, 256], F32, [("m12", 1)])]; o += 2 * KB
    B_Ocs = view(o, [128, 4 * 194], F32, ["Ocs"])
    B_uv = view(o, [128, 1024], F32, [("uv", 0), ("uv", 1)]); o += 4 * KB
    B_gates = view(o, [128, 24], F32, ["gates"]); o += 128
    B_csr = view(o, [128, 64], F32, ["Bcsr"]); o += 256
    B_OTs = view(o, [128, 512], F32, ["OTs"])
    B_tmp = view(o, [128, 4, 256], F32, ["Btmp%d" % k_ for k_ in range(4)]); o += 4 * KB
    B_qtm = view(o, [128, 512], BF16, ["Bqtma", "Bqtmb"]); o += KB
    B_QZ = [view(o, [128, 512], BF16, [("QZ", 0)]), view(o + KB, [128, 512], BF16, [("QZ", 1)])]; o += 2 * KB
    B_vn = view(o, [128, 512], BF16, ["vn"]); o += KB
    B_oa = view(o, [128, 512], F32, ["oa"]); o += 2 * KB
    B_junk = view(o, [128, 1024], BF16, ["junk"]); o += 2 * KB
    B_cat = view(o, [128, 1024], BF16, ["cat_a", "cat_b"]); o += 2 * KB
    B_Pt = [view(o + k * KB, [128, 512], BF16, [("Pt", k)]) for k in range(3)]; o += 3 * KB
    B_imp = view(o, [128, 128], F32, ["imp"]); o += 512
    B_score = view(o, [128, 128], F32, ["score"]); o += 512
    B_wk = view(o, [128, 128], F32, ["wk"]); o += 512
    B_m8 = view(o, [128, 16], F32, ["m8a", "m8b"]); o += 64
    B_sel = view(o, [128, 128], BF16, ["sel"]); o += 256
    B_selT = view(o, [128, 4, 128], BF16, ["selT"]); o += KB
    B_rd = view(o, [128, 16], F32, ["rdc", "rds", "rdw"]); o += 64
    B_fac = view(o, [128, 16], F32, ["fac"]); o += 64
    B_ob = view(o, [128, 512], F32, [("ob", 0), ("ob", 1)]); o += 2 * KB
    B_t2 = view(o, [128, 256], F32, ["t2"]); o += KB
    B_rs = view(o, [128, 8], F32, ["Brs", "Brsv", "Brsa", "Brsb"]); o += 32
    dmwm = view(o, [128, 12, 128], BF16, ["dmwm"]); o += 3 * KB
    P.dma("pool", dmwm, dmwm_d.rearrange("p (r q) -> p r q", q=128), w=["dmwm"])
    P.op("pool", lambda e: e.memset(B_QZ[0], 0.0), w=[("QZ", 0)])
    P.op("pool", lambda e: e.memset(B_QZ[1], 0.0), w=[("QZ", 1)])
    assert o <= ARENA, o

    d_oa = dbg_out("oa", [128, 512])
    d_ob = dbg_out("ob", [128, 512])
    d_score = dbg_out("score", [128, 256])
    d_imp = dbg_out("imp", [128, 256])
    d_oc = dbg_out("Oc", [128, 2 * 776])
    d_os = dbg_out("Os", [128, 1024])
    d_ow = dbg_out("Ow", [128, 1024])
    d_gates = dbg_out("gates", [128, 24])
    DBG_TILE = 1

    pcount = [0]
    scount = [0]

    LA = 3
    SBANKS = [0, 1, 2, 7]
    pipe = []

    def pipe_step(s1, later):
        s1()
        pipe.append(later)
        if len(pipe) > LA:
            pipe.pop(0)()

    def pipe_flush():
        while pipe:
            pipe.pop(0)()

    def nsa_group(i, g):
        Qg = B_QZ[g]
        mb = i % 2
        OcK = bk(3) + bk(4)
        OsK = bk(5)
        OwK = bk(6)
        Oc = PS[:, 3 * 512:5 * 512].rearrange("p (j x) -> p j x", x=256)
        Os = bank(5).rearrange("p (j x) -> p j x", x=128)
        Ow = bank(6).rearrange("p (j x) -> p j x", x=128)
        Ocs = B_Ocs.rearrange("p (j x) -> p j x", x=194)

        def score_step(lhsT, lkeys, masks):
            sbk = SBANKS[scount[0] % len(SBANKS)]
            scount[0] += 1

            def s1():
                P.op("pe", lambda e: e.matmul(bank(sbk), lhsT=lhsT, rhs=Qg, start=True, stop=(len(masks) == 0), skip_group_check=True),
                     r=lkeys + [("QZ", g)], w=bk(sbk))
                for mi_, (ml, mr, mk) in enumerate(masks):
                    if mr.shape[-1] == 512:
                        last = (mi_ == len(masks) - 1)
                        P.op("pe", lambda e, ml=ml, mr=mr, last=last: e.matmul(bank(sbk), lhsT=ml, rhs=mr, start=False, stop=last, skip_group_check=True),
                             r=mk, w=bk(sbk))
                        continue
                    for j in range(4):
                        last = (mi_ == len(masks) - 1) and j == 3
                        P.op("pe", lambda e, ml=ml, mr=mr, j=j, last=last: e.matmul(bank(sbk, j * 128, (j + 1) * 128), lhsT=ml, rhs=mr,
                                                                                  start=False, stop=last, skip_group_check=True),
                             r=mk, w=bk(sbk))
            return sbk, s1

        def exp_pv(sbk, rhs_of_j, rkeys, outs, okeys, first, lastf, post=None):
            def later():
                pb = pcount[0] % 3
                pcount[0] += 1
                P.op("act", lambda e: e.activation(out=B_Pt[pb], in_=bank(sbk), func=AF.Exp), r=bk(sbk), w=[("Pt", pb)])
                for j in range(4):
                    P.op("pe", lambda e, j=j: e.matmul(outs[j], lhsT=B_Pt[pb][:, j * 128:(j + 1) * 128], rhs=rhs_of_j,
                                                       start=(first and j in okeys[1]), stop=lastf, skip_group_check=True),
                         r=[("Pt", pb)] + rkeys, w=okeys[0])
                if post is not None:
                    post()
            return later

        def exp_pvT(sbk, vext, rkeys, obank, okey, first, lastf, post=None):
            def later():
                pb = pcount[0] % 3
                pcount[0] += 1
                P.op("act", lambda e: e.activation(out=B_Pt[pb], in_=bank(sbk), func=AF.Exp), r=bk(sbk), w=[("Pt", pb)])
                P.op("pe", lambda e: e.matmul(PS[0:65, obank * 512:(obank + 1) * 512], lhsT=vext, rhs=B_Pt[pb], start=first, stop=lastf, skip_group_check=True),
                     r=[("Pt", pb)] + rkeys, w=okey)
                if post is not None:
                    post()
            return later

        def untranspose(obank, okey):
            P.op("act", lambda e: e.activation(out=B_OTs[0:65, :], in_=PS[0:65, obank * 512:(obank + 1) * 512], func=AF.Copy), r=okey, w=["OTs"])
            for j in range(4):
                P.op("pe", lambda e, j=j: e.transpose(bank(obank, j * 128, j * 128 + 65), B_OTs[0:65, j * 128:(j + 1) * 128], identf[0:65, 0:65]),
                     r=["OTs", "identf"], w=okey)

        n_ct = (32 * i + 30) // 128 + 1
        oc_outs = [PS[:, 3 * 512 + j * 256: 3 * 512 + j * 256 + 194] for j in range(4)]

        def post_cmp():
            P.op("act", lambda e: e.activation(out=Ocs, in_=Oc[:, :, 0:194], func=AF.Copy), r=OcK, w=["Ocs"])
            P.op("dve", lambda e: e.tensor_scalar(out=B_rd[:, 0:4], in0=Ocs[:, :, 64], scalar1=1e-30, scalar2=None, op0=ALU.max), r=["Ocs"], w=["rdc"])
            P.op("dve", lambda e: e.reciprocal(out=B_rd[:, 0:4], in_=B_rd[:, 0:4]), r=["rdc"], w=["rdc"])
            P.op("dve", lambda e: e.tensor_scalar(out=B_imp, in0=Ocs[:, 0, 66:194], scalar1=B_rd[:, 0:1], scalar2=None, op0=ALU.mult), r=["Ocs", "rdc"], w=["imp"])
            for j in range(1, 4):
                P.op("dve", lambda e, j=j: e.scalar_tensor_tensor(out=B_imp, in0=Ocs[:, j, 66:194], scalar=B_rd[:, j:j + 1], in1=B_imp, op0=ALU.mult, op1=ALU.add),
                     r=["Ocs", "rdc", "imp"], w=["imp"])
            P.op("dve", lambda e: e.tensor_tensor(out=B_score, in0=B_imp, in1=B_m12[mb][:, 0:128], op=ALU.mult), r=["imp", ("m12", mb)], w=["score"])
            P.op("dve", lambda e: e.tensor_tensor(out=B_score, in0=B_score, in1=B_m12[mb][:, 128:256], op=ALU.add), r=["score", ("m12", mb)], w=["score"])
            if dbg and i == DBG_TILE:
                P.dma("sp", d_score[:, g * 128:(g + 1) * 128], B_score, r=["score"], key=("dbgo", 200 + g))
                P.dma("sp", d_imp[:, g * 128:(g + 1) * 128], B_imp, r=["imp"], key=("dbgo", 202 + g))
                P.dma("sp", d_oc[:, g * 776:(g + 1) * 776], B_Ocs, r=["Ocs"], key=("dbgo", 204 + g))
            P.op("dve", lambda e: e.max(out=B_m8[:, 0:8], in_=B_score), r=["score"], w=["m8a"])
            P.op("dve", lambda e: e.match_replace(out=B_wk, in_to_replace=B_m8[:, 0:8], in_values=B_score, imm_value=-1e30), r=["score", "m8a"], w=["wk"])
            P.op("dve", lambda e: e.max(out=B_m8[:, 8:16], in_=B_wk), r=["wk"], w=["m8b"])
            P.op("dve", lambda e: e.tensor_scalar(out=B_sel, in0=B_score, scalar1=B_m8[:, 15:16], scalar2=None, op0=ALU.is_ge), r=["score", "m8b"], w=["sel"])
            P.op("dve", lambda e: e.tensor_scalar(out=B_sel, in0=B_sel, scalar1=-1.0, scalar2=30000.0, op0=ALU.add, op1=ALU.mult), r=["sel"], w=["sel"])
            tpv = bankb(3)[:, 0:128]
            P.op("pe", lambda e: e.transpose(tpv, B_sel, identb[:]), r=["sel", "identb"], w=bk(3))
            P.op("dve", lambda e: e.tensor_copy(out=B_selT, in_=bc_mid(tpv, 4)), r=bk(3), w=["selT"])

        for ct in range(n_ct):
            masks = []
            if ct >= n_ct - 2:
                mi = ct - (n_ct - 2)
                masks.append((identb[:], B_mC[mb][:, mi, :], ["identb", ("mC", mb)]))
            sbk, s1 = score_step(kcT[:, ct * 128:(ct + 1) * 128], ["kcT"], masks)
            pipe_step(s1, exp_pv(sbk, vcx[:, ct, g, :], VCX, oc_outs, (OcK, (0, 2)), ct == 0, ct == n_ct - 1,
                                 post=(post_cmp if ct == n_ct - 1 else None)))

        ow_outs = [bank(6, j * 128, j * 128 + 65) for j in range(4)]
        rlist = [r_ for r_ in range(8) if 4 * i - 4 + r_ >= 0]
        for r_ in rlist:
            kt = 4 * i - 4 + r_
            masks = [(identb[:], dmwm[:, 4 + r_, :], ["identb", "dmwm"])]
            sbk, s1 = score_step(KVs[:, 1, kt * 128:(kt + 1) * 128], ["KVs"], masks)
            pipe_step(s1, exp_pv(sbk, Vtm[1][:, kt, g, :], ["Vtm", "Vtm_one1"], ow_outs, (OwK, (0,)), r_ == rlist[0], r_ == rlist[-1]))

        os_outs = [bank(5, j * 128, j * 128 + 65) for j in range(4)]
        n_kt = 4 * i + 4

        def post_group():
            if dbg and i == DBG_TILE:
                stg = view(TB + 58 * KB, [128, 1024], F32, ["stg2"])
                P.op("dve", lambda e: e.memset(stg, 0.0), w=["stg2"])
                P.op("dve", lambda e: e.tensor_copy(out=stg[:, 0:512].rearrange("p (j x) -> p j x", x=128)[:, :, 0:65], in_=Os[:, :, 0:65]), r=OsK + ["stg2"], w=["stg2"])
                P.dma("sp", d_os[:, g * 512:(g + 1) * 512], stg[:, 0:512], r=["stg2"], key=("dbgo", 102 + g))
                P.op("dve", lambda e: e.tensor_copy(out=stg[:, 0:512].rearrange("p (j x) -> p j x", x=128)[:, :, 0:65], in_=Ow[:, :, 0:65]), r=OwK, w=["stg2"])
                P.dma("sp", d_ow[:, g * 512:(g + 1) * 512], stg[:, 0:512], r=["stg2"], key=("dbgo", 104 + g))
            P.op("dve", lambda e: e.reciprocal(out=B_rd[:, 4:8], in_=Os[:, :, 64]), r=OsK, w=["rds"])
            P.op("dve", lambda e: e.reciprocal(out=B_rd[:, 8:12], in_=Ow[:, :, 64]), r=OwK, w=["rdw"])
            gsl = B_gates[:, g * 12:(g + 1) * 12].rearrange("p (h b) -> p b h", b=3)
            P.op("dve", lambda e: e.tensor_tensor(out=B_fac[:, 0:12].rearrange("p (b h) -> p b h", h=4), in0=B_rd[:, 0:12].rearrange("p (b h) -> p b h", h=4),
                                                  in1=gsl, op=ALU.mult), r=["rdc", "rds", "rdw", "gates"], w=["fac"])
            obg = B_ob[:, g * 256:(g + 1) * 256].rearrange("p (j d) -> p j d", d=64)
            t2 = B_t2.rearrange("p (j d) -> p j d", d=64)
            P.op("pool", lambda e: e.tensor_tensor(out=obg, in0=Ocs[:, :, 0:64], in1=bc_last(B_fac[:, 0:4], 64), op=ALU.mult), r=["Ocs", "fac"], w=[("ob", g)])
            P.op("dve", lambda e: e.tensor_tensor(out=t2, in0=Os[:, :, 0:64], in1=bc_last(B_fac[:, 4:8], 64), op=ALU.mult), r=OsK + ["fac"], w=["t2"])
            P.op("pool", lambda e: e.tensor_tensor(out=obg, in0=obg, in1=t2, op=ALU.add), r=[("ob", g), "t2"], w=[("ob", g)])
            P.op("dve", lambda e: e.tensor_tensor(out=t2, in0=Ow[:, :, 0:64], in1=bc_last(B_fac[:, 8:12], 64), op=ALU.mult), r=OwK + ["fac"], w=["t2"])
            P.op("pool", lambda e: e.tensor_tensor(out=obg, in0=obg, in1=t2, op=ALU.add), r=[("ob", g), "t2"], w=[("ob", g)])

        for kt in range(n_kt):
            masks = [(Ewide[:, kt, :], B_selT.rearrange("p j q -> p (j q)"), ["Ewide", "selT"])]
            if kt >= 4 * i:
                masks.append((identb[:], dmwm[:, kt - 4 * i, :], ["identb", "dmwm"]))
            sbk, s1 = score_step(KVs[:, 0, kt * 128:(kt + 1) * 128], ["KVs"], masks)

            def post_sel():
                untranspose(5, OsK)
                post_group()
            pipe_step(s1, exp_pv(sbk, Vtm[0][:, kt, g, :], ["Vtm", "Vtm_one0"], os_outs, (OsK, (0,)), kt == 0, kt == n_kt - 1,
                                 post=(post_group if kt == n_kt - 1 else None)))

    n_own = NOWN if stop_after not in ("B1x",) else 2
    for i in range(n_own):
        ib = i % 2
        P.dma("pool", B_xTo[ib], xTo_d[i].rearrange("(c p) t -> p c t", p=128), w=[("xTo", ib)])
        P.dma("sp", B_cso[ib], cso_d[i * 128:(i + 1) * 128, :], w=[("cso", ib)])
        P.dma("pool", B_mC[ib], mC_d[i].rearrange("p (k q) -> p k q", q=128), w=[("mC", ib)])
        P.dma("sp", B_m12[ib], m12_d[i], w=[("m12", ib)])
        xs = B_xTo[ib]
        P.op("act", lambda e, xs=xs: e.activation(out=B_sq, in_=xs, func=AF.Square), r=[("xTo", ib)], w=["Bsq"])
        for dc in range(8):
            P.op("pe", lambda e, dc=dc: e.matmul(bank(6, 0, 1), lhsT=B_sq[:, dc, :], rhs=ones[:, 0:1], start=(dc == 0), stop=(dc == 7)),
                 r=["Bsq", "ones"], w=bk(6))
        for half in range(2):
            for dc in range(8):
                P.op("pe", lambda e, dc=dc, half=half, xs=xs: e.matmul(bank(half), lhsT=xs[:, dc, :], rhs=Wown[:, dc, half * 512:(half + 1) * 512],
                                                                     start=(dc == 0), stop=(dc == 7)), r=[("xTo", ib), "Wown"], w=bk(half))
        for dc in range(8):
            P.op("pe", lambda e, dc=dc, xs=xs: e.matmul(bank(2), lhsT=xs[:, dc, :], rhs=Wown[:, dc, 1024:1536], start=(dc == 0), stop=(dc == 7)),
                 r=[("xTo", ib), "Wown"], w=bk(2))
        for dc in range(8):
            P.op("pe", lambda e, dc=dc, xs=xs: e.matmul(bank(5, 0, 24), lhsT=xs[:, dc, :], rhs=Wown[:, dc, 1536:1560], start=(dc == 0), stop=(dc == 7)),
                 r=[("xTo", ib), "Wown"], w=bk(5))
        rs = B_rs[:, 0:1]
        rstd_from_ssq(bank(6, 0, 1), D, rs, bk(6), "Brs")
        for half in range(2):
            P.op("act", lambda e, half=half: e.activation(out=B_uv[:, half * 512:(half + 1) * 512], in_=bank(half), func=AF.Gelu_apprx_tanh, scale=rs),
                 r=bk(half) + ["Brs"], w=[("uv", half)])
        P.op("act", lambda e: e.activation(out=B_gates, in_=bank(5, 0, 24), func=AF.Sigmoid, scale=rs), r=bk(5) + ["Brs"], w=["gates"])
        P.op("dve", lambda e, ib=ib: e.tensor_scalar(out=B_csr, in0=B_cso[ib], scalar1=rs, scalar2=0.125, op0=ALU.mult, op1=ALU.mult),
             r=[("cso", ib), "Brs"], w=["Bcsr"])
        rope_tm(bank(2), 8, B_csr, B_qtm, B_tmp, bk(2) + ["Bcsr"], "Btmp", "Bqtm")
        tq = bankb(7)[:, 0:512].rearrange("p (j t) -> p j t", t=128)
        for j in range(4):
            P.op("pe", lambda e, j=j: e.transpose(tq[:, j, :], B_qtm[:, j * 128:(j + 1) * 128], identb[:]), r=["Bqtma", "Bqtmb", "identb"], w=bk(7, (0, 1)))
        for gq in range(2):
            P.op("act", lambda e, gq=gq: e.activation(out=B_QZ[gq][gq * 64:(gq + 1) * 64].rearrange("p (j q) -> p j q", q=128),
                                                      in_=tq[gq * 64:(gq + 1) * 64], func=AF.Copy), r=bk(7), w=[("QZ", gq)])
        P.op("act", lambda e: e.activation(out=B_junk[:, 0:512], in_=B_uv[:, 512:1024], func=AF.Square, accum_out=B_rs[:, 1:2]), r=[("uv", 1)], w=["junk", "Brsv"])
        rstd_from_ssq(B_rs[:, 1:2], 512, B_rs[:, 1:2], ["Brsv"], "Brsv")
        P.op("dve", lambda e: e.scalar_tensor_tensor(out=B_vn, in0=B_uv[:, 512:1024], scalar=B_rs[:, 1:2], in1=gv[:], op0=ALU.mult, op1=ALU.mult),
             r=[("uv", 1), "Brsv", "gv"], w=["vn"])
        for g8 in range(8):
            P.op("pe", lambda e, g8=g8: e.matmul(bank(2, g8 * 64, (g8 + 1) * 64), lhsT=wsT[:, g8, :], rhs=B_vn[:, g8 * 64:(g8 + 1) * 64], start=True, stop=True),
                 r=["wsT", "vn"], w=bk(2))
        oa3 = B_oa.rearrange("p (g d) -> p g d", d=64)
        P.op("dve", lambda e: e.tensor_tensor(out=oa3, in0=bank(2).rearrange("p (g d) -> p g d", d=64), in1=bc_last(bsT[:, :], 64), op=ALU.add),
             r=bk(2) + ["bsT"], w=["oa"])
        P.op("dve", lambda e: e.tensor_tensor(out=B_oa, in0=B_oa, in1=B_uv[:, 0:512], op=ALU.mult), r=["oa", ("uv", 0)], w=["oa"])
        P.op("act", lambda e: e.activation(out=B_junk[:, 0:512], in_=B_oa, func=AF.Square, accum_out=B_rs[:, 2:3]), r=["oa"], w=["junk", "Brsa"])
        rstd_from_ssq(B_rs[:, 2:3], 512, B_rs[:, 2:3], ["Brsa"], "Brsa")
        P.op("dve", lambda e: e.scalar_tensor_tensor(out=B_cat[:, 0:512], in0=B_oa, scalar=B_rs[:, 2:3], in1=ga[:], op0=ALU.mult, op1=ALU.mult),
             r=["oa", "Brsa", "ga"], w=["cat_a"])
        if dbg and i == DBG_TILE:
            stg = view(TB + 58 * KB, [128, 1024], F32, ["stg2"])
            P.dma("sp", d_oa, B_oa, r=["oa"], key=("dbgo", 12))
            P.dma("sp", d_gates, B_gates, r=["gates"], key=("dbgo", 13))
        for g in range(2):
            nsa_group(i, g)
        pipe_flush()
        if dbg and i == DBG_TILE:
            P.dma("sp", d_ob, B_ob, r=[("ob", 0), ("ob", 1)], key=("dbgo", 14))
        P.op("act", lambda e: e.activation(out=B_junk[:, 0:512], in_=B_ob, func=AF.Square, accum_out=B_rs[:, 3:4]), r=[("ob", 0), ("ob", 1)], w=["junk", "Brsb"])
        rstd_from_ssq(B_rs[:, 3:4], 512, B_rs[:, 3:4], ["Brsb"], "Brsb")
        P.op("dve", lambda e: e.scalar_tensor_tensor(out=B_cat[:, 512:1024], in0=B_ob, scalar=B_rs[:, 3:4], in1=gb[:], op0=ALU.mult, op1=ALU.mult),
             r=[("ob", 0), ("ob", 1), "Brsb", "gb"], w=["cat_b"])
        tc = bankb(7).rearrange("p (k t) -> p k t", t=128)
        for kc in range(8):
            P.op("pe", lambda e, kc=kc: e.transpose(tc[:, kc, :], B_cat[:, kc * 128:(kc + 1) * 128], identb[:]), r=["cat_a", "cat_b", "identb"], w=bk(7))
        if i == 0:
            P.alias(("catT", 0), [("W1", 0), ("W1", 1)])
        P.op("act", lambda e, i=i: e.activation(out=catT[:, :, i * 128:(i + 1) * 128], in_=tc, func=AF.Copy), r=bk(7), w=[("catT", i)])

    if stop_after in ("B1", "B1x"):
        if dbg:
            d_catT = dbg_out("catT", [128, 8 * NOWN * 128])
            for kc in range(8):
                stg = view(TB + 58 * KB, [128, 1024], F32, ["stg2"])
                for hh in range(2):
                    P.op("dve", lambda e, kc=kc, hh=hh: e.tensor_copy(out=stg, in_=catT[:, kc, hh * 1024:(hh + 1) * 1024]),
                         r=[("catT", i) for i in range(n_own)], w=["stg2"])
                    P.dma("sp", d_catT[:, kc * 2048 + hh * 1024: kc * 2048 + (hh + 1) * 1024], stg, r=["stg2"], key=("dbgo", 15))
        P.finalize()
        nc.in_names = in_names
        return nc

    P.dma("pool", Wo, wo_d.rearrange("(c p) n -> p c n", p=128), w=["Wo"])
    C_gmoe = view(16 * KB, [128, D], F32, ["gmoe"])
    C_hn2 = [view(20 * KB, [128, D], F32, [("hn2", 0)]), view(24 * KB, [128, D], F32, [("hn2", 1)])]
    o = TB + 32 * KB
    C_xo = [view(o, [128, D], F32, [("xo", 0)]), view(o + 4 * KB, [128, D], F32, [("xo", 1)])]; o += 8 * KB
    C_hn2Tf = [view(o, [128, 8, 128], F32, [("hn2Tf", 0)]), view(o + 4 * KB, [128, 8, 128], F32, [("hn2Tf", 1)])]; o += 8 * KB
    C_junk = view(o, [128, D], BF16, ["Cjunk"]); o += 2 * KB
    RK2 = ["q_mg", "q_sg", "q_l1", "q_l2", "q_e2", "rstd2"] + [("ssq2", i_) for i_ in range(NOWN)]
    C_lgall = view(o, [128, NOWN, 20], F32, [("lg", i_) for i_ in range(NOWN)]); o += 1280
    C_r = view(o, [128, 12, NOWN * 4], F32, ["r_ohg", "r_dg", "r_les", "r_eq1", "r_x2", "r_sel2", "r_ee"]); o += 3072
    C_q = view(o, [128, 12, NOWN], F32, RK2); o += 768
    C_t44 = view(o, [128, NOWN * 16], F32, ["t44"]); o += 1024
    assert o <= ARENA
    P.dma("sp", C_gmoe, gmoe_d, w=["gmoe"])


    def B2_front(i):
        ib = i % 2
        hb = 0 if i % 2 == 0 else 2
        P.dma("sp", C_xo[ib], xo_d[i], w=[("xo", ib)])
        for half in range(2):
            for kc in range(8):
                P.op("pe", lambda e, kc=kc, half=half: e.matmul(bank(hb + half), lhsT=catT[:, kc, i * 128:(i + 1) * 128], rhs=Wo[:, kc, half * 512:(half + 1) * 512],
                                                              start=(kc == 0), stop=(kc == 7)), r=[("catT", i), "Wo"], w=bk(hb + half))

    def B2_back(i):
        ib = i % 2
        hb = 0 if i % 2 == 0 else 2
        for half in range(2):
            P.op("dve", lambda e, half=half: e.tensor_tensor(out=h1[:, i, half * 512:(half + 1) * 512], in0=bank(hb + half),
                                                             in1=C_xo[ib][:, half * 512:(half + 1) * 512], op=ALU.add),
                 r=bk(hb + half) + [("xo", ib)], w=[("h1", i)])
        P.op("act", lambda e: e.activation(out=C_junk, in_=h1[:, i, :], func=AF.Square, accum_out=C_q[:, 0, i:i + 1]), r=[("h1", i)], w=["Cjunk", ("ssq2", i)])

    for i in range(NOWN):
        B2_front(i)
        if i > 0:
            B2_back(i - 1)
    B2_back(NOWN - 1)
    SS2 = [("ssq2", i) for i in range(NOWN)]
    P.op("act", lambda e: e.activation(out=C_q[:, 1, :], in_=C_q[:, 0, :], func=AF.Sqrt, scale=1.0 / D, bias=EPS), r=SS2, w=["rstd2"])
    P.op("dve", lambda e: e.reciprocal(out=C_q[:, 1, :], in_=C_q[:, 1, :]), r=["rstd2"], w=["rstd2"])

    def B2c_front(i):
        ib = i % 2
        P.op("dve", lambda e: e.scalar_tensor_tensor(out=C_hn2[ib], in0=h1[:, i, :], scalar=C_q[:, 1, i:i + 1], in1=C_gmoe, op0=ALU.mult, op1=ALU.mult),
             r=[("h1", i), "rstd2", "gmoe"], w=[("hn2", ib)])

    def B2c_back(i):
        ib = i % 2
        tb_ = 4 if i % 2 == 0 else 6
        tf = PS[:, tb_ * 512:(tb_ + 2) * 512].rearrange("p (c t) -> p c t", t=128)
        for dc in range(8):
            P.op("pe", lambda e, dc=dc: e.transpose(tf[:, dc, :], C_hn2[ib][:, dc * 128:(dc + 1) * 128], identf[:]), r=[("hn2", ib), "identf"], w=bk(tb_) + bk(tb_ + 1))
        P.op("act", lambda e: e.activation(out=C_hn2Tf[ib], in_=tf, func=AF.Copy), r=bk(tb_) + bk(tb_ + 1), w=[("hn2Tf", ib)])
        P.op("dve", lambda e: e.tensor_copy(out=hn2T[:, :, i * 128:(i + 1) * 128], in_=tf), r=bk(tb_) + bk(tb_ + 1), w=[("hn2T", i)])
        for dc in range(8):
            P.op("pe", lambda e, dc=dc: e.matmul(bank(tb_, 0, 20), lhsT=C_hn2Tf[ib][:, dc, :], rhs=rcat[:, dc, :], start=(dc == 0), stop=(dc == 7)),
                 r=[("hn2Tf", ib), "rcat"], w=bk(tb_))
        P.op("act", lambda e: e.activation(out=C_lgall[:, i, :], in_=bank(tb_, 0, 20), func=AF.Copy), r=bk(tb_), w=[("lg", i)])

    for i in range(NOWN):
        B2c_front(i)
        if i > 0:
            B2c_back(i - 1)
    B2c_back(NOWN - 1)

    LGA = [("lg", i) for i in range(NOWN)]
    G3 = C_lgall[:, :, 0:4]
    E4 = C_lgall[:, :, 4:20].rearrange("p t (g e) -> p t g e", e=4)
    R3 = lambda k: C_r[:, k, :].rearrange("p (t x) -> p t x", x=4)
    Q = lambda k: C_q[:, k, :]
    bq = lambda k: C_q[:, k, :].unsqueeze(2).to_broadcast([128, NOWN, 4])
    P.op("dve", lambda e: e.tensor_reduce(out=Q(2), in_=G3, axis=AX.X, op=ALU.max), r=LGA, w=["q_mg"])
    P.op("dve", lambda e: e.tensor_tensor(out=R3(0), in0=G3, in1=bq(2), op=ALU.is_equal), r=LGA + ["q_mg"], w=["r_ohg"])
    P.op("dve", lambda e: e.tensor_tensor(out=R3(1), in0=G3, in1=bq(2), op=ALU.subtract), r=LGA + ["q_mg"], w=["r_dg"])
    P.op("act", lambda e: e.activation(out=R3(1), in_=R3(1), func=AF.Exp), r=["r_dg"], w=["r_dg"])
    P.op("dve", lambda e: e.tensor_reduce(out=Q(3), in_=R3(1), axis=AX.X, op=ALU.add), r=["r_dg"], w=["q_sg"])
    P.op("dve", lambda e: e.reciprocal(out=Q(3), in_=Q(3)), r=["q_sg"], w=["q_sg"])
    t44 = C_t44.rearrange("p (t g e) -> p t g e", g=4, e=4)
    P.op("dve", lambda e: e.tensor_tensor(out=t44, in0=E4, in1=R3(0).unsqueeze(3).to_broadcast([128, NOWN, 4, 4]), op=ALU.mult), r=LGA + ["r_ohg"], w=["t44"])
    P.op("dve", lambda e: e.tensor_reduce(out=R3(2), in_=C_t44.rearrange("p (t g e) -> p t e g", g=4, e=4), axis=AX.X, op=ALU.add), r=["t44"], w=["r_les"])
    P.op("dve", lambda e: e.tensor_reduce(out=Q(4), in_=R3(2), axis=AX.X, op=ALU.max), r=["r_les"], w=["q_l1"])
    P.op("dve", lambda e: e.tensor_tensor(out=R3(3), in0=R3(2), in1=bq(4), op=ALU.is_equal), r=["r_les", "q_l1"], w=["r_eq1"])
    P.op("dve", lambda e: e.scalar_tensor_tensor(out=C_r[:, 4, :], in0=C_r[:, 3, :], scalar=-1e30, in1=C_r[:, 2, :], op0=ALU.mult, op1=ALU.add), r=["r_eq1", "r_les"], w=["r_x2"])
    P.op("dve", lambda e: e.tensor_reduce(out=Q(5), in_=R3(4), axis=AX.X, op=ALU.max), r=["r_x2"], w=["q_l2"])
    P.op("dve", lambda e: e.tensor_tensor(out=R3(5), in0=R3(2), in1=bq(5), op=ALU.is_ge), r=["r_les", "q_l2"], w=["r_sel2"])
    P.op("dve", lambda e: e.tensor_tensor(out=R3(6), in0=R3(2), in1=bq(4), op=ALU.subtract), r=["r_les", "q_l1"], w=["r_ee"])
    P.op("act", lambda e: e.activation(out=R3(6), in_=R3(6), func=AF.Exp), r=["r_ee"], w=["r_ee"])
    P.op("dve", lambda e: e.tensor_tensor(out=Q(6), in0=Q(5), in1=Q(4), op=ALU.subtract), r=["q_l2", "q_l1"], w=["q_e2"])
    P.op("act", lambda e: e.activation(out=Q(6), in_=Q(6), func=AF.Exp), r=["q_e2"], w=["q_e2"])
    P.op("dve", lambda e: e.tensor_scalar(out=Q(6), in0=Q(6), scalar1=1.0, scalar2=None, op0=ALU.add), r=["q_e2"], w=["q_e2"])
    P.op("dve", lambda e: e.reciprocal(out=Q(6), in_=Q(6)), r=["q_e2"], w=["q_e2"])
    P.op("dve", lambda e: e.tensor_tensor(out=Q(6), in0=Q(6), in1=Q(3), op=ALU.mult), r=["q_e2", "q_sg"], w=["q_e2"])
    P.op("dve", lambda e: e.tensor_tensor(out=R3(6), in0=R3(6), in1=R3(5), op=ALU.mult), r=["r_ee", "r_sel2"], w=["r_ee"])
    P.op("dve", lambda e: e.tensor_tensor(out=R3(6), in0=R3(6), in1=bq(6), op=ALU.mult), r=["r_ee", "q_e2"], w=["r_ee"])
    P.op("dve", lambda e: e.tensor_tensor(out=comb[:].rearrange("p t (g e) -> p t g e", e=4), in0=R3(0).unsqueeze(3).to_broadcast([128, NOWN, 4, 4]),
                                          in1=R3(6).unsqueeze(2).to_broadcast([128, NOWN, 4, 4]), op=ALU.mult),
         r=["r_ohg", "r_ee"], w=[("comb", i) for i in range(NOWN)])

    d_h1 = dbg_out("h1", [128, NOWN * D])
    d_comb = dbg_out("comb", [128, NOWN * 16])
    if dbg:
        for i in range(NOWN):
            P.dma("sp", d_h1[:, i * D:(i + 1) * D], h1[:, i, :], r=[("h1", i)], key=("dbgo", 16, i))
        P.dma("sp", d_comb, comb[:].rearrange("p a b -> p (a b)"), r=[("comb", i) for i in range(NOWN)], key=("dbgo", 17))
    if stop_after == "B2":
        P.finalize()
        nc.in_names = in_names
        return nc

    o = TB + 32 * KB
    M_sg = [view(o, [128, 512], BF16, [("Msg", 0)]), view(o + KB, [128, 512], BF16, [("Msg", 1)])]; o += 2 * KB
    M_hid = [view(o, [128, 4, 512], BF16, [("Mhid", 0)]), view(o + 4 * KB, [128, 4, 512], BF16, [("Mhid", 1)])]; o += 8 * KB
    first_c = True
    HN2T_ALL = [("hn2T", i) for i in range(NOWN)]
    gcount = 0
    ycount = 0
    for ex in range(16):
        eb = ex % 2
        Wg_, Wu_, Wd_ = Wexp[eb]
        if ex == 0:
            P.alias(("Wg", 0), ["Wo"])
            P.alias(("Wu", 0), ["Wo"])
            P.alias(("Wd", 0), ["Wo", "Wown"])
        if ex == 1:
            cts = [("catT", i) for i in range(NOWN)]
            P.alias(("Wg", 1), cts)
            P.alias(("Wu", 1), cts)
            P.alias(("Wd", 1), cts)
        P.dma("pool", Wg_, wg_d[ex].rearrange("(c p) n -> p c n", p=128), w=[("Wg", eb)])
        P.dma("pool", Wu_, wu_d[ex].rearrange("(c p) n -> p c n", p=128), w=[("Wu", eb)])
        P.dma("pool", Wd_, wd_d[ex].rearrange("(c p) n -> p c n", p=128), w=[("Wd", eb)])
        for grp in range(4):
            hb_ = (ex * 4 + grp) % 2
            hid = M_hid[hb_]
            if first_c:
                pass
                pass
                P.alias(("Msg", 0), [("xo", 0), ("xo", 1)])
                P.alias(("Msg", 1), [("xo", 0), ("xo", 1)])
                first_c = False
            rk = [("hn2T", 4 * grp + t_) for t_ in range(4)]
            for fc in range(4):
                gb_ = 0 if gcount % 2 == 0 else 2
                sgb = gcount % 2
                gcount += 1
                for dc in range(8):
                    P.op("pe", lambda e, dc=dc, fc=fc, grp=grp, gb_=gb_, Wg_=Wg_: e.matmul(bank(gb_), lhsT=Wg_[:, dc, fc * 128:(fc + 1) * 128],
                                                                                         rhs=hn2T[:, dc, grp * 512:(grp + 1) * 512], start=(dc == 0), stop=(dc == 7)),
                         r=[("Wg", eb)] + rk, w=bk(gb_))
                for dc in range(8):
                    P.op("pe", lambda e, dc=dc, fc=fc, grp=grp, gb_=gb_, Wu_=Wu_: e.matmul(bank(gb_ + 1), lhsT=Wu_[:, dc, fc * 128:(fc + 1) * 128],
                                                                                         rhs=hn2T[:, dc, grp * 512:(grp + 1) * 512], start=(dc == 0), stop=(dc == 7)),
                         r=[("Wu", eb)] + rk, w=bk(gb_ + 1))
                P.op("act", lambda e, gb_=gb_, sgb=sgb: e.activation(out=M_sg[sgb], in_=bank(gb_), func=AF.Silu), r=bk(gb_), w=[("Msg", sgb)])
                P.op("dve", lambda e, gb_=gb_, sgb=sgb, fc=fc, hid=hid: e.tensor_tensor(out=hid[:, fc, :], in0=bank(gb_ + 1), in1=M_sg[sgb], op=ALU.mult),
                     r=bk(gb_ + 1) + [("Msg", sgb)], w=[("Mhid", hb_)])
            for tl in range(4):
                tile = 4 * grp + tl
                for half in range(2):
                    yb = 4 + ycount % 4
                    ycount += 1
                    for fc in range(4):
                        P.op("pe", lambda e, fc=fc, tl=tl, half=half, yb=yb, hid=hid, Wd_=Wd_: e.matmul(bank(yb), lhsT=hid[:, fc, tl * 128:(tl + 1) * 128],
                                                                                                      rhs=Wd_[:, fc, half * 512:(half + 1) * 512], start=(fc == 0), stop=(fc == 3)),
                             r=[("Mhid", hb_), ("Wd", eb)], w=bk(yb))
                    P.op("dve", lambda e, tile=tile, half=half, yb=yb, ex=ex: e.scalar_tensor_tensor(
                        out=h1[:, tile, half * 512:(half + 1) * 512], in0=bank(yb), scalar=comb[:, tile, ex:ex + 1],
                        in1=h1[:, tile, half * 512:(half + 1) * 512], op0=ALU.mult, op1=ALU.add),
                        r=bk(yb) + [("comb", tile), ("h1", tile)], w=[("h1", tile)])

    d_h2 = dbg_out("h2", [128, NOWN * D])
    if dbg:
        for i in range(NOWN):
            P.dma("sp", d_h2[:, i * D:(i + 1) * D], h1[:, i, :], r=[("h1", i)], key=("dbgo", 18, i))

    P.dma("pool", Wpg, wpg_d.rearrange("(c p) n -> p c n", p=128), w=["Wpg"])
    P.dma("pool", Wpp, wpp_d.rearrange("(c p) n -> p c n", p=128), w=["Wpp"])
    o = TB + 20 * KB
    D_pT = [view(o, [128, 2, 128], BF16, [("pT", 0)]), view(o + 512, [128, 2, 128], BF16, [("pT", 1)])]; o += KB
    D_gple = view(o, [128, D], F32, ["gple"]); o += 4 * KB
    D_gfin = view(o, [128, D], F32, ["gfin"]); o += 4 * KB
    D_hn3 = [view(o, [128, D], BF16, [("hn3", 0)]), view(o + 2 * KB, [128, D], BF16, [("hn3", 1)])]; o += 4 * KB
    D_hn3T = [view(o, [128, 8, 128], BF16, [("hn3T", 0)]), view(o + 2 * KB, [128, 8, 128], BF16, [("hn3T", 1)])]; o += 4 * KB
    D_sgt = [view(o, [128, D], F32, [("sgt", 0, 0), ("sgt", 0, 1)]), view(o + 4 * KB, [128, D], F32, [("sgt", 1, 0), ("sgt", 1, 1)])]; o += 8 * KB
    D_junk = view(o, [128, D], BF16, ["Djunk"]); o += 2 * KB
    D_out = [view(o, [128, D], F32, [("Dout", 0)]), view(o + 4 * KB, [128, D], F32, [("Dout", 1)])]; o += 8 * KB
    D_s = view(o, [128, 8], F32, [("Drs", 0), ("Drs", 1), ("Drs2", 0), ("Drs2", 1)]); o += 32
    assert o <= ARENA
    P.dma("sp", D_gple, gple_d, w=["gple"])
    P.dma("sp", D_gfin, gfin_d, w=["gfin"])

    D_q = view(o, [128, 4, NOWN], F32, ["rstd3", "rstd4"] + [("ssq3", i_) for i_ in range(NOWN)] + [("ssq4", i_) for i_ in range(NOWN)]); o += 256
    assert o <= ARENA
    for i in range(NOWN):
        P.op("act", lambda e, i=i: e.activation(out=D_junk, in_=h1[:, i, :], func=AF.Square, accum_out=D_q[:, 0, i:i + 1]), r=[("h1", i)], w=["Djunk", ("ssq3", i)])
    P.op("act", lambda e: e.activation(out=D_q[:, 1, :], in_=D_q[:, 0, :], func=AF.Sqrt, scale=1.0 / D, bias=EPS), r=[("ssq3", i) for i in range(NOWN)], w=["rstd3"])
    P.op("dve", lambda e: e.reciprocal(out=D_q[:, 1, :], in_=D_q[:, 1, :]), r=["rstd3"], w=["rstd3"])

    def D_front(i):
        ib = i % 2
        gbk = 0 if ib == 0 else 4
        trb = 6 + ib
        P.dma("pool", D_pT[ib], pTo_d[:, i * 128:(i + 1) * 128].rearrange("(c p) t -> p c t", p=128), w=[("pT", ib)])
        P.op("dve", lambda e: e.scalar_tensor_tensor(out=D_hn3[ib], in0=h1[:, i, :], scalar=D_q[:, 1, i:i + 1], in1=D_gple, op0=ALU.mult, op1=ALU.mult),
             r=[("h1", i), "rstd3", "gple"], w=[("hn3", ib)])
        tc = bankb(trb).rearrange("p (k t) -> p k t", t=128)
        for kc in range(8):
            P.op("pe", lambda e, kc=kc: e.transpose(tc[:, kc, :], D_hn3[ib][:, kc * 128:(kc + 1) * 128], identb[:]), r=[("hn3", ib), "identb"], w=bk(trb))
        P.op("act", lambda e: e.activation(out=D_hn3T[ib], in_=tc, func=AF.Copy), r=bk(trb), w=[("hn3T", ib)])
        for half in range(2):
            for dc in range(8):
                P.op("pe", lambda e, dc=dc, half=half: e.matmul(bank(gbk + half), lhsT=D_hn3T[ib][:, dc, :], rhs=Wpg[:, dc, half * 512:(half + 1) * 512],
                                                              start=(dc == 0), stop=(dc == 7)), r=[("hn3T", ib), "Wpg"], w=bk(gbk + half))

    def D_back(i):
        ib = i % 2
        gbk = 0 if ib == 0 else 4
        sg = D_sgt[ib]
        for half in range(2):
            for kc in range(2):
                P.op("pe", lambda e, kc=kc, half=half: e.matmul(bank(2 + half), lhsT=D_pT[ib][:, kc, :], rhs=Wpp[:, kc, half * 512:(half + 1) * 512],
                                                              start=(kc == 0), stop=(kc == 1)), r=[("pT", ib), "Wpp"], w=bk(2 + half))
        for half in range(2):
            P.op("act", lambda e, half=half: e.activation(out=sg[:, half * 512:(half + 1) * 512], in_=bank(gbk + half), func=AF.Sigmoid), r=bk(gbk + half), w=[("sgt", ib, half)])
            P.op("dve", lambda e, half=half: e.tensor_tensor(out=sg[:, half * 512:(half + 1) * 512], in0=bank(2 + half), in1=sg[:, half * 512:(half + 1) * 512], op=ALU.mult),
                 r=bk(2 + half) + [("sgt", ib, half)], w=[("sgt", ib, half)])
        P.op("pool", lambda e: e.tensor_tensor(out=h1[:, i, :], in0=h1[:, i, :], in1=sg, op=ALU.add), r=[("h1", i), ("sgt", ib, 0), ("sgt", ib, 1)], w=[("h1", i)])
        P.op("act", lambda e: e.activation(out=D_junk, in_=h1[:, i, :], func=AF.Square, accum_out=D_q[:, 2, i:i + 1]), r=[("h1", i)], w=["Djunk", ("ssq4", i)])

    for i in range(NOWN):
        D_front(i)
        if i > 0:
            D_back(i - 1)
    D_back(NOWN - 1)
    P.op("act", lambda e: e.activation(out=D_q[:, 3, :], in_=D_q[:, 2, :], func=AF.Sqrt, scale=1.0 / D, bias=EPS), r=[("ssq4", i) for i in range(NOWN)], w=["rstd4"])
    P.op("dve", lambda e: e.reciprocal(out=D_q[:, 3, :], in_=D_q[:, 3, :]), r=["rstd4"], w=["rstd4"])
    for i in range(NOWN):
        ib = i % 2
        P.op("dve", lambda e, i=i, ib=ib: e.scalar_tensor_tensor(out=D_out[ib], in0=h1[:, i, :], scalar=D_q[:, 3, i:i + 1], in1=D_gfin, op0=ALU.mult, op1=ALU.mult),
             r=[("h1", i), "rstd4", "gfin"], w=[("Dout", ib)])
        P.dma("sp", out_d[i], D_out[ib], r=[("Dout", ib)], key=("outq", ib))

    P.finalize()
    nc.in_names = in_names
    return nc


def host_inputs(inp):
    f = np.float32
    x = np.asarray(inp["x"], f)
    p = np.asarray(inp["p"], f)[0]
    w_in = np.asarray(inp["w_in"], f)[0]
    HD = 64
    OFF_Q, OFF_KV, OFF_GATE = 1024, 1536, 2304
    kvc = [w_in[:, OFF_KV + j * 128: OFF_KV + (j + 1) * 128] for j in range(6)]
    wkv = np.concatenate([kvc[0], kvc[2], kvc[4], kvc[1], kvc[3], kvc[5]], axis=1)
    wq = w_in[:, OFF_Q:OFF_KV].reshape(D, 8, HD)
    wq_r = np.concatenate([np.concatenate([wq[:, j], wq[:, 4 + j]], axis=1) for j in range(4)], axis=1)
    wown = np.concatenate([w_in[:, 0:1024], wq_r, w_in[:, OFF_GATE:OFF_GATE + 24]], axis=1)
    shared = {
        "wkv": np.ascontiguousarray(wkv), "wown": np.ascontiguousarray(wown),
        "gmix": np.ascontiguousarray(np.asarray(inp["norm_mix"], f)[0].reshape(8, 128).T),
        "wsT": np.ascontiguousarray(np.asarray(inp["gmlp_w_s"], f)[0].transpose(2, 0, 1).reshape(128, 8 * 128)),
        "tri": np.triu(np.ones((128, 128), f)),
        "bsT": np.ascontiguousarray(np.asarray(inp["gmlp_b_s"], f)[0].T),
        "gv": np.ascontiguousarray(np.broadcast_to(np.asarray(inp["gmlp_v_norm"], f)[0][None, :], (128, 512))),
        "ga": np.ascontiguousarray(np.broadcast_to(np.asarray(inp["out_norm_a"], f)[0][None, :], (128, 512))),
        "gb": np.ascontiguousarray(np.broadcast_to(np.asarray(inp["out_norm_b"], f)[0][None, :], (128, 512))),
        "gmoe": np.ascontiguousarray(np.broadcast_to(np.asarray(inp["norm_moe"], f)[0][None, :], (128, D))),
        "gple": np.ascontiguousarray(np.broadcast_to(np.asarray(inp["norm_ple"], f)[0][None, :], (128, D))),
        "gfin": np.ascontiguousarray(np.broadcast_to(np.asarray(inp["norm_final"], f)[None, :], (128, D))),
        "w1k": np.ascontiguousarray(np.asarray(inp["cmp_w1_k"], f)[0].reshape(32, 64, 256).transpose(1, 0, 2).reshape(64, 32 * 256)),
        "w1v": np.ascontiguousarray(np.asarray(inp["cmp_w1_v"], f)[0].reshape(32, 64, 256).transpose(1, 0, 2).reshape(64, 32 * 256)),
        "peTk": np.ascontiguousarray(np.asarray(inp["cmp_pe_k"], f)[0].T),
        "peTv": np.ascontiguousarray(np.asarray(inp["cmp_pe_v"], f)[0].T),
        "w2k": np.ascontiguousarray(np.asarray(inp["cmp_w2_k"], f)[0]),
        "w2v": np.ascontiguousarray(np.asarray(inp["cmp_w2_v"], f)[0]),
        "wo": np.ascontiguousarray(np.asarray(inp["w_o"], f)[0]),
        "rcat": np.ascontiguousarray(np.concatenate([np.asarray(inp["router_group"], f)[0], np.asarray(inp["router_expert"], f)[0]], axis=1)),
        "moe_wg": np.ascontiguousarray(np.asarray(inp["moe_w_gate"], f)[0]),
        "moe_wu": np.ascontiguousarray(np.asarray(inp["moe_w_up"], f)[0]),
        "moe_wd": np.ascontiguousarray(np.asarray(inp["moe_w_down"], f)[0]),
        "wpp": np.ascontiguousarray(np.asarray(inp["w_ple_proj"], f)[0]),
        "wpg": np.ascontiguousarray(np.asarray(inp["w_ple_gate"], f)[0]),
        "identf": np.eye(128, dtype=f),
    }
    half = 32
    inv = (1.0 / (np.float32(10000.0) ** (np.arange(half, dtype=f) / np.float32(half)))).astype(f)
    ang = (np.arange(T, dtype=f)[:, None] * inv[None, :]).astype(f)
    cs = np.concatenate([np.cos(ang), np.sin(ang)], axis=1).astype(f)
    shared["cs"] = cs
    ew = np.zeros((128, 64, 128), f)
    for kt in range(64):
        ew[2 * kt, kt, 0:64] = 1.0
        ew[2 * kt + 1, kt, 64:128] = 1.0
    shared["ewide"] = ew.reshape(128, 64 * 128)
    cidx = np.arange(512)
    sidx = np.arange(128)
    ovl = ((cidx[:, None] * 16 < sidx[None, :] * 64 + 64) & (cidx[:, None] * 16 + 32 > sidx[None, :] * 64)).astype(f)
    ovl[511, :] = 0.0
    shared["ovl"] = ovl
    kk = np.arange(128)[:, None]
    tt = np.arange(128)[None, :]
    tri_m = (kk <= tt).astype(f)
    ntri_m = (kk > tt).astype(f)
    onesm = np.ones((128, 128), f)
    zerom = np.zeros((128, 128), f)

    maps = []
    for core in range(8):
        b, c = core // 4, core % 4
        own_tiles = [4 * i + c for i in range(NOWN)]
        tok = np.concatenate([np.arange(128 * qt, 128 * qt + 128) for qt in own_tiles])
        m = dict(shared)
        m["xT"] = np.ascontiguousarray(x[b].T)
        m["xo"] = np.ascontiguousarray(x[b][tok].reshape(NOWN, 128, D))
        m["xTo"] = np.ascontiguousarray(x[b][tok].reshape(NOWN, 128, D).transpose(0, 2, 1))
        m["pTo"] = np.ascontiguousarray(p[b][tok].T)
        m["cso"] = np.ascontiguousarray(cs[tok])
        dm = []
        for r in range(4):
            dm.append(onesm if r < c else (tri_m if r == c else zerom))
        for r in range(8):
            dlt = r - 4 - c
            dm.append(ntri_m if dlt == -4 else (onesm if -3 <= dlt <= -1 else (tri_m if dlt == 0 else zerom)))
        m["dmwm"] = np.ascontiguousarray(((np.stack(dm, axis=1) - 1.0) * 30000.0).astype(f).reshape(128, 12 * 128))
        mC = np.zeros((NOWN, 128, 2, 128), f)
        m12 = np.zeros((NOWN, 128, 256), f)
        for i, qt in enumerate(own_tiles):
            n_ct = (32 * i + 30) // 128 + 1
            t = 128 * qt + np.arange(128)
            for k in range(2):
                ct = n_ct - 2 + k
                if ct < 0:
                    continue
                cc = ct * 128 + np.arange(128)
                mC[i, :, k, :] = ((16 * cc[:, None] + 31) <= t[None, :]).astype(f)
            cur = (t // 64)[:, None]
            s = np.arange(128)[None, :]
            forced = (s == 0) | (s == cur) | (s == cur - 1)
            valid = (s * 64) <= t[:, None]
            m12[i, :, 0:128] = np.where(forced, 0.0, np.where(valid, 1.0, 0.0))
            m12[i, :, 128:256] = np.where(forced, FORCE, np.where(valid, 0.0, -FORCE))
        m["mC"] = np.ascontiguousarray(((mC - 1.0) * 30000.0).astype(f).reshape(NOWN, 128, 256))
        m["m12"] = m12
        maps.append(m)
    return maps


_NC_CACHE = {}


def kernel(**inputs):
    maps = host_inputs(inputs)
    if "nc" not in _NC_CACHE:
        _NC_CACHE["nc"] = build_program()
    nc = _NC_CACHE["nc"]
    res = run_bass_kernel_spmd(nc, maps, core_ids=list(range(8)))
    out = np.zeros((2, T, D), np.float32)
    for core in range(8):
        b, c = core // 4, core % 4
        o = np.asarray(res.results[core]["out"], np.float32)
        for i in range(NOWN):
            qt = 4 * i + c
            out[b, 128 * qt:128 * (qt + 1), :] = o[i]
    return out
```

```python
from contextlib import ExitStack
import types
import numpy as np
import concourse.bass as bass
import concourse.mybir as mybir
from concourse.bass_utils import run_bass_kernel_spmd

F32 = mybir.dt.float32
BF16 = mybir.dt.bfloat16
AF = mybir.ActivationFunctionType
ALU = mybir.AluOpType
AX = mybir.AxisListType

T = 8192
D = 1024
NT = 64
NOWN = 16
EPS = 1e-6
FORCE = 1e6
ENG = ("pe", "act", "dve", "pool", "sp")
SKIP = set()


def _freeze(fn):
    if fn.__closure__ is None:
        return fn
    cells = []
    for c in fn.__closure__:
        try:
            cells.append(types.CellType(c.cell_contents))
        except ValueError:
            cells.append(c)
    return types.FunctionType(fn.__code__, fn.__globals__, fn.__name__, fn.__defaults__, tuple(cells))


class Prog:
    def __init__(self, nc):
        self.nc = nc
        self.ops = []
        self.last_w = {}
        self.readers = {}
        self.stack = ExitStack()
        self.group_keys = set()
        self.pending_alias = {}
        self.ranges = {}
        self._ovl_cache = {}

    def sb(self, name, shape, dt):
        return self.stack.enter_context(self.nc.sbuf_tensor(name, list(shape), dt))

    def ps(self, name, shape, dt):
        return self.stack.enter_context(self.nc.psum_tensor(name, list(shape), dt))

    def reg(self, keys, lo, hi):
        for k in keys:
            if k in self.ranges:
                a, b = self.ranges[k]
                lo2, hi2 = min(a, lo), max(b, hi)
            else:
                lo2, hi2 = lo, hi
            self.ranges[k] = (lo2, hi2)
        self._ovl_cache = {}

    def overlaps(self, k):
        if k not in self.ranges:
            return ()
        if k not in self._ovl_cache:
            lo, hi = self.ranges[k]
            self._ovl_cache[k] = [k2 for k2, (a, b) in self.ranges.items() if k2 != k and a < hi and lo < b]
        return self._ovl_cache[k]

    def alias(self, newkey, oldkeys):
        self.pending_alias.setdefault(newkey, []).extend(oldkeys)

    @staticmethod
    def _is_psum(k):
        return isinstance(k, str) and len(k) == 2 and k[0] == "B" and k[1].isdigit()

    def op(self, eng, fn, r=(), w=(), dma_key=None):
        i = len(self.ops)
        deps = set()
        w = list(w)
        r = list(r)
        xw = [k for k in r if self._is_psum(k) and k not in w]
        extra = []
        for k in w:
            if k in self.pending_alias:
                extra.extend(self.pending_alias.pop(k))
            extra.extend(self.overlaps(k))
        for k in r:
            if k in self.last_w:
                deps.add(self.last_w[k])
        for k in w + extra + xw:
            if k in self.last_w:
                deps.add(self.last_w[k])
            for j in self.readers.get(k, ()):
                deps.add(j)
        deps.discard(i)
        self.ops.append(dict(eng=eng, fn=_freeze(fn), deps=deps, dma=dma_key, r=tuple(r), w=tuple(w)))
        for k in r:
            self.readers.setdefault(k, []).append(i)
        for k in w:
            self.last_w[k] = i
            self.readers[k] = []
        for k in xw:
            self.last_w[k] = i
            self.readers[k] = []
        return i

    def dma(self, eng, out, in_, r=(), w=(), key=None, group=False, **kw):
        if key is None:
            key = w[0] if len(w) else r[0]
        if group:
            self.group_keys.add(key)
        return self.op(eng, lambda e: e.dma_start(out=out, in_=in_, **kw), r=r, w=w, dma_key=key)

    def finalize(self, final_wait_eng="sp"):
        nc = self.nc
        ops = self.ops
        n = len(ops)
        needed = [False] * n
        for i, o in enumerate(ops):
            keep = set()
            for d in o["deps"]:
                od = ops[d]
                if od["dma"] is None and o["dma"] is None and od["eng"] == o["eng"]:
                    if o["eng"] == "pe":
                        continue
                    if not (set(od["w"]) & (set(o["r"]) | set(o["w"]))):
                        continue
                keep.add(d)
            o["deps"] = keep
            for d in keep:
                needed[d] = True
        final_dma = [i for i, o in enumerate(ops) if o["dma"] is not None and not needed[i]]
        for i in final_dma:
            needed[i] = True
        sems = {}
        cnt = {}
        names = {}

        def sem_name(key):
            if key not in names:
                names[key] = "d%d" % len(names)
            return names[key]

        for i, o in enumerate(ops):
            if not needed[i]:
                o["sig"] = None
                continue
            if o["dma"] is not None:
                sname = sem_name(o["dma"])
                inc = 16
            else:
                sname = "e_" + o["eng"]
                inc = 1
            cnt[sname] = cnt.get(sname, 0) + inc
            o["sig"] = (sname, inc, cnt[sname])
            if sname not in sems:
                sems[sname] = self.stack.enter_context(nc.semaphore(sname))
        gfinal = {sem_name(k): cnt.get(sem_name(k), 0) for k in self.group_keys}
        self.n_sems = len(sems)
        plan = {e: [] for e in ENG}
        waited = {e: {} for e in ENG}
        for i, o in enumerate(ops):
            e = o["eng"]
            waits = {}
            for d in o["deps"]:
                sname, inc, val = ops[d]["sig"]
                if sname in gfinal:
                    val = gfinal[sname]
                if waited[e].get(sname, 0) >= val:
                    continue
                waits[sname] = max(waits.get(sname, 0), val)
            for sname, val in waits.items():
                waited[e][sname] = val
            plan[e].append((o, waits))
        final_waits = {}
        for i in final_dma:
            sname, inc, val = ops[i]["sig"]
            final_waits[sname] = max(final_waits.get(sname, 0), val)
        blk = self.stack.enter_context(nc.Block())

        def emit(engobj, ename):
            for o, waits in plan[ename]:
                for sname, val in waits.items():
                    engobj.wait_ge(sems[sname], val)
                ins = o["fn"](engobj)
                if o["sig"] is not None:
                    ins.then_inc(sems[o["sig"][0]], o["sig"][1])
            if ename == final_wait_eng:
                for sname, val in final_waits.items():
                    engobj.wait_ge(sems[sname], val)

        @blk.tensor
        def _(e):
            emit(e, "pe")

        @blk.scalar
        def _(e):
            emit(e, "act")

        @blk.vector
        def _(e):
            emit(e, "dve")

        @blk.gpsimd
        def _(e):
            emit(e, "pool")

        @blk.sync
        def _(e):
            emit(e, "sp")

        self.stack.close()
        return nc


def build_program(dbg=False, stop_after=None):
    nc = bass.Bass("TRN2", target_bir_lowering=False)
    P = Prog(nc)

    in_names = []
    late = stop_after in (None, "B2")

    def din(name, shape, big=False):
        if big and not late:
            shape = [1] * len(shape)
        in_names.append(name)
        return nc.dram_tensor(name, list(shape), F32, kind="ExternalInput").ap()

    def dout(name, shape):
        return nc.dram_tensor(name, list(shape), F32, kind="ExternalOutput").ap()

    xT_d = din("xT", [D, T])
    xTo_d = din("xTo", [NOWN, D, 128])
    xo_d = din("xo", [NOWN, 128, D], big=True)
    pTo_d = din("pTo", [256, NOWN * 128], big=True)
    cs_d = din("cs", [T, 64])
    cso_d = din("cso", [NOWN * 128, 64])
    wkv_d = din("wkv", [D, 768])
    wown_d = din("wown", [D, 1560])
    gmix_d = din("gmix", [128, 8])
    wsT_d = din("wsT", [128, 8 * 128])
    tri_d = din("tri", [128, 128])
    bsT_d = din("bsT", [128, 8])
    gv_d = din("gv", [128, 512])
    ga_d = din("ga", [128, 512])
    gb_d = din("gb", [128, 512])
    gmoe_d = din("gmoe", [128, D], big=True)
    gple_d = din("gple", [128, D], big=True)
    gfin_d = din("gfin", [128, D], big=True)
    w1_d = [din("w1k", [64, 32 * 256]), din("w1v", [64, 32 * 256])]
    peT_d = [din("peTk", [64, 32]), din("peTv", [64, 32])]
    w2_d = [din("w2k", [256, 64]), din("w2v", [256, 64])]
    wo_d = din("wo", [D, D], big=True)
    rcat_d = din("rcat", [D, 20])
    wg_d = din("moe_wg", [16, D, 512], big=True)
    wu_d = din("moe_wu", [16, D, 512], big=True)
    wd_d = din("moe_wd", [16, 512, D], big=True)
    wpp_d = din("wpp", [256, D], big=True)
    wpg_d = din("wpg", [D, D], big=True)
    identf_d = din("identf", [128, 128])
    ewide_d = din("ewide", [128, 64 * 128])
    ovl_d = din("ovl", [512, 128])
    mC_d = din("mC", [NOWN, 128, 2 * 128])
    m12_d = din("m12", [NOWN, 128, 256])
    dmwm_d = din("dmwm", [128, 12 * 128])
    out_d = dout("out", [NOWN, 128, D])
    dbg_d = {}

    def dbg_out(name, shape):
        if dbg:
            dbg_d[name] = dout("dbg_" + name, shape)
        return dbg_d.get(name)

    KB = 1024
    ARENA = 192 * KB
    arena = P.sb("arena", [128, ARENA // 2], BF16)

    def view(off, shape, dt, keys=None):
        esz = 2 if dt == BF16 else 4
        nel = int(np.prod(shape[1:]))
        assert off % 4 == 0 and off + nel * esz <= ARENA, (off, shape)
        if keys is not None:
            P.reg(keys, off, off + nel * esz)
        a = arena[:, off // 2: off // 2 + nel * esz // 2]
        if dt != BF16:
            a = a.bitcast(dt)
        if len(shape) == 2:
            return a
        nm = " ".join("d%d" % i for i in range(1, len(shape)))
        kw = {"d%d" % i: shape[i] for i in range(2, len(shape))}
        return a.rearrange("p (%s) -> p %s" % (nm, nm), **kw)

    KVc = view(0, [128, 2, T], BF16, ["KVc"])
    Wown = view(0, [128, 8, 1560], BF16, ["Wown"])
    Wo = view(0, [128, 8, D], BF16, ["Wo"])
    Wkv = view(130 * KB + 48 * KB, [128, 8, 768], BF16, ["Wkv"])
    W1 = [view(32 * KB, [128, 32, 256], BF16, [("W1", 0)]), view(48 * KB, [128, 32, 256], BF16, [("W1", 1)])]
    catT = view(32 * KB, [128, 8, NOWN * 128], BF16, [("catT", i_) for i_ in range(NOWN)])
    KVs = view(64 * KB, [128, 2, T], BF16, ["KVs"])
    VTM_B = 64 * 2 * 65 * 2
    Vtm = [view(96 * KB, [128, 64, 2, 65], BF16, ["Vtm", "Vtm_one0"]), view(96 * KB + VTM_B, [128, 64, 2, 65], BF16, ["Vtm", "Vtm_one1"])]
    h1 = view(64 * KB, [128, NOWN, D], F32)
    for i_ in range(NOWN):
        P.reg([("h1", i_)], 64 * KB + i_ * 4096, 64 * KB + (i_ + 1) * 4096)
    h1 = h1
    TB = 130 * KB
    hn2T = view(130 * KB, [128, 8, NOWN * 128], BF16, [("hn2T", i_) for i_ in range(NOWN)])
    Wexp = [(view(0, [128, 8, 512], BF16, [("Wg", 0)]), view(8 * KB, [128, 8, 512], BF16, [("Wu", 0)]), view(16 * KB, [128, 4, D], BF16, [("Wd", 0)])),
            (view(32 * KB, [128, 8, 512], BF16, [("Wg", 1)]), view(40 * KB, [128, 8, 512], BF16, [("Wu", 1)]), view(48 * KB, [128, 4, D], BF16, [("Wd", 1)]))]
    Wpg = view(130 * KB, [128, 8, D], BF16, ["Wpg"])
    Wpp = view(146 * KB, [128, 2, D], BF16, ["Wpp"])

    identb = P.sb("identb", [128, 128], BF16)
    identf = P.sb("identf_s", [128, 128], F32)
    ones = P.sb("ones", [128, 16], BF16)
    trib = P.sb("trib", [128, 128], BF16)
    wsT = P.sb("wsT_s", [128, 8, 128], BF16)
    bsT = P.sb("bsT_s", [128, 8], F32)
    gmix = P.sb("gmix_s", [128, 8], F32)
    gv = P.sb("gv_s", [128, 512], F32)
    ga = P.sb("ga_s", [128, 512], F32)
    gb = P.sb("gb_s", [128, 512], F32)
    kcT = P.sb("kcT", [128, 512], BF16)
    vcx = P.sb("vcx", [128, 4, 2, 194], BF16)
    w2 = [P.sb("w2k_s", [128, 2, 64], BF16), P.sb("w2v_s", [128, 2, 64], BF16)]
    peT = [P.sb("peTk_s", [64, 32], BF16), P.sb("peTv_s", [64, 32], BF16)]
    cbias = P.sb("cbias", [128, 4], F32)
    rcat = P.sb("rcat_s", [128, 8, 20], F32)
    comb = P.sb("comb", [128, NOWN, 16], F32)
    pad8 = P.sb("pad8", [128, 8], F32)

    PS = P.ps("PS", [128, 4096], F32)

    def bank(k, lo=0, hi=512):
        return PS[:, k * 512 + lo: k * 512 + hi]

    def bankb(k):
        return PS[:, k * 512:(k + 1) * 512].bitcast(BF16)

    def bk(k, slots=None):
        return ["B%d" % k]

    def bc_mid(ap2d, n):
        return ap2d.unsqueeze(1).to_broadcast([128, n, ap2d.shape[-1]])

    def bc_last(ap2d, n):
        return ap2d.unsqueeze(2).to_broadcast([128, ap2d.shape[-1], n])

    def cload(eng, dst, src, key):
        P.dma(eng, dst, src, w=[key], key="const_" + eng, group=True)

    cload("pool", identb[:], identf_d, "identb")
    cload("sp", identf[:], identf_d, "identf")
    cload("pool", trib[:], tri_d, "trib")
    cload("pool", wsT[:], wsT_d.rearrange("p (g t) -> p g t", t=128), "wsT")
    cload("sp", bsT[:], bsT_d, "bsT")
    cload("sp", gmix[:], gmix_d, "gmix")
    cload("sp", gv[:], gv_d, "gv")
    cload("sp", ga[:], ga_d, "ga")
    cload("sp", gb[:], gb_d, "gb")
    for kv in range(2):
        cload("pool", w2[kv][:], w2_d[kv].rearrange("(h p) d -> p h d", p=128), "w2%d" % kv)
        cload("pool", peT[kv][:], peT_d[kv], "peT%d" % kv)
    cload("sp", rcat[:], rcat_d.rearrange("(c p) n -> p c n", p=128), "rcat")
    P.op("pool", lambda e: e.memset(ones[:], 1.0), w=["ones"])
    P.op("pool", lambda e: e.memset(pad8[:], -1e30), w=["pad8"])
    P.op("pool", lambda e: e.memset(vcx[:].rearrange("p a g d -> p (a g d)"), 1.0), w=["vcx_one"])
    P.op("pool", lambda e: e.memset(Vtm[0].rearrange("p a g d -> p (a g d)"), 1.0), w=["Vtm_one0"])
    P.op("pool", lambda e: e.memset(Vtm[1].rearrange("p a g d -> p (a g d)"), 1.0), w=["Vtm_one1"])
    for g in range(2):
        for ct in range(4):
            P.dma("pool", vcx[:, ct, g, 66:194], ovl_d[ct * 128:(ct + 1) * 128, :], r=["vcx_one"], w=["vcx_ovl"], key="vcx_ovl")
    if "wst" not in SKIP:
        P.op("dve", lambda e: e.tensor_tensor(out=wsT[:], in0=wsT[:], in1=bc_mid(trib[:, :], 8), op=ALU.mult),
             r=["wsT", "trib"], w=["wsT"])

    P.dma("pool", Wkv, wkv_d.rearrange("(c p) n -> p c n", p=128), w=["Wkv"])
    for dc in range(8 if "wkvs" not in SKIP else 0):
        P.op("dve", lambda e, dc=dc: e.tensor_scalar(out=Wkv[:, dc, :], in0=Wkv[:, dc, :], scalar1=gmix[:, dc:dc + 1],
                                                      scalar2=None, op0=ALU.mult), r=["Wkv", "gmix"], w=["Wkv"])

    if stop_after == "const":
        P.finalize()
        nc.in_names = in_names
        return nc

    def rstd_from_ssq(ssq_ap, n, dst, rkeys, wkey):
        P.op("act", lambda e: e.activation(out=dst, in_=ssq_ap, func=AF.Sqrt, scale=1.0 / n, bias=EPS), r=rkeys, w=[wkey])
        P.op("dve", lambda e: e.reciprocal(out=dst, in_=dst), r=[wkey], w=[wkey])

    def rope_tm(src_ps, nblk, csr, dst_bf, tmp, rkeys, tkey, wkey):
        s4 = src_ps.rearrange("p (b h d) -> p b h d", h=2, d=32)
        d4 = dst_bf.rearrange("p (b h d) -> p b h d", h=2, d=32)
        cr = csr[:, 0:32].unsqueeze(1).to_broadcast([128, nblk, 32])
        sr = csr[:, 32:64].unsqueeze(1).to_broadcast([128, nblk, 32])
        tv = [tmp[:, k, :].rearrange("p (b d) -> p b d", d=32) for k in range(4)]
        P.op("dve", lambda e: e.tensor_tensor(out=tv[0], in0=s4[:, :, 0, :], in1=cr, op=ALU.mult), r=rkeys, w=[tkey + "0"])
        P.op("dve", lambda e: e.tensor_tensor(out=tv[1], in0=s4[:, :, 1, :], in1=sr, op=ALU.mult), r=rkeys, w=[tkey + "1"])
        P.op("dve", lambda e: e.tensor_tensor(out=tv[2], in0=s4[:, :, 0, :], in1=sr, op=ALU.mult), r=rkeys, w=[tkey + "2"])
        P.op("dve", lambda e: e.tensor_tensor(out=tv[3], in0=s4[:, :, 1, :], in1=cr, op=ALU.mult), r=rkeys, w=[tkey + "3"])
        P.op("pool", lambda e: e.tensor_tensor(out=d4[:, :, 0, :], in0=tv[0], in1=tv[1], op=ALU.subtract),
             r=[tkey + "0", tkey + "1"], w=[wkey + "a"])
        P.op("pool", lambda e: e.tensor_tensor(out=d4[:, :, 1, :], in0=tv[2], in1=tv[3], op=ALU.add),
             r=[tkey + "2", tkey + "3"], w=[wkey + "b"])

    A_xTb = [view(TB + 0, [128, 8, 256], BF16, [("xTb", 0)]), view(TB + 4 * KB, [128, 8, 256], BF16, [("xTb", 1)])]
    A_xf = [view(TB + 32 * KB, [128, 8, 256], F32, [("xf", 0)]), view(TB + 40 * KB, [128, 8, 256], F32, [("xf", 1)])]
    A_cs = [view(TB + 16 * KB, [128, 2, 64], F32, [("Acs", 0)]), view(TB + 17 * KB, [128, 2, 64], F32, [("Acs", 1)])]
    A_sq = [view(TB + 18 * KB, [128, 8, 128], BF16, [("Asq", 0)]), view(TB + 20 * KB, [128, 8, 128], BF16, [("Asq", 1)])]
    A_tmp = [view(TB + 22 * KB, [128, 4, 192], F32, ["Atmp0%d" % k_ for k_ in range(4)]), view(TB + 25 * KB, [128, 4, 192], F32, ["Atmp1%d" % k_ for k_ in range(4)])]
    A_ktm = [view(TB + 28 * KB, [128, 512], BF16, ["Aktm0a", "Aktm0b", "Aktm0v"]), view(TB + 29 * KB, [128, 512], BF16, ["Aktm1a", "Aktm1b", "Aktm1v"])]
    A_csr = [view(TB + 30 * KB, [128, 64], F32, [("Acsr", 0)]), view(TB + 30 * KB + 256, [128, 64], F32, [("Acsr", 1)])]
    A_rs = view(TB + 31 * KB, [128, 8], F32, [("Ars", 0), ("Ars", 1)])
    A_u = [view(TB + 8 * KB, [128, 384], F32, [("Au", 0, 0), ("Au", 0, 1)]), view(TB + 10 * KB, [128, 384], F32, [("Au", 1, 0), ("Au", 1, 1)])]

    for kv in range(2):
        for half in range(2):
            P.dma("pool", W1[kv][half * 64:(half + 1) * 64], w1_d[kv].rearrange("p (l h) -> p l h", h=256), w=[("W1", kv)])
    n_chunks = 32
    TPC = 2

    def A_front(ck, tl):
        cb = ck % 2
        tile = ck * TPC + tl
        tb = tile % 2
        xs = A_xTb[cb][:, :, tl * 128:(tl + 1) * 128]
        P.op("act", lambda e: e.activation(out=A_sq[tb], in_=xs, func=AF.Square), r=[("xTb", cb)], w=[("Asq", tb)])
        zb = 2 * (tile % 3)
        for dc in range(8):
            P.op("pe", lambda e, dc=dc: e.matmul(bank(zb + 1, 300, 301), lhsT=A_sq[tb][:, dc, :], rhs=ones[:, 0:1],
                                                 start=(dc == 0), stop=(dc == 7)), r=[("Asq", tb), "ones"], w=bk(zb + 1))
        for dc in range(8):
            P.op("pe", lambda e, dc=dc: e.matmul(bank(zb), lhsT=xs[:, dc, :], rhs=Wkv[:, dc, 0:512],
                                                 start=(dc == 0), stop=(dc == 7)), r=[("xTb", cb), "Wkv"], w=bk(zb))
        for dc in range(8):
            P.op("pe", lambda e, dc=dc: e.matmul(bank(zb + 1, 0, 256), lhsT=xs[:, dc, :], rhs=Wkv[:, dc, 512:768],
                                                 start=(dc == 0), stop=(dc == 7)), r=[("xTb", cb), "Wkv"], w=bk(zb + 1))

    def A_back(ck, tl):
        cb = ck % 2
        tile = ck * TPC + tl
        tb = tile % 2
        zb = 2 * (tile % 3)
        rs = A_rs[:, tb:tb + 1]
        s4 = bank(zb, 0, 384).rearrange("p (b h d) -> p b h d", h=2, d=32)
        cr = A_cs[cb][:, tl, 0:32].unsqueeze(1).to_broadcast([128, 6, 32])
        sr = A_cs[cb][:, tl, 32:64].unsqueeze(1).to_broadcast([128, 6, 32])
        tv = [A_tmp[tb][:, k, :].rearrange("p (b d) -> p b d", d=32) for k in range(4)]
        tk = "Atmp%d" % tb
        P.op("dve", lambda e: e.tensor_tensor(out=tv[0], in0=s4[:, :, 0, :], in1=cr, op=ALU.mult), r=bk(zb) + [("Acs", cb)], w=[tk + "0"])
        P.op("dve", lambda e: e.tensor_tensor(out=tv[1], in0=s4[:, :, 1, :], in1=sr, op=ALU.mult), r=bk(zb) + [("Acs", cb)], w=[tk + "1"])
        P.op("dve", lambda e: e.tensor_tensor(out=tv[2], in0=s4[:, :, 0, :], in1=sr, op=ALU.mult), r=bk(zb) + [("Acs", cb)], w=[tk + "2"])
        P.op("dve", lambda e: e.tensor_tensor(out=tv[3], in0=s4[:, :, 1, :], in1=cr, op=ALU.mult), r=bk(zb) + [("Acs", cb)], w=[tk + "3"])
        u4 = A_u[tb].rearrange("p (b h d) -> p b h d", h=2, d=32)
        P.op("pool", lambda e: e.tensor_tensor(out=u4[:, :, 0, :], in0=tv[0], in1=tv[1], op=ALU.subtract), r=[tk + "0", tk + "1"], w=[("Au", tb, 0)])
        P.op("dve", lambda e: e.tensor_tensor(out=u4[:, :, 1, :], in0=tv[2], in1=tv[3], op=ALU.add), r=[tk + "2", tk + "3"], w=[("Au", tb, 1)])
        rstd_from_ssq(bank(zb + 1, 300, 301), D, rs, bk(zb + 1), ("Ars", tb))
        P.op("act", lambda e: e.activation(out=A_ktm[tb][:, 0:384], in_=A_u[tb], func=AF.Copy, scale=rs),
             r=[("Au", tb, 0), ("Au", tb, 1), ("Ars", tb)], w=["Aktm%da" % tb, "Aktm%db" % tb])
        P.op("act", lambda e: e.activation(out=A_ktm[tb][:, 384:512], in_=bank(zb, 384, 512), func=AF.Copy, scale=rs),
             r=bk(zb) + [("Ars", tb)], w=["Aktm%dv" % tb])
        for j in range(2):
            P.op("act", lambda e, j=j: e.activation(
                out=Vtm[j][:, tile, :, 0:64], in_=bank(zb + 1, j * 128, (j + 1) * 128).rearrange("p (g d) -> p g d", d=64),
                func=AF.Copy, scale=rs), r=bk(zb + 1) + [("Ars", tb)], w=["Vtm"])
        tp = bankb(6 + tb)[:, 0:512].rearrange("p (a t) -> p a t", t=128)
        src_order = [0, 3, 1, 2]
        for a_, sblk in enumerate(src_order):
            P.op("pe", lambda e, a_=a_, sblk=sblk: e.transpose(tp[:, a_, :], A_ktm[tb][:, sblk * 128:(sblk + 1) * 128], identb[:]),
                 r=["Aktm%da" % tb, "Aktm%db" % tb, "Aktm%dv" % tb, "identb"], w=bk(6 + tb))
        P.op("dve", lambda e: e.tensor_copy(out=KVc[:, :, tile * 128:(tile + 1) * 128], in_=tp[:, 0:2, :]),
             r=bk(6 + tb), w=["KVc"])
        P.op("act", lambda e: e.activation(out=KVs[:, :, tile * 128:(tile + 1) * 128], in_=tp[:, 2:4, :], func=AF.Copy),
             r=bk(6 + tb), w=["KVs"])

    pend = []
    for ck in range(n_chunks):
        cb = ck % 2
        CT = 128 * TPC
        P.dma("sp", A_xf[cb], xT_d[:, ck * CT:(ck + 1) * CT].rearrange("(c p) t -> p c t", p=128), w=[("xf", cb)])
        P.dma("sp", A_cs[cb], cs_d[ck * CT:(ck + 1) * CT, :].rearrange("(a p) n -> p a n", p=128), w=[("Acs", cb)])
        P.op("dve", lambda e: e.tensor_copy(out=A_xTb[cb], in_=A_xf[cb]), r=[("xf", cb)], w=[("xTb", cb)])
        for tl in range(TPC):
            A_front(ck, tl)
            pend.append((ck, tl))
            if len(pend) > 2:
                A_back(*pend.pop(0))
    while pend:
        A_back(*pend.pop(0))

    if dbg:
        d_kvs = dbg_out("KVs", [128, 2 * T])
        d_kvc = dbg_out("KVc", [128, 2 * T])
        d_vtm = dbg_out("Vtm0", [128, 64 * 130])
        stg = view(TB + 32 * KB, [128, T // 2], F32, ["stg"])
        HT = T // 2
        for hh in range(2):
            for qq in range(2):
                P.op("dve", lambda e, hh=hh, qq=qq: e.tensor_copy(out=stg, in_=KVs[:, hh, qq * HT:(qq + 1) * HT]), r=["KVs"], w=["stg"])
                P.dma("sp", d_kvs[:, hh * T + qq * HT:hh * T + (qq + 1) * HT], stg, r=["stg"], key=("dbgo", 1))
                P.op("dve", lambda e, hh=hh, qq=qq: e.tensor_copy(out=stg, in_=KVc[:, hh, qq * HT:(qq + 1) * HT]), r=["KVc"], w=["stg"])
                P.dma("sp", d_kvc[:, hh * T + qq * HT:hh * T + (qq + 1) * HT], stg, r=["stg"], key=("dbgo", 2))
        for qq in range(4):
            P.op("dve", lambda e, qq=qq: e.tensor_copy(out=stg[:, 0:16 * 130], in_=Vtm[0][:, qq * 16:(qq + 1) * 16].rearrange("p a g d -> p (a g d)")), r=["Vtm", "Vtm_one0"], w=["stg"])
            P.dma("sp", d_vtm[:, qq * 16 * 130:(qq + 1) * 16 * 130], stg[:, 0:16 * 130], r=["stg"], key=("dbgo", 3))
    if stop_after == "A":
        P.finalize()
        nc.in_names = in_names
        return nc

    for kv in range(2):
        for half in range(2):
            col = kv * 2 + half
            for l in range(32):
                P.op("pe", lambda e, kv=kv, half=half, l=l, col=col: e.matmul(
                    bank(4, 8 + col, 9 + col), lhsT=W1[kv][0:64, l, half * 128:(half + 1) * 128], rhs=peT[kv][:, l:l + 1],
                    start=(l == 0), stop=(l == 31)), r=[("W1", kv), "peT%d" % kv], w=bk(4))
    P.op("dve", lambda e: e.tensor_copy(out=cbias[:], in_=bank(4, 8, 12)), r=bk(4), w=["cbias"])
    Ap_hid = [view(TB + 0, [128, 2, 512], BF16, ["hid0"]), view(TB + 2 * KB, [128, 2, 512], BF16, ["hid1"])]
    KVd = [view(TB + 12 * KB, [128, 16, 512], BF16, [("KVd", 0)]), view(TB + 28 * KB, [128, 16, 512], BF16, [("KVd", 1)])]
    P.op("dve", lambda e: e.tensor_copy(out=KVd[0], in_=KVc[:, 0, :].rearrange("p (m b) -> p b m", b=16)), r=["KVc"], w=[("KVd", 0)])
    P.op("pool", lambda e: e.tensor_copy(out=KVd[1], in_=KVc[:, 1, :].rearrange("p (m b) -> p b m", b=16)), r=["KVc"], w=[("KVd", 1)])
    P.alias("hid0", [("xTb", 0)])
    P.alias("hid1", [("xTb", 0)])
    P.op("pool", lambda e: e.memset(Ap_hid[0], 0.0), w=["hid0"])
    P.op("pool", lambda e: e.memset(Ap_hid[1], 0.0), w=["hid1"])
    it = 0
    for kv in range(2):
        for g in range(2):
            hb = it % 2
            it += 1
            hid = Ap_hid[hb]
            for half in range(2):
                hbank = half
                for l in range(32):
                    P.op("pe", lambda e, kv=kv, g=g, half=half, l=l, hbank=hbank: e.matmul(
                        bank(hbank, 0, 511), lhsT=W1[kv][g * 64:(g + 1) * 64, l, half * 128:(half + 1) * 128],
                        rhs=KVd[kv][g * 64:(g + 1) * 64, l % 16, (l // 16):(l // 16) + 511], start=(l == 0), stop=(l == 31)),
                        r=[("W1", kv), ("KVd", kv)], w=bk(hbank))
                P.op("act", lambda e, kv=kv, half=half, hbank=hbank, hid=hid: e.activation(
                    out=hid[:, half, 0:511], in_=bank(hbank, 0, 511), func=AF.Gelu_apprx_tanh, bias=cbias[:, kv * 2 + half:kv * 2 + half + 1]),
                    r=bk(hbank) + ["cbias"], w=["hid%d" % hb])
            if kv == 0:
                for half in range(2):
                    P.op("pe", lambda e, g=g, half=half, hid=hid: e.matmul(PS[g * 64:(g + 1) * 64, 2 * 512:3 * 512], lhsT=w2[0][:, half, :],
                                                                          rhs=hid[:, half, :], start=(half == 0), stop=(half == 1)),
                         r=["hid%d" % hb, "w20"], w=bk(2))
                P.op("dve", lambda e, g=g: e.tensor_copy(out=kcT[g * 64:(g + 1) * 64, :], in_=PS[g * 64:(g + 1) * 64, 2 * 512:3 * 512]),
                     r=bk(2), w=["kcT"])
            else:
                for ct in range(4):
                    for half in range(2):
                        P.op("pe", lambda e, ct=ct, half=half, hid=hid: e.matmul(bank(3, ct * 64, (ct + 1) * 64), lhsT=hid[:, half, ct * 128:(ct + 1) * 128],
                                                                                rhs=w2[1][:, half, :], start=(half == 0), stop=(half == 1)),
                             r=["hid%d" % hb, "w21"], w=bk(3))
                P.op("dve", lambda e, g=g: e.tensor_copy(out=vcx[:, :, g, 0:64], in_=bank(3, 0, 256).rearrange("p (c d) -> p c d", d=64)),
                     r=bk(3) + ["vcx_one"], w=["vcx_v%d" % g])
    VCX = ["vcx_ovl", "vcx_one", "vcx_v0", "vcx_v1"]

    if dbg:
        d_kct = dbg_out("kcT", [128, 512])
        d_vcx = dbg_out("vcx", [128, 4 * 2 * 194])
        stg = view(TB + 32 * KB, [128, 4 * 2 * 194], F32, ["stg"])
        P.op("dve", lambda e: e.tensor_copy(out=stg[:, 0:512], in_=kcT[:]), r=["kcT"], w=["stg"])
        P.dma("sp", d_kct, stg[:, 0:512], r=["stg"], key=("dbgo", 4))
        P.op("dve", lambda e: e.tensor_copy(out=stg, in_=vcx[:].rearrange("p a g d -> p (a g d)")), r=VCX, w=["stg"])
        P.dma("sp", d_vcx, stg, r=["stg"], key=("dbgo", 5))
    if stop_after == "Ap":
        P.finalize()
        nc.in_names = in_names
        return nc

    P.alias("Wown", ["KVc"])
    P.dma("pool", Wown, wown_d.rearrange("(c p) n -> p c n", p=128), w=["Wown"])
    for dc in range(8):
        P.op("dve", lambda e, dc=dc: e.tensor_scalar(out=Wown[:, dc, :], in0=Wown[:, dc, :], scalar1=gmix[:, dc:dc + 1],
                                                      scalar2=None, op0=ALU.mult), r=["Wown", "gmix"], w=["Wown"])
    Ewide = view(TB + 0, [128, 64, 128], BF16, ["Ewide"])
    P.alias("Ewide", [("xTb", 0), ("xTb", 1), "hid0", "hid1"])
    P.dma("pool", Ewide, ewide_d.rearrange("p (k q) -> p k q", q=128), w=["Ewide"])
    o = TB + 16 * KB
    B_xTo = [view(o, [128, 8, 128], BF16, [("xTo", 0)]), view(o + 2 * KB, [128, 8, 128], BF16, [("xTo", 1)])]; o += 4 * KB
    B_sq = view(o, [128, 8, 128], BF16, ["Bsq"]); o += 2 * KB
    B_cso = [view(o, [128, 64], F32, [("cso", 0)]), view(o + 256, [128, 64], F32, [("cso", 1)])]; o += 512
    B_mC = [view(o, [128, 2, 128], BF16, [("mC", 0)]), view(o + 512, [128, 2, 128], BF16, [("mC", 1)])]; o += 1 * KB
    B_m12 = [view(o, [128, 256], F32, [("m12", 0)]), view(o + KB, [128, 256], F32, [("m12", 1)])]; o += 2 * KB
    B_Ocs = view(o, [128, 4 * 194], F32, ["Ocs"])
    B_uv = view(o, [128, 1024], F32, [("uv", 0), ("uv", 1)]); o += 4 * KB
    B_gates = view(o, [128, 24], F32, ["gates"]); o += 128
    B_csr = view(o, [128, 64], F32, ["Bcsr"]); o += 256
    B_OTs = view(o, [128, 512], F32, ["OTs"])
    B_tmp = view(o, [128, 4, 256], F32, ["Btmp%d" % k_ for k_ in range(4)]); o += 4 * KB
    B_qtm = view(o, [128, 512], BF16, ["Bqtma", "Bqtmb"]); o += KB
    B_QZ = [view(o, [128, 512], BF16, [("QZ", 0)]), view(o + KB, [128, 512], BF16, [("QZ", 1)])]; o += 2 * KB
    B_vn = view(o, [128, 512], BF16, ["vn"]); o += KB
    B_oa = view(o, [128, 512], F32, ["oa"]); o += 2 * KB
    B_junk = view(o, [128, 1024], BF16, ["junk"]); o += 2 * KB
    B_cat = view(o, [128, 1024], BF16, ["cat_a", "cat_b"]); o += 2 * KB
    B_Pt = [view(o + k * KB, [128, 512], BF16, [("Pt", k)]) for k in range(3)]; o += 3 * KB
    B_imp = view(o, [128, 128], F32, ["imp"]); o += 512
    B_score = view(o, [128, 128], F32, ["score"]); o += 512
    B_wk = view(o, [128, 128], F32, ["wk"]); o += 512
    B_m8 = view(o, [128, 16], F32, ["m8a", "m8b"]); o += 64
    B_sel = view(o, [128, 128], BF16, ["sel"]); o += 256
    B_selT = view(o, [128, 4, 128], BF16, ["selT"]); o += KB
    B_rd = view(o, [128, 16], F32, ["rdc", "rds", "rdw"]); o += 64
    B_fac = view(o, [128, 16], F32, ["fac"]); o += 64
    B_ob = view(o, [128, 512], F32, [("ob", 0), ("ob", 1)]); o += 2 * KB
    B_t2 = view(o, [128, 256], F32, ["t2"]); o += KB
    B_rs = view(o, [128, 8], F32, ["Brs", "Brsv", "Brsa", "Brsb"]); o += 32
    dmwm = view(o, [128, 12, 128], BF16, ["dmwm"]); o += 3 * KB
    P.dma("pool", dmwm, dmwm_d.rearrange("p (r q) -> p r q", q=128), w=["dmwm"])
    P.op("pool", lambda e: e.memset(B_QZ[0], 0.0), w=[("QZ", 0)])
    P.op("pool", lambda e: e.memset(B_QZ[1], 0.0), w=[("QZ", 1)])
    assert o <= ARENA, o

    d_oa = dbg_out("oa", [128, 512])
    d_ob = dbg_out("ob", [128, 512])
    d_score = dbg_out("score", [128, 256])
    d_imp = dbg_out("imp", [128, 256])
    d_oc = dbg_out("Oc", [128, 2 * 776])
    d_os = dbg_out("Os", [128, 1024])
    d_ow = dbg_out("Ow", [128, 1024])
    d_gates = dbg_out("gates", [128, 24])
    DBG_TILE = 1

    pcount = [0]
    scount = [0]

    LA = 3
    SBANKS = [0, 1, 2, 7]
    pipe = []

    def pipe_step(s1, later):
        s1()
        pipe.append(later)
        if len(pipe) > LA:
            pipe.pop(0)()

    def pipe_flush():
        while pipe:
            pipe.pop(0)()

    def nsa_group(i, g):
        Qg = B_QZ[g]
        mb = i % 2
        OcK = bk(3) + bk(4)
        OsK = bk(5)
        OwK = bk(6)
        Oc = PS[:, 3 * 512:5 * 512].rearrange("p (j x) -> p j x", x=256)
        Os = bank(5).rearrange("p (j x) -> p j x", x=128)
        Ow = bank(6).rearrange("p (j x) -> p j x", x=128)
        Ocs = B_Ocs.rearrange("p (j x) -> p j x", x=194)

        def score_step(lhsT, lkeys, masks):
            sbk = SBANKS[scount[0] % len(SBANKS)]
            scount[0] += 1

            def s1():
                P.op("pe", lambda e: e.matmul(bank(sbk), lhsT=lhsT, rhs=Qg, start=True, stop=(len(masks) == 0), skip_group_check=True),
                     r=lkeys + [("QZ", g)], w=bk(sbk))
                for mi_, (ml, mr, mk) in enumerate(masks):
                    if mr.shape[-1] == 512:
                        last = (mi_ == len(masks) - 1)
                        P.op("pe", lambda e, ml=ml, mr=mr, last=last: e.matmul(bank(sbk), lhsT=ml, rhs=mr, start=False, stop=last, skip_group_check=True),
                             r=mk, w=bk(sbk))
                        continue
                    for j in range(4):
                        last = (mi_ == len(masks) - 1) and j == 3
                        P.op("pe", lambda e, ml=ml, mr=mr, j=j, last=last: e.matmul(bank(sbk, j * 128, (j + 1) * 128), lhsT=ml, rhs=mr,
                                                                                  start=False, stop=last, skip_group_check=True),
                             r=mk, w=bk(sbk))
            return sbk, s1

        def exp_pv(sbk, rhs_of_j, rkeys, outs, okeys, first, lastf, post=None):
            def later():
                pb = pcount[0] % 3
                pcount[0] += 1
                P.op("act", lambda e: e.activation(out=B_Pt[pb], in_=bank(sbk), func=AF.Exp), r=bk(sbk), w=[("Pt", pb)])
                for j in range(4):
                    P.op("pe", lambda e, j=j: e.matmul(outs[j], lhsT=B_Pt[pb][:, j * 128:(j + 1) * 128], rhs=rhs_of_j,
                                                       start=(first and j in okeys[1]), stop=lastf, skip_group_check=True),
                         r=[("Pt", pb)] + rkeys, w=okeys[0])
                if post is not None:
                    post()
            return later

        def exp_pvT(sbk, vext, rkeys, obank, okey, first, lastf, post=None):
            def later():
                pb = pcount[0] % 3
                pcount[0] += 1
                P.op("act", lambda e: e.activation(out=B_Pt[pb], in_=bank(sbk), func=AF.Exp), r=bk(sbk), w=[("Pt", pb)])
                P.op("pe", lambda e: e.matmul(PS[0:65, obank * 512:(obank + 1) * 512], lhsT=vext, rhs=B_Pt[pb], start=first, stop=lastf, skip_group_check=True),
                     r=[("Pt", pb)] + rkeys, w=okey)
                if post is not None:
                    post()
            return later

        def untranspose(obank, okey):
            P.op("act", lambda e: e.activation(out=B_OTs[0:65, :], in_=PS[0:65, obank * 512:(obank + 1) * 512], func=AF.Copy), r=okey, w=["OTs"])
            for j in range(4):
                P.op("pe", lambda e, j=j: e.transpose(bank(obank, j * 128, j * 128 + 65), B_OTs[0:65, j * 128:(j + 1) * 128], identf[0:65, 0:65]),
                     r=["OTs", "identf"], w=okey)

        n_ct = (32 * i + 30) // 128 + 1
        oc_outs = [PS[:, 3 * 512 + j * 256: 3 * 512 + j * 256 + 194] for j in range(4)]

        def post_cmp():
            P.op("act", lambda e: e.activation(out=Ocs, in_=Oc[:, :, 0:194], func=AF.Copy), r=OcK, w=["Ocs"])
            P.op("dve", lambda e: e.tensor_scalar(out=B_rd[:, 0:4], in0=Ocs[:, :, 64], scalar1=1e-30, scalar2=None, op0=ALU.max), r=["Ocs"], w=["rdc"])
            P.op("dve", lambda e: e.reciprocal(out=B_rd[:, 0:4], in_=B_rd[:, 0:4]), r=["rdc"], w=["rdc"])
            P.op("dve", lambda e: e.tensor_scalar(out=B_imp, in0=Ocs[:, 0, 66:194], scalar1=B_rd[:, 0:1], scalar2=None, op0=ALU.mult), r=["Ocs", "rdc"], w=["imp"])
            for j in range(1, 4):
                P.op("dve", lambda e, j=j: e.scalar_tensor_tensor(out=B_imp, in0=Ocs[:, j, 66:194], scalar=B_rd[:, j:j + 1], in1=B_imp, op0=ALU.mult, op1=ALU.add),
                     r=["Ocs", "rdc", "imp"], w=["imp"])
            P.op("dve", lambda e: e.tensor_tensor(out=B_score, in0=B_imp, in1=B_m12[mb][:, 0:128], op=ALU.mult), r=["imp", ("m12", mb)], w=["score"])
            P.op("dve", lambda e: e.tensor_tensor(out=B_score, in0=B_score, in1=B_m12[mb][:, 128:256], op=ALU.add), r=["score", ("m12", mb)], w=["score"])
            if dbg and i == DBG_TILE:
                P.dma("sp", d_score[:, g * 128:(g + 1) * 128], B_score, r=["score"], key=("dbgo", 200 + g))
                P.dma("sp", d_imp[:, g * 128:(g + 1) * 128], B_imp, r=["imp"], key=("dbgo", 202 + g))
                P.dma("sp", d_oc[:, g * 776:(g + 1) * 776], B_Ocs, r=["Ocs"], key=("dbgo", 204 + g))
            P.op("dve", lambda e: e.max(out=B_m8[:, 0:8], in_=B_score), r=["score"], w=["m8a"])
            P.op("dve", lambda e: e.match_replace(out=B_wk, in_to_replace=B_m8[:, 0:8], in_values=B_score, imm_value=-1e30), r=["score", "m8a"], w=["wk"])
            P.op("dve", lambda e: e.max(out=B_m8[:, 8:16], in_=B_wk), r=["wk"], w=["m8b"])
            P.op("dve", lambda e: e.tensor_scalar(out=B_sel, in0=B_score, scalar1=B_m8[:, 15:16], scalar2=None, op0=ALU.is_ge), r=["score", "m8b"], w=["sel"])
            P.op("dve", lambda e: e.tensor_scalar(out=B_sel, in0=B_sel, scalar1=-1.0, scalar2=30000.0, op0=ALU.add, op1=ALU.mult), r=["sel"], w=["sel"])
            tpv = bankb(3)[:, 0:128]
            P.op("pe", lambda e: e.transpose(tpv, B_sel, identb[:]), r=["sel", "identb"], w=bk(3))
            P.op("dve", lambda e: e.tensor_copy(out=B_selT, in_=bc_mid(tpv, 4)), r=bk(3), w=["selT"])

        for ct in range(n_ct):
            masks = []
            if ct >= n_ct - 2:
                mi = ct - (n_ct - 2)
                masks.append((identb[:], B_mC[mb][:, mi, :], ["identb", ("mC", mb)]))
            sbk, s1 = score_step(kcT[:, ct * 128:(ct + 1) * 128], ["kcT"], masks)
            pipe_step(s1, exp_pv(sbk, vcx[:, ct, g, :], VCX, oc_outs, (OcK, (0, 2)), ct == 0, ct == n_ct - 1,
                                 post=(post_cmp if ct == n_ct - 1 else None)))

        ow_outs = [bank(6, j * 128, j * 128 + 65) for j in range(4)]
        rlist = [r_ for r_ in range(8) if 4 * i - 4 + r_ >= 0]
        for r_ in rlist:
            kt = 4 * i - 4 + r_
            masks = [(identb[:], dmwm[:, 4 + r_, :], ["identb", "dmwm"])]
            sbk, s1 = score_step(KVs[:, 1, kt * 128:(kt + 1) * 128], ["KVs"], masks)
            pipe_step(s1, exp_pv(sbk, Vtm[1][:, kt, g, :], ["Vtm", "Vtm_one1"], ow_outs, (OwK, (0,)), r_ == rlist[0], r_ == rlist[-1]))

        os_outs = [bank(5, j * 128, j * 128 + 65) for j in range(4)]
        n_kt = 4 * i + 4

        def post_group():
            if dbg and i == DBG_TILE:
                stg = view(TB + 58 * KB, [128, 1024], F32, ["stg2"])
                P.op("dve", lambda e: e.memset(stg, 0.0), w=["stg2"])
                P.op("dve", lambda e: e.tensor_copy(out=stg[:, 0:512].rearrange("p (j x) -> p j x", x=128)[:, :, 0:65], in_=Os[:, :, 0:65]), r=OsK + ["stg2"], w=["stg2"])
                P.dma("sp", d_os[:, g * 512:(g + 1) * 512], stg[:, 0:512], r=["stg2"], key=("dbgo", 102 + g))
                P.op("dve", lambda e: e.tensor_copy(out=stg[:, 0:512].rearrange("p (j x) -> p j x", x=128)[:, :, 0:65], in_=Ow[:, :, 0:65]), r=OwK, w=["stg2"])
                P.dma("sp", d_ow[:, g * 512:(g + 1) * 512], stg[:, 0:512], r=["stg2"], key=("dbgo", 104 + g))
            P.op("dve", lambda e: e.reciprocal(out=B_rd[:, 4:8], in_=Os[:, :, 64]), r=OsK, w=["rds"])
            P.op("dve", lambda e: e.reciprocal(out=B_rd[:, 8:12], in_=Ow[:, :, 64]), r=OwK, w=["rdw"])
            gsl = B_gates[:, g * 12:(g + 1) * 12].rearrange("p (h b) -> p b h", b=3)
            P.op("dve", lambda e: e.tensor_tensor(out=B_fac[:, 0:12].rearrange("p (b h) -> p b h", h=4), in0=B_rd[:, 0:12].rearrange("p (b h) -> p b h", h=4),
                                                  in1=gsl, op=ALU.mult), r=["rdc", "rds", "rdw", "gates"], w=["fac"])
            obg = B_ob[:, g * 256:(g + 1) * 256].rearrange("p (j d) -> p j d", d=64)
            t2 = B_t2.rearrange("p (j d) -> p j d", d=64)
            P.op("pool", lambda e: e.tensor_tensor(out=obg, in0=Ocs[:, :, 0:64], in1=bc_last(B_fac[:, 0:4], 64), op=ALU.mult), r=["Ocs", "fac"], w=[("ob", g)])
            P.op("dve", lambda e: e.tensor_tensor(out=t2, in0=Os[:, :, 0:64], in1=bc_last(B_fac[:, 4:8], 64), op=ALU.mult), r=OsK + ["fac"], w=["t2"])
            P.op("pool", lambda e: e.tensor_tensor(out=obg, in0=obg, in1=t2, op=ALU.add), r=[("ob", g), "t2"], w=[("ob", g)])
            P.op("dve", lambda e: e.tensor_tensor(out=t2, in0=Ow[:, :, 0:64], in1=bc_last(B_fac[:, 8:12], 64), op=ALU.mult), r=OwK + ["fac"], w=["t2"])
            P.op("pool", lambda e: e.tensor_tensor(out=obg, in0=obg, in1=t2, op=ALU.add), r=[("ob", g), "t2"], w=[("ob", g)])

        for kt in range(n_kt):
            masks = [(Ewide[:, kt, :], B_selT.rearrange("p j q -> p (j q)"), ["Ewide", "selT"])]
            if kt >= 4 * i:
                masks.append((identb[:], dmwm[:, kt - 4 * i, :], ["identb", "dmwm"]))
            sbk, s1 = score_step(KVs[:, 0, kt * 128:(kt + 1) * 128], ["KVs"], masks)

            def post_sel():
                untranspose(5, OsK)
                post_group()
            pipe_step(s1, exp_pv(sbk, Vtm[0][:, kt, g, :], ["Vtm", "Vtm_one0"], os_outs, (OsK, (0,)), kt == 0, kt == n_kt - 1,
                                 post=(post_group if kt == n_kt - 1 else None)))

    n_own = NOWN if stop_after not in ("B1x",) else 2
    for i in range(n_own):
        ib = i % 2
        P.dma("pool", B_xTo[ib], xTo_d[i].rearrange("(c p) t -> p c t", p=128), w=[("xTo", ib)])
        P.dma("sp", B_cso[ib], cso_d[i * 128:(i + 1) * 128, :], w=[("cso", ib)])
        P.dma("pool", B_mC[ib], mC_d[i].rearrange("p (k q) -> p k q", q=128), w=[("mC", ib)])
        P.dma("sp", B_m12[ib], m12_d[i], w=[("m12", ib)])
        xs = B_xTo[ib]
        P.op("act", lambda e, xs=xs: e.activation(out=B_sq, in_=xs, func=AF.Square), r=[("xTo", ib)], w=["Bsq"])
        for dc in range(8):
            P.op("pe", lambda e, dc=dc: e.matmul(bank(6, 0, 1), lhsT=B_sq[:, dc, :], rhs=ones[:, 0:1], start=(dc == 0), stop=(dc == 7)),
                 r=["Bsq", "ones"], w=bk(6))
        for half in range(2):
            for dc in range(8):
                P.op("pe", lambda e, dc=dc, half=half, xs=xs: e.matmul(bank(half), lhsT=xs[:, dc, :], rhs=Wown[:, dc, half * 512:(half + 1) * 512],
                                                                     start=(dc == 0), stop=(dc == 7)), r=[("xTo", ib), "Wown"], w=bk(half))
        for dc in range(8):
            P.op("pe", lambda e, dc=dc, xs=xs: e.matmul(bank(2), lhsT=xs[:, dc, :], rhs=Wown[:, dc, 1024:1536], start=(dc == 0), stop=(dc == 7)),
                 r=[("xTo", ib), "Wown"], w=bk(2))
        for dc in range(8):
            P.op("pe", lambda e, dc=dc, xs=xs: e.matmul(bank(5, 0, 24), lhsT=xs[:, dc, :], rhs=Wown[:, dc, 1536:1560], start=(dc == 0), stop=(dc == 7)),
                 r=[("xTo", ib), "Wown"], w=bk(5))
        rs = B_rs[:, 0:1]
        rstd_from_ssq(bank(6, 0, 1), D, rs, bk(6), "Brs")
        for half in range(2):
            P.op("act", lambda e, half=half: e.activation(out=B_uv[:, half * 512:(half + 1) * 512], in_=bank(half), func=AF.Gelu_apprx_tanh, scale=rs),
                 r=bk(half) + ["Brs"], w=[("uv", half)])
        P.op("act", lambda e: e.activation(out=B_gates, in_=bank(5, 0, 24), func=AF.Sigmoid, scale=rs), r=bk(5) + ["Brs"], w=["gates"])
        P.op("dve", lambda e, ib=ib: e.tensor_scalar(out=B_csr, in0=B_cso[ib], scalar1=rs, scalar2=0.125, op0=ALU.mult, op1=ALU.mult),
             r=[("cso", ib), "Brs"], w=["Bcsr"])
        rope_tm(bank(2), 8, B_csr, B_qtm, B_tmp, bk(2) + ["Bcsr"], "Btmp", "Bqtm")
        tq = bankb(7)[:, 0:512].rearrange("p (j t) -> p j t", t=128)
        for j in range(4):
            P.op("pe", lambda e, j=j: e.transpose(tq[:, j, :], B_qtm[:, j * 128:(j + 1) * 128], identb[:]), r=["Bqtma", "Bqtmb", "identb"], w=bk(7, (0, 1)))
        for gq in range(2):
            P.op("act", lambda e, gq=gq: e.activation(out=B_QZ[gq][gq * 64:(gq + 1) * 64].rearrange("p (j q) -> p j q", q=128),
                                                      in_=tq[gq * 64:(gq + 1) * 64], func=AF.Copy), r=bk(7), w=[("QZ", gq)])
        P.op("act", lambda e: e.activation(out=B_junk[:, 0:512], in_=B_uv[:, 512:1024], func=AF.Square, accum_out=B_rs[:, 1:2]), r=[("uv", 1)], w=["junk", "Brsv"])
        rstd_from_ssq(B_rs[:, 1:2], 512, B_rs[:, 1:2], ["Brsv"], "Brsv")
        P.op("dve", lambda e: e.scalar_tensor_tensor(out=B_vn, in0=B_uv[:, 512:1024], scalar=B_rs[:, 1:2], in1=gv[:], op0=ALU.mult, op1=ALU.mult),
             r=[("uv", 1), "Brsv", "gv"], w=["vn"])
        for g8 in range(8):
            P.op("pe", lambda e, g8=g8: e.matmul(bank(2, g8 * 64, (g8 + 1) * 64), lhsT=wsT[:, g8, :], rhs=B_vn[:, g8 * 64:(g8 + 1) * 64], start=True, stop=True),
                 r=["wsT", "vn"], w=bk(2))
        oa3 = B_oa.rearrange("p (g d) -> p g d", d=64)
        P.op("dve", lambda e: e.tensor_tensor(out=oa3, in0=bank(2).rearrange("p (g d) -> p g d", d=64), in1=bc_last(bsT[:, :], 64), op=ALU.add),
             r=bk(2) + ["bsT"], w=["oa"])
        P.op("dve", lambda e: e.tensor_tensor(out=B_oa, in0=B_oa, in1=B_uv[:, 0:512], op=ALU.mult), r=["oa", ("uv", 0)], w=["oa"])
        P.op("act", lambda e: e.activation(out=B_junk[:, 0:512], in_=B_oa, func=AF.Square, accum_out=B_rs[:, 2:3]), r=["oa"], w=["junk", "Brsa"])
        rstd_from_ssq(B_rs[:, 2:3], 512, B_rs[:, 2:3], ["Brsa"], "Brsa")
        P.op("dve", lambda e: e.scalar_tensor_tensor(out=B_cat[:, 0:512], in0=B_oa, scalar=B_rs[:, 2:3], in1=ga[:], op0=ALU.mult, op1=ALU.mult),
             r=["oa", "Brsa", "ga"], w=["cat_a"])
        if dbg and i == DBG_TILE:
            stg = view(TB + 58 * KB, [128, 1024], F32, ["stg2"])
            P.dma("sp", d_oa, B_oa, r=["oa"], key=("dbgo", 12))
            P.dma("sp", d_gates, B_gates, r=["gates"], key=("dbgo", 13))
        for g in range(2):
            nsa_group(i, g)
        pipe_flush()
        if dbg and i == DBG_TILE:
            P.dma("sp", d_ob, B_ob, r=[("ob", 0), ("ob", 1)], key=("dbgo", 14))
        P.op("act", lambda e: e.activation(out=B_junk[:, 0:512], in_=B_ob, func=AF.Square, accum_out=B_rs[:, 3:4]), r=[("ob", 0), ("ob", 1)], w=["junk", "Brsb"])
        rstd_from_ssq(B_rs[:, 3:4], 512, B_rs[:, 3:4], ["Brsb"], "Brsb")
        P.op("dve", lambda e: e.scalar_tensor_tensor(out=B_cat[:, 512:1024], in0=B_ob, scalar=B_rs[:, 3:4], in1=gb[:], op0=ALU.mult, op1=ALU.mult),
             r=[("ob", 0), ("ob", 1), "Brsb", "gb"], w=["cat_b"])
        tc = bankb(7).rearrange("p (k t) -> p k t", t=128)
        for kc in range(8):
            P.op("pe", lambda e, kc=kc: e.transpose(tc[:, kc, :], B_cat[:, kc * 128:(kc + 1) * 128], identb[:]), r=["cat_a", "cat_b", "identb"], w=bk(7))
        if i == 0:
            P.alias(("catT", 0), [("W1", 0), ("W1", 1)])
        P.op("act", lambda e, i=i: e.activation(out=catT[:, :, i * 128:(i + 1) * 128], in_=tc, func=AF.Copy), r=bk(7), w=[("catT", i)])

    if stop_after in ("B1", "B1x"):
        if dbg:
            d_catT = dbg_out("catT", [128, 8 * NOWN * 128])
            for kc in range(8):
                stg = view(TB + 58 * KB, [128, 1024], F32, ["stg2"])
                for hh in range(2):
                    P.op("dve", lambda e, kc=kc, hh=hh: e.tensor_copy(out=stg, in_=catT[:, kc, hh * 1024:(hh + 1) * 1024]),
                         r=[("catT", i) for i in range(n_own)], w=["stg2"])
                    P.dma("sp", d_catT[:, kc * 2048 + hh * 1024: kc * 2048 + (hh + 1) * 1024], stg, r=["stg2"], key=("dbgo", 15))
        P.finalize()
        nc.in_names = in_names
        return nc

    P.dma("pool", Wo, wo_d.rearrange("(c p) n -> p c n", p=128), w=["Wo"])
    C_gmoe = view(16 * KB, [128, D], F32, ["gmoe"])
    C_hn2 = [view(20 * KB, [128, D], F32, [("hn2", 0)]), view(24 * KB, [128, D], F32, [("hn2", 1)])]
    o = TB + 32 * KB
    C_xo = [view(o, [128, D], F32, [("xo", 0)]), view(o + 4 * KB, [128, D], F32, [("xo", 1)])]; o += 8 * KB
    C_hn2Tf = [view(o, [128, 8, 128], F32, [("hn2Tf", 0)]), view(o + 4 * KB, [128, 8, 128], F32, [("hn2Tf", 1)])]; o += 8 * KB
    C_junk = view(o, [128, D], BF16, ["Cjunk"]); o += 2 * KB
    RK2 = ["q_mg", "q_sg", "q_l1", "q_l2", "q_e2", "rstd2"] + [("ssq2", i_) for i_ in range(NOWN)]
    C_lgall = view(o, [128, NOWN, 20], F32, [("lg", i_) for i_ in range(NOWN)]); o += 1280
    C_r = view(o, [128, 12, NOWN * 4], F32, ["r_ohg", "r_dg", "r_les", "r_eq1", "r_x2", "r_sel2", "r_ee"]); o += 3072
    C_q = view(o, [128, 12, NOWN], F32, RK2); o += 768
    C_t44 = view(o, [128, NOWN * 16], F32, ["t44"]); o += 1024
    assert o <= ARENA
    P.dma("sp", C_gmoe, gmoe_d, w=["gmoe"])


    def B2_front(i):
        ib = i % 2
        hb = 0 if i % 2 == 0 else 2
        P.dma("sp", C_xo[ib], xo_d[i], w=[("xo", ib)])
        for half in range(2):
            for kc in range(8):
                P.op("pe", lambda e, kc=kc, half=half: e.matmul(bank(hb + half), lhsT=catT[:, kc, i * 128:(i + 1) * 128], rhs=Wo[:, kc, half * 512:(half + 1) * 512],
                                                              start=(kc == 0), stop=(kc == 7)), r=[("catT", i), "Wo"], w=bk(hb + half))

    def B2_back(i):
        ib = i % 2
        hb = 0 if i % 2 == 0 else 2
        for half in range(2):
            P.op("dve", lambda e, half=half: e.tensor_tensor(out=h1[:, i, half * 512:(half + 1) * 512], in0=bank(hb + half),
                                                             in1=C_xo[ib][:, half * 512:(half + 1) * 512], op=ALU.add),
                 r=bk(hb + half) + [("xo", ib)], w=[("h1", i)])
        P.op("act", lambda e: e.activation(out=C_junk, in_=h1[:, i, :], func=AF.Square, accum_out=C_q[:, 0, i:i + 1]), r=[("h1", i)], w=["Cjunk", ("ssq2", i)])

    for i in range(NOWN):
        B2_front(i)
        if i > 0:
            B2_back(i - 1)
    B2_back(NOWN - 1)
    SS2 = [("ssq2", i) for i in range(NOWN)]
    P.op("act", lambda e: e.activation(out=C_q[:, 1, :], in_=C_q[:, 0, :], func=AF.Sqrt, scale=1.0 / D, bias=EPS), r=SS2, w=["rstd2"])
    P.op("dve", lambda e: e.reciprocal(out=C_q[:, 1, :], in_=C_q[:, 1, :]), r=["rstd2"], w=["rstd2"])

    def B2c_front(i):
        ib = i % 2
        P.op("dve", lambda e: e.scalar_tensor_tensor(out=C_hn2[ib], in0=h1[:, i, :], scalar=C_q[:, 1, i:i + 1], in1=C_gmoe, op0=ALU.mult, op1=ALU.mult),
             r=[("h1", i), "rstd2", "gmoe"], w=[("hn2", ib)])

    def B2c_back(i):
        ib = i % 2
        tb_ = 4 if i % 2 == 0 else 6
        tf = PS[:, tb_ * 512:(tb_ + 2) * 512].rearrange("p (c t) -> p c t", t=128)
        for dc in range(8):
            P.op("pe", lambda e, dc=dc: e.transpose(tf[:, dc, :], C_hn2[ib][:, dc * 128:(dc + 1) * 128], identf[:]), r=[("hn2", ib), "identf"], w=bk(tb_) + bk(tb_ + 1))
        P.op("act", lambda e: e.activation(out=C_hn2Tf[ib], in_=tf, func=AF.Copy), r=bk(tb_) + bk(tb_ + 1), w=[("hn2Tf", ib)])
        P.op("dve", lambda e: e.tensor_copy(out=hn2T[:, :, i * 128:(i + 1) * 128], in_=tf), r=bk(tb_) + bk(tb_ + 1), w=[("hn2T", i)])
        for dc in range(8):
            P.op("pe", lambda e, dc=dc: e.matmul(bank(tb_, 0, 20), lhsT=C_hn2Tf[ib][:, dc, :], rhs=rcat[:, dc, :], start=(dc == 0), stop=(dc == 7)),
                 r=[("hn2Tf", ib), "rcat"], w=bk(tb_))
        P.op("act", lambda e: e.activation(out=C_lgall[:, i, :], in_=bank(tb_, 0, 20), func=AF.Copy), r=bk(tb_), w=[("lg", i)])

    for i in range(NOWN):
        B2c_front(i)
        if i > 0:
            B2c_back(i - 1)
    B2c_back(NOWN - 1)

    LGA = [("lg", i) for i in range(NOWN)]
    G3 = C_lgall[:, :, 0:4]
    E4 = C_lgall[:, :, 4:20].rearrange("p t (g e) -> p t g e", e=4)
    R3 = lambda k: C_r[:, k, :].rearrange("p (t x) -> p t x", x=4)
    Q = lambda k: C_q[:, k, :]
    bq = lambda k: C_q[:, k, :].unsqueeze(2).to_broadcast([128, NOWN, 4])
    P.op("dve", lambda e: e.tensor_reduce(out=Q(2), in_=G3, axis=AX.X, op=ALU.max), r=LGA, w=["q_mg"])
    P.op("dve", lambda e: e.tensor_tensor(out=R3(0), in0=G3, in1=bq(2), op=ALU.is_equal), r=LGA + ["q_mg"], w=["r_ohg"])
    P.op("dve", lambda e: e.tensor_tensor(out=R3(1), in0=G3, in1=bq(2), op=ALU.subtract), r=LGA + ["q_mg"], w=["r_dg"])
    P.op("act", lambda e: e.activation(out=R3(1), in_=R3(1), func=AF.Exp), r=["r_dg"], w=["r_dg"])
    P.op("dve", lambda e: e.tensor_reduce(out=Q(3), in_=R3(1), axis=AX.X, op=ALU.add), r=["r_dg"], w=["q_sg"])
    P.op("dve", lambda e: e.reciprocal(out=Q(3), in_=Q(3)), r=["q_sg"], w=["q_sg"])
    t44 = C_t44.rearrange("p (t g e) -> p t g e", g=4, e=4)
    P.op("dve", lambda e: e.tensor_tensor(out=t44, in0=E4, in1=R3(0).unsqueeze(3).to_broadcast([128, NOWN, 4, 4]), op=ALU.mult), r=LGA + ["r_ohg"], w=["t44"])
    P.op("dve", lambda e: e.tensor_reduce(out=R3(2), in_=C_t44.rearrange("p (t g e) -> p t e g", g=4, e=4), axis=AX.X, op=ALU.add), r=["t44"], w=["r_les"])
    P.op("dve", lambda e: e.tensor_reduce(out=Q(4), in_=R3(2), axis=AX.X, op=ALU.max), r=["r_les"], w=["q_l1"])
    P.op("dve", lambda e: e.tensor_tensor(out=R3(3), in0=R3(2), in1=bq(4), op=ALU.is_equal), r=["r_les", "q_l1"], w=["r_eq1"])
    P.op("dve", lambda e: e.scalar_tensor_tensor(out=C_r[:, 4, :], in0=C_r[:, 3, :], scalar=-1e30, in1=C_r[:, 2, :], op0=ALU.mult, op1=ALU.add), r=["r_eq1", "r_les"], w=["r_x2"])
    P.op("dve", lambda e: e.tensor_reduce(out=Q(5), in_=R3(4), axis=AX.X, op=ALU.max), r=["r_x2"], w=["q_l2"])
    P.op("dve", lambda e: e.tensor_tensor(out=R3(5), in0=R3(2), in1=bq(5), op=ALU.is_ge), r=["r_les", "q_l2"], w=["r_sel2"])
    P.op("dve", lambda e: e.tensor_tensor(out=R3(6), in0=R3(2), in1=bq(4), op=ALU.subtract), r=["r_les", "q_l1"], w=["r_ee"])
    P.op("act", lambda e: e.activation(out=R3(6), in_=R3(6), func=AF.Exp), r=["r_ee"], w=["r_ee"])
    P.op("dve", lambda e: e.tensor_tensor(out=Q(6), in0=Q(5), in1=Q(4), op=ALU.subtract), r=["q_l2", "q_l1"], w=["q_e2"])
    P.op("act", lambda e: e.activation(out=Q(6), in_=Q(6), func=AF.Exp), r=["q_e2"], w=["q_e2"])
    P.op("dve", lambda e: e.tensor_scalar(out=Q(6), in0=Q(6), scalar1=1.0, scalar2=None, op0=ALU.add), r=["q_e2"], w=["q_e2"])
    P.op("dve", lambda e: e.reciprocal(out=Q(6), in_=Q(6)), r=["q_e2"], w=["q_e2"])
    P.op("dve", lambda e: e.tensor_tensor(out=Q(6), in0=Q(6), in1=Q(3), op=ALU.mult), r=["q_e2", "q_sg"], w=["q_e2"])
    P.op("dve", lambda e: e.tensor_tensor(out=R3(6), in0=R3(6), in1=R3(5), op=ALU.mult), r=["r_ee", "r_sel2"], w=["r_ee"])
    P.op("dve", lambda e: e.tensor_tensor(out=R3(6), in0=R3(6), in1=bq(6), op=ALU.mult), r=["r_ee", "q_e2"], w=["r_ee"])
    P.op("dve", lambda e: e.tensor_tensor(out=comb[:].rearrange("p t (g e) -> p t g e", e=4), in0=R3(0).unsqueeze(3).to_broadcast([128, NOWN, 4, 4]),
                                          in1=R3(6).unsqueeze(2).to_broadcast([128, NOWN, 4, 4]), op=ALU.mult),
         r=["r_ohg", "r_ee"], w=[("comb", i) for i in range(NOWN)])

    d_h1 = dbg_out("h1", [128, NOWN * D])
    d_comb = dbg_out("comb", [128, NOWN * 16])
    if dbg:
        for i in range(NOWN):
            P.dma("sp", d_h1[:, i * D:(i + 1) * D], h1[:, i, :], r=[("h1", i)], key=("dbgo", 16, i))
        P.dma("sp", d_comb, comb[:].rearrange("p a b -> p (a b)"), r=[("comb", i) for i in range(NOWN)], key=("dbgo", 17))
    if stop_after == "B2":
        P.finalize()
        nc.in_names = in_names
        return nc

    o = TB + 32 * KB
    M_sg = [view(o, [128, 512], BF16, [("Msg", 0)]), view(o + KB, [128, 512], BF16, [("Msg", 1)])]; o += 2 * KB
    M_hid = [view(o, [128, 4, 512], BF16, [("Mhid", 0)]), view(o + 4 * KB, [128, 4, 512], BF16, [("Mhid", 1)])]; o += 8 * KB
    first_c = True
    HN2T_ALL = [("hn2T", i) for i in range(NOWN)]
    gcount = 0
    ycount = 0
    for ex in range(16):
        eb = ex % 2
        Wg_, Wu_, Wd_ = Wexp[eb]
        if ex == 0:
            P.alias(("Wg", 0), ["Wo"])
            P.alias(("Wu", 0), ["Wo"])
            P.alias(("Wd", 0), ["Wo", "Wown"])
        if ex == 1:
            cts = [("catT", i) for i in range(NOWN)]
            P.alias(("Wg", 1), cts)
            P.alias(("Wu", 1), cts)
            P.alias(("Wd", 1), cts)
        P.dma("pool", Wg_, wg_d[ex].rearrange("(c p) n -> p c n", p=128), w=[("Wg", eb)])
        P.dma("pool", Wu_, wu_d[ex].rearrange("(c p) n -> p c n", p=128), w=[("Wu", eb)])
        P.dma("pool", Wd_, wd_d[ex].rearrange("(c p) n -> p c n", p=128), w=[("Wd", eb)])
        for grp in range(4):
            hb_ = (ex * 4 + grp) % 2
            hid = M_hid[hb_]
            if first_c:
                pass
                pass
                P.alias(("Msg", 0), [("xo", 0), ("xo", 1)])
                P.alias(("Msg", 1), [("xo", 0), ("xo", 1)])
                first_c = False
            rk = [("hn2T", 4 * grp + t_) for t_ in range(4)]
            for fc in range(4):
                gb_ = 0 if gcount % 2 == 0 else 2
                sgb = gcount % 2
                gcount += 1
                for dc in range(8):
                    P.op("pe", lambda e, dc=dc, fc=fc, grp=grp, gb_=gb_, Wg_=Wg_: e.matmul(bank(gb_), lhsT=Wg_[:, dc, fc * 128:(fc + 1) * 128],
                                                                                         rhs=hn2T[:, dc, grp * 512:(grp + 1) * 512], start=(dc == 0), stop=(dc == 7)),
                         r=[("Wg", eb)] + rk, w=bk(gb_))
                for dc in range(8):
                    P.op("pe", lambda e, dc=dc, fc=fc, grp=grp, gb_=gb_, Wu_=Wu_: e.matmul(bank(gb_ + 1), lhsT=Wu_[:, dc, fc * 128:(fc + 1) * 128],
                                                                                         rhs=hn2T[:, dc, grp * 512:(grp + 1) * 512], start=(dc == 0), stop=(dc == 7)),
                         r=[("Wu", eb)] + rk, w=bk(gb_ + 1))
                P.op("act", lambda e, gb_=gb_, sgb=sgb: e.activation(out=M_sg[sgb], in_=bank(gb_), func=AF.Silu), r=bk(gb_), w=[("Msg", sgb)])
                P.op("dve", lambda e, gb_=gb_, sgb=sgb, fc=fc, hid=hid: e.tensor_tensor(out=hid[:, fc, :], in0=bank(gb_ + 1), in1=M_sg[sgb], op=ALU.mult),
                     r=bk(gb_ + 1) + [("Msg", sgb)], w=[("Mhid", hb_)])
            for tl in range(4):
                tile = 4 * grp + tl
                for half in range(2):
                    yb = 4 + ycount % 4
                    ycount += 1
                    for fc in range(4):
                        P.op("pe", lambda e, fc=fc, tl=tl, half=half, yb=yb, hid=hid, Wd_=Wd_: e.matmul(bank(yb), lhsT=hid[:, fc, tl * 128:(tl + 1) * 128],
                                                                                                      rhs=Wd_[:, fc, half * 512:(half + 1) * 512], start=(fc == 0), stop=(fc == 3)),
                             r=[("Mhid", hb_), ("Wd", eb)], w=bk(yb))
                    P.op("dve", lambda e, tile=tile, half=half, yb=yb, ex=ex: e.scalar_tensor_tensor(
                        out=h1[:, tile, half * 512:(half + 1) * 512], in0=bank(yb), scalar=comb[:, tile, ex:ex + 1],
                        in1=h1[:, tile, half * 512:(half + 1) * 512], op0=ALU.mult, op1=ALU.add),
                        r=bk(yb) + [("comb", tile), ("h1", tile)], w=[("h1", tile)])

    d_h2 = dbg_out("h2", [128, NOWN * D])
    if dbg:
        for i in range(NOWN):
            P.dma("sp", d_h2[:, i * D:(i + 1) * D], h1[:, i, :], r=[("h1", i)], key=("dbgo", 18, i))

    P.dma("pool", Wpg, wpg_d.rearrange("(c p) n -> p c n", p=128), w=["Wpg"])
    P.dma("pool", Wpp, wpp_d.rearrange("(c p) n -> p c n", p=128), w=["Wpp"])
    o = TB + 20 * KB
    D_pT = [view(o, [128, 2, 128], BF16, [("pT", 0)]), view(o + 512, [128, 2, 128], BF16, [("pT", 1)])]; o += KB
    D_gple = view(o, [128, D], F32, ["gple"]); o += 4 * KB
    D_gfin = view(o, [128, D], F32, ["gfin"]); o += 4 * KB
    D_hn3 = [view(o, [128, D], BF16, [("hn3", 0)]), view(o + 2 * KB, [128, D], BF16, [("hn3", 1)])]; o += 4 * KB
    D_hn3T = [view(o, [128, 8, 128], BF16, [("hn3T", 0)]), view(o + 2 * KB, [128, 8, 128], BF16, [("hn3T", 1)])]; o += 4 * KB
    D_sgt = [view(o, [128, D], F32, [("sgt", 0, 0), ("sgt", 0, 1)]), view(o + 4 * KB, [128, D], F32, [("sgt", 1, 0), ("sgt", 1, 1)])]; o += 8 * KB
    D_junk = view(o, [128, D], BF16, ["Djunk"]); o += 2 * KB
    D_out = [view(o, [128, D], F32, [("Dout", 0)]), view(o + 4 * KB, [128, D], F32, [("Dout", 1)])]; o += 8 * KB
    D_s = view(o, [128, 8], F32, [("Drs", 0), ("Drs", 1), ("Drs2", 0), ("Drs2", 1)]); o += 32
    assert o <= ARENA
    P.dma("sp", D_gple, gple_d, w=["gple"])
    P.dma("sp", D_gfin, gfin_d, w=["gfin"])

    D_q = view(o, [128, 4, NOWN], F32, ["rstd3", "rstd4"] + [("ssq3", i_) for i_ in range(NOWN)] + [("ssq4", i_) for i_ in range(NOWN)]); o += 256
    assert o <= ARENA
    for i in range(NOWN):
        P.op("act", lambda e, i=i: e.activation(out=D_junk, in_=h1[:, i, :], func=AF.Square, accum_out=D_q[:, 0, i:i + 1]), r=[("h1", i)], w=["Djunk", ("ssq3", i)])
    P.op("act", lambda e: e.activation(out=D_q[:, 1, :], in_=D_q[:, 0, :], func=AF.Sqrt, scale=1.0 / D, bias=EPS), r=[("ssq3", i) for i in range(NOWN)], w=["rstd3"])
    P.op("dve", lambda e: e.reciprocal(out=D_q[:, 1, :], in_=D_q[:, 1, :]), r=["rstd3"], w=["rstd3"])

    def D_front(i):
        ib = i % 2
        gbk = 0 if ib == 0 else 4
        trb = 6 + ib
        P.dma("pool", D_pT[ib], pTo_d[:, i * 128:(i + 1) * 128].rearrange("(c p) t -> p c t", p=128), w=[("pT", ib)])
        P.op("dve", lambda e: e.scalar_tensor_tensor(out=D_hn3[ib], in0=h1[:, i, :], scalar=D_q[:, 1, i:i + 1], in1=D_gple, op0=ALU.mult, op1=ALU.mult),
             r=[("h1", i), "rstd3", "gple"], w=[("hn3", ib)])
        tc = bankb(trb).rearrange("p (k t) -> p k t", t=128)
        for kc in range(8):
            P.op("pe", lambda e, kc=kc: e.transpose(tc[:, kc, :], D_hn3[ib][:, kc * 128:(kc + 1) * 128], identb[:]), r=[("hn3", ib), "identb"], w=bk(trb))
        P.op("act", lambda e: e.activation(out=D_hn3T[ib], in_=tc, func=AF.Copy), r=bk(trb), w=[("hn3T", ib)])
        for half in range(2):
            for dc in range(8):
                P.op("pe", lambda e, dc=dc, half=half: e.matmul(bank(gbk + half), lhsT=D_hn3T[ib][:, dc, :], rhs=Wpg[:, dc, half * 512:(half + 1) * 512],
                                                              start=(dc == 0), stop=(dc == 7)), r=[("hn3T", ib), "Wpg"], w=bk(gbk + half))

    def D_back(i):
        ib = i % 2
        gbk = 0 if ib == 0 else 4
        sg = D_sgt[ib]
        for half in range(2):
            for kc in range(2):
                P.op("pe", lambda e, kc=kc, half=half: e.matmul(bank(2 + half), lhsT=D_pT[ib][:, kc, :], rhs=Wpp[:, kc, half * 512:(half + 1) * 512],
                                                              start=(kc == 0), stop=(kc == 1)), r=[("pT", ib), "Wpp"], w=bk(2 + half))
        for half in range(2):
            P.op("act", lambda e, half=half: e.activation(out=sg[:, half * 512:(half + 1) * 512], in_=bank(gbk + half), func=AF.Sigmoid), r=bk(gbk + half), w=[("sgt", ib, half)])
            P.op("dve", lambda e, half=half: e.tensor_tensor(out=sg[:, half * 512:(half + 1) * 512], in0=bank(2 + half), in1=sg[:, half * 512:(half + 1) * 512], op=ALU.mult),
                 r=bk(2 + half) + [("sgt", ib, half)], w=[("sgt", ib, half)])
        P.op("pool", lambda e: e.tensor_tensor(out=h1[:, i, :], in0=h1[:, i, :], in1=sg, op=ALU.add), r=[("h1", i), ("sgt", ib, 0), ("sgt", ib, 1)], w=[("h1", i)])
        P.op("act", lambda e: e.activation(out=D_junk, in_=h1[:, i, :], func=AF.Square, accum_out=D_q[:, 2, i:i + 1]), r=[("h1", i)], w=["Djunk", ("ssq4", i)])

    for i in range(NOWN):
        D_front(i)
        if i > 0:
            D_back(i - 1)
    D_back(NOWN - 1)
    P.op("act", lambda e: e.activation(out=D_q[:, 3, :], in_=D_q[:, 2, :], func=AF.Sqrt, scale=1.0 / D, bias=EPS), r=[("ssq4", i) for i in range(NOWN)], w=["rstd4"])
    P.op("dve", lambda e: e.reciprocal(out=D_q[:, 3, :], in_=D_q[:, 3, :]), r=["rstd4"], w=["rstd4"])
    for i in range(NOWN):
        ib = i % 2
        P.op("dve", lambda e, i=i, ib=ib: e.scalar_tensor_tensor(out=D_out[ib], in0=h1[:, i, :], scalar=D_q[:, 3, i:i + 1], in1=D_gfin, op0=ALU.mult, op1=ALU.mult),
             r=[("h1", i), "rstd4", "gfin"], w=[("Dout", ib)])
        P.dma("sp", out_d[i], D_out[ib], r=[("Dout", ib)], key=("outq", ib))

    P.finalize()
    nc.in_names = in_names
    return nc


def host_inputs(inp):
    f = np.float32
    x = np.asarray(inp["x"], f)
    p = np.asarray(inp["p"], f)[0]
    w_in = np.asarray(inp["w_in"], f)[0]
    HD = 64
    OFF_Q, OFF_KV, OFF_GATE = 1024, 1536, 2304
    kvc = [w_in[:, OFF_KV + j * 128: OFF_KV + (j + 1) * 128] for j in range(6)]
    wkv = np.concatenate([kvc[0], kvc[2], kvc[4], kvc[1], kvc[3], kvc[5]], axis=1)
    wq = w_in[:, OFF_Q:OFF_KV].reshape(D, 8, HD)
    wq_r = np.concatenate([np.concatenate([wq[:, j], wq[:, 4 + j]], axis=1) for j in range(4)], axis=1)
    wown = np.concatenate([w_in[:, 0:1024], wq_r, w_in[:, OFF_GATE:OFF_GATE + 24]], axis=1)
    shared = {
        "wkv": np.ascontiguousarray(wkv), "wown": np.ascontiguousarray(wown),
        "gmix": np.ascontiguousarray(np.asarray(inp["norm_mix"], f)[0].reshape(8, 128).T),
        "wsT": np.ascontiguousarray(np.asarray(inp["gmlp_w_s"], f)[0].transpose(2, 0, 1).reshape(128, 8 * 128)),
        "tri": np.triu(np.ones((128, 128), f)),
        "bsT": np.ascontiguousarray(np.asarray(inp["gmlp_b_s"], f)[0].T),
        "gv": np.ascontiguousarray(np.broadcast_to(np.asarray(inp["gmlp_v_norm"], f)[0][None, :], (128, 512))),
        "ga": np.ascontiguousarray(np.broadcast_to(np.asarray(inp["out_norm_a"], f)[0][None, :], (128, 512))),
        "gb": np.ascontiguousarray(np.broadcast_to(np.asarray(inp["out_norm_b"], f)[0][None, :], (128, 512))),
        "gmoe": np.ascontiguousarray(np.broadcast_to(np.asarray(inp["norm_moe"], f)[0][None, :], (128, D))),
        "gple": np.ascontiguousarray(np.broadcast_to(np.asarray(inp["norm_ple"], f)[0][None, :], (128, D))),
        "gfin": np.ascontiguousarray(np.broadcast_to(np.asarray(inp["norm_final"], f)[None, :], (128, D))),
        "w1k": np.ascontiguousarray(np.asarray(inp["cmp_w1_k"], f)[0].reshape(32, 64, 256).transpose(1, 0, 2).reshape(64, 32 * 256)),
        "w1v": np.ascontiguousarray(np.asarray(inp["cmp_w1_v"], f)[0].reshape(32, 64, 256).transpose(1, 0, 2).reshape(64, 32 * 256)),
        "peTk": np.ascontiguousarray(np.asarray(inp["cmp_pe_k"], f)[0].T),
        "peTv": np.ascontiguousarray(np.asarray(inp["cmp_pe_v"], f)[0].T),
        "w2k": np.ascontiguousarray(np.asarray(inp["cmp_w2_k"], f)[0]),
        "w2v": np.ascontiguousarray(np.asarray(inp["cmp_w2_v"], f)[0]),
        "wo": np.ascontiguousarray(np.asarray(inp["w_o"], f)[0]),
        "rcat": np.ascontiguousarray(np.concatenate([np.asarray(inp["router_group"], f)[0], np.asarray(inp["router_expert"], f)[0]], axis=1)),
        "moe_wg": np.ascontiguousarray(np.asarray(inp["moe_w_gate"], f)[0]),
        "moe_wu": np.ascontiguousarray(np.asarray(inp["moe_w_up"], f)[0]),
        "moe_wd": np.ascontiguousarray(np.asarray(inp["moe_w_down"], f)[0]),
        "wpp": np.ascontiguousarray(np.asarray(inp["w_ple_proj"], f)[0]),
        "wpg": np.ascontiguousarray(np.asarray(inp["w_ple_gate"], f)[0]),
        "identf": np.eye(128, dtype=f),
    }
    half = 32
    inv = (1.0 / (np.float32(10000.0) ** (np.arange(half, dtype=f) / np.float32(half)))).astype(f)
    ang = (np.arange(T, dtype=f)[:, None] * inv[None, :]).astype(f)
    cs = np.concatenate([np.cos(ang), np.sin(ang)], axis=1).astype(f)
    shared["cs"] = cs
    ew = np.zeros((128, 64, 128), f)
    for kt in range(64):
        ew[2 * kt, kt, 0:64] = 1.0
        ew[2 * kt + 1, kt, 64:128] = 1.0
    shared["ewide"] = ew.reshape(128, 64 * 128)
    cidx = np.arange(512)
    sidx = np.arange(128)
    ovl = ((cidx[:, None] * 16 < sidx[None, :] * 64 + 64) & (cidx[:, None] * 16 + 32 > sidx[None, :] * 64)).astype(f)
    ovl[511, :] = 0.0
    shared["ovl"] = ovl
    kk = np.arange(128)[:, None]
    tt = np.arange(128)[None, :]
    tri_m = (kk <= tt).astype(f)
    ntri_m = (kk > tt).astype(f)
    onesm = np.ones((128, 128), f)
    zerom = np.zeros((128, 128), f)

    maps = []
    for core in range(8):
        b, c = core // 4, core % 4
        own_tiles = [4 * i + c for i in range(NOWN)]
        tok = np.concatenate([np.arange(128 * qt, 128 * qt + 128) for qt in own_tiles])
        m = dict(shared)
        m["xT"] = np.ascontiguousarray(x[b].T)
        m["xo"] = np.ascontiguousarray(x[b][tok].reshape(NOWN, 128, D))
        m["xTo"] = np.ascontiguousarray(x[b][tok].reshape(NOWN, 128, D).transpose(0, 2, 1))
        m["pTo"] = np.ascontiguousarray(p[b][tok].T)
        m["cso"] = np.ascontiguousarray(cs[tok])
        dm = []
        for r in range(4):
            dm.append(onesm if r < c else (tri_m if r == c else zerom))
        for r in range(8):
            dlt = r - 4 - c
            dm.append(ntri_m if dlt == -4 else (onesm if -3 <= dlt <= -1 else (tri_m if dlt == 0 else zerom)))
        m["dmwm"] = np.ascontiguousarray(((np.stack(dm, axis=1) - 1.0) * 30000.0).astype(f).reshape(128, 12 * 128))
        mC = np.zeros((NOWN, 128, 2, 128), f)
        m12 = np.zeros((NOWN, 128, 256), f)
        for i, qt in enumerate(own_tiles):
            n_ct = (32 * i + 30) // 128 + 1
            t = 128 * qt + np.arange(128)
            for k in range(2):
                ct = n_ct - 2 + k
                if ct < 0:
                    continue
                cc = ct * 128 + np.arange(128)
                mC[i, :, k, :] = ((16 * cc[:, None] + 31) <= t[None, :]).astype(f)
            cur = (t // 64)[:, None]
            s = np.arange(128)[None, :]
            forced = (s == 0) | (s == cur) | (s == cur - 1)
            valid = (s * 64) <= t[:, None]
            m12[i, :, 0:128] = np.where(forced, 0.0, np.where(valid, 1.0, 0.0))
            m12[i, :, 128:256] = np.where(forced, FORCE, np.where(valid, 0.0, -FORCE))
        m["mC"] = np.ascontiguousarray(((mC - 1.0) * 30000.0).astype(f).reshape(NOWN, 128, 256))
        m["m12"] = m12
        maps.append(m)
    return maps


_NC_CACHE = {}


def kernel(**inputs):
    maps = host_inputs(inputs)
    if "nc" not in _NC_CACHE:
        _NC_CACHE["nc"] = build_program()
    nc = _NC_CACHE["nc"]
    res = run_bass_kernel_spmd(nc, maps, core_ids=list(range(8)))
    out = np.zeros((2, T, D), np.float32)
    for core in range(8):
        b, c = core // 4, core % 4
        o = np.asarray(res.results[core]["out"], np.float32)
        for i in range(NOWN):
            qt = 4 * i + c
            out[b, 128 * qt:128 * (qt + 1), :] = o[i]
    return out
```
